# Optimizing a Trainium2 kernel written in Bass

```python
import math
import jax, jax.numpy as jnp
from jax import lax
import numpy as np

D_MODEL = 1024
BATCH = 16
SEQ = 2048
DEPTH = 4

HEAD_DIM = 64
N_SB = 4
N_MOBA = 4
N_FOX = 4
N_DSA = 4
BRANCH_W = 4 * HEAD_DIM
N_BRANCH = 4
Q_BLOCK = 128
MOBA_BLOCK = 256
MOBA_TOPK = 3
MOBA_Q_CHUNK = 32
DSA_TOPK = 256
IDX_HEADS = 8
IDX_DIM = 32
T5_BUCKETS = 32
T5_MAX_DIST = 128
D_FF = 2816
CONV_W = 3
RMS_EPS = 1e-6
NEG_BIG = -1e30

IN_SIZES = (
    [BRANCH_W] * 3
    + [BRANCH_W] * 3
    + [BRANCH_W] * 3 + [N_FOX]
    + [BRANCH_W, HEAD_DIM, HEAD_DIM]
    + [IDX_HEADS * IDX_DIM, IDX_DIM, IDX_HEADS]
)
P_IN = int(sum(IN_SIZES))
IN_SPLITS = tuple(int(v) for v in np.cumsum(IN_SIZES)[:-1])

kernel_name = "hybrid_gated_sb_moba_fox_dsa_trunk"


def rmsnorm(x, g):
    xf = x.astype(jnp.float32)
    y = xf * lax.rsqrt(jnp.mean(xf * xf, axis=-1, keepdims=True) + RMS_EPS)
    return (y * g.astype(jnp.float32)).astype(x.dtype)


def to_heads(t, n):
    b, s, _ = t.shape
    return t.reshape(b, s, n, HEAD_DIM).transpose(0, 2, 1, 3)


def merge_heads(o):
    b, h, s, d = o.shape
    return o.transpose(0, 2, 1, 3).reshape(b, s, h * d)


def t5_bucket(n):
    n = jnp.maximum(n, 0)
    max_exact = T5_BUCKETS // 2
    nf = jnp.maximum(n, 1).astype(jnp.float32)
    large = max_exact + (jnp.log(nf / max_exact) / math.log(T5_MAX_DIST / max_exact)
                         * (T5_BUCKETS - max_exact)).astype(jnp.int32)
    large = jnp.minimum(large, T5_BUCKETS - 1)
    return jnp.where(n < max_exact, n, large)


def stick_breaking_attention(q, k, v):
    s_len = q.shape[2]
    scale = HEAD_DIM ** -0.5
    outs = []
    for i in range(s_len // Q_BLOCK):
        t0, t1 = i * Q_BLOCK, (i + 1) * Q_BLOCK
        z = jnp.einsum('bhqd,bhkd->bhqk', q[:, :, t0:t1], k[:, :, :t1]).astype(jnp.float32) * scale
        strict = jnp.arange(t1)[None, :] < jnp.arange(t0, t1)[:, None]
        log_neg = jnp.where(strict, jax.nn.log_sigmoid(-z), 0.0)
        excl = lax.cumsum(log_neg, axis=3, reverse=True) - log_neg
        w = jnp.where(strict, jnp.exp(jax.nn.log_sigmoid(z) + excl), 0.0)
        outs.append(jnp.einsum('bhqk,bhkd->bhqd', w.astype(v.dtype), v[:, :, :t1]))
    return jnp.concatenate(outs, axis=2)


def forgetting_attention(q, k, v, f_logit):
    s_len = q.shape[2]
    scale = HEAD_DIM ** -0.5
    c = jnp.cumsum(jax.nn.log_sigmoid(f_logit.astype(jnp.float32)), axis=1).transpose(0, 2, 1)
    outs = []
    for i in range(s_len // Q_BLOCK):
        t0, t1 = i * Q_BLOCK, (i + 1) * Q_BLOCK
        logits = jnp.einsum('bhqd,bhkd->bhqk', q[:, :, t0:t1], k[:, :, :t1]).astype(jnp.float32) * scale
        logits = logits + c[:, :, t0:t1, None] - c[:, :, None, :t1]
        causal = jnp.arange(t1)[None, :] <= jnp.arange(t0, t1)[:, None]
        p = jax.nn.softmax(jnp.where(causal, logits, NEG_BIG), axis=-1)
        outs.append(jnp.einsum('bhqk,bhkd->bhqd', p.astype(v.dtype), v[:, :, :t1]))
    return jnp.concatenate(outs, axis=2)


def moba_attention(q, k, v, bias_table):
    b, h, s_len, d = q.shape
    scale = HEAD_DIM ** -0.5
    nb = -(-s_len // MOBA_BLOCK)
    pad = nb * MOBA_BLOCK - s_len
    kp = jnp.pad(k, ((0, 0), (0, 0), (0, pad), (0, 0)))
    vp = jnp.pad(v, ((0, 0), (0, 0), (0, pad), (0, 0)))
    kb = kp.reshape(b, h, nb, MOBA_BLOCK, d)
    vb = vp.reshape(b, h, nb, MOBA_BLOCK, d)
    kmean = jnp.mean(kb.astype(jnp.float32), axis=3).astype(k.dtype)
    topk = min(MOBA_TOPK, nb)
    table_t = bias_table.T
    bi = jnp.arange(b)[:, None, None, None]
    hi = jnp.arange(h)[None, :, None, None]

    def chunk(ci):
        t0 = ci * MOBA_Q_CHUNK
        qc = lax.dynamic_slice_in_dim(q, t0, MOBA_Q_CHUNK, axis=2)
        tpos = t0 + jnp.arange(MOBA_Q_CHUNK)
        own = t0 // MOBA_BLOCK
        gate = jnp.einsum('bhqd,bhnd->bhqn', qc, kmean).astype(jnp.float32)
        past = jnp.arange(nb) < own
        gate = jnp.where(past, gate, -jnp.inf)
        _, gidx = lax.top_k(gate, topk)
        sel_valid = gidx < own
        ks = kb[bi, hi, gidx]
        vs = vb[bi, hi, gidx]
        s_sel = jnp.einsum('bhqd,bhqnkd->bhqnk', qc, ks).astype(jnp.float32) * scale
        kpos = gidx[..., None] * MOBA_BLOCK + jnp.arange(MOBA_BLOCK)
        bucket = t5_bucket(tpos[None, None, :, None, None] - kpos)
        s_sel = s_sel + table_t[hi[..., None], bucket].astype(jnp.float32)
        s_sel = jnp.where(sel_valid[..., None], s_sel, NEG_BIG)
        ko = lax.dynamic_slice_in_dim(kp, own * MOBA_BLOCK, MOBA_BLOCK, axis=2)
        vo = lax.dynamic_slice_in_dim(vp, own * MOBA_BLOCK, MOBA_BLOCK, axis=2)
        kpos_own = own * MOBA_BLOCK + jnp.arange(MOBA_BLOCK)
        dist_own = tpos[:, None] - kpos_own[None, :]
        s_own = jnp.einsum('bhqd,bhkd->bhqk', qc, ko).astype(jnp.float32) * scale
        s_own = s_own + table_t[:, t5_bucket(dist_own)].astype(jnp.float32)[None]
        s_own = jnp.where(dist_own >= 0, s_own, NEG_BIG)
        logits = jnp.concatenate([s_sel.reshape(b, h, MOBA_Q_CHUNK, topk * MOBA_BLOCK), s_own], axis=-1)
        p = jax.nn.softmax(logits, axis=-1).astype(v.dtype)
        p_sel = p[..., :topk * MOBA_BLOCK].reshape(b, h, MOBA_Q_CHUNK, topk, MOBA_BLOCK)
        p_own = p[..., topk * MOBA_BLOCK:]
        return (jnp.einsum('bhqnk,bhqnkd->bhqd', p_sel, vs)
                + jnp.einsum('bhqk,bhkd->bhqd', p_own, vo))

    out = lax.map(chunk, jnp.arange(s_len // MOBA_Q_CHUNK))
    return out.transpose(1, 2, 0, 3, 4).reshape(b, h, s_len, d)


def dsa_attention(q, k, v, q_idx, k_idx, w_idx, bias_table):
    b, h, s_len, d = q.shape
    scale = HEAD_DIM ** -0.5
    ksel = min(DSA_TOPK, s_len // 4)
    table_t = bias_table.T
    bi = jnp.arange(b)[:, None, None]
    outs = []
    for i in range(s_len // Q_BLOCK):
        t0, t1 = i * Q_BLOCK, (i + 1) * Q_BLOCK
        kk = min(ksel, t1)
        tpos = jnp.arange(t0, t1)
        isc = jax.nn.relu(jnp.einsum('bqhe,bke->bqhk', q_idx[:, t0:t1], k_idx[:, :t1]))
        isc = jnp.einsum('bqh,bqhk->bqk', w_idx[:, t0:t1], isc).astype(jnp.float32)
        admissible = jnp.arange(t1)[None, :] <= tpos[:, None]
        _, idx = lax.top_k(jnp.where(admissible, isc, -jnp.inf), kk)
        dist = tpos[None, :, None] - idx
        ks = k[bi, idx]
        vs = v[bi, idx]
        logits = jnp.einsum('bhqd,bqkd->bhqk', q[:, :, t0:t1], ks).astype(jnp.float32) * scale
        logits = logits + table_t[:, t5_bucket(dist)].transpose(1, 0, 2, 3).astype(jnp.float32)
        logits = jnp.where((dist >= 0)[:, None], logits, NEG_BIG)
        p = jax.nn.softmax(logits, axis=-1).astype(v.dtype)
        outs.append(jnp.einsum('bhqk,bqkd->bhqd', p, vs))
    return jnp.concatenate(outs, axis=2)


def token_mixer(xn, w_in, fox_b_f, t5_bias, w_gate, w_branch, w_out):
    b, s_len, _ = xn.shape
    proj = xn @ w_in
    (sb_q, sb_k, sb_v, mb_q, mb_k, mb_v, fx_q, fx_k, fx_v, fx_f,
     ds_q, ds_k, ds_v, ix_q, ix_k, ix_w) = jnp.split(proj, IN_SPLITS, axis=-1)
    o_sb = stick_breaking_attention(to_heads(sb_q, N_SB), to_heads(sb_k, N_SB), to_heads(sb_v, N_SB))
    o_mb = moba_attention(to_heads(mb_q, N_MOBA), to_heads(mb_k, N_MOBA), to_heads(mb_v, N_MOBA),
                          t5_bias[:, :N_MOBA])
    o_fx = forgetting_attention(to_heads(fx_q, N_FOX), to_heads(fx_k, N_FOX), to_heads(fx_v, N_FOX),
                                fx_f + fox_b_f)
    o_ds = dsa_attention(to_heads(ds_q, N_DSA), ds_k, ds_v,
                         ix_q.reshape(b, s_len, IDX_HEADS, IDX_DIM), ix_k, ix_w, t5_bias[:, N_MOBA:])
    branches = (o_sb, o_mb, o_fx, o_ds)
    mixed = None
    for i in range(N_BRANCH):
        y = jax.nn.sigmoid(xn @ w_gate[i]) * (merge_heads(branches[i]) @ w_branch[i])
        mixed = y if mixed is None else mixed + y
    return mixed @ w_out


def conv_ffn(xn, w_up, conv_w, conv_b, w_down):
    s_len = xn.shape[1]
    hdn = xn @ w_up
    hp = jnp.pad(hdn, ((0, 0), (CONV_W - 1, 0), (0, 0)))
    acc = conv_b
    for j in range(CONV_W):
        acc = acc + conv_w[j] * hp[:, j:j + s_len]
    g, u = jnp.split(acc, 2, axis=-1)
    return (jax.nn.gelu(g, approximate=False) * u) @ w_down


def setup_inputs(seed: int = 0) -> dict:
    key = jax.random.key(seed)
    ks = jax.random.split(key, 14)
    nrm = jax.random.normal
    f32 = jnp.float32
    return {
        "x": nrm(ks[0], (BATCH, SEQ, D_MODEL), f32),
        "norm_mix_g": 1.0 + 0.02 * nrm(ks[1], (DEPTH, D_MODEL), f32),
        "norm_ffn_g": 1.0 + 0.02 * nrm(ks[2], (DEPTH, D_MODEL), f32),
        "norm_final_g": 1.0 + 0.02 * nrm(ks[3], (D_MODEL,), f32),
        "w_in": nrm(ks[4], (DEPTH, D_MODEL, P_IN), f32) * D_MODEL ** -0.5,
        "fox_b_f": 1.0 + 0.1 * nrm(ks[5], (DEPTH, N_FOX), f32),
        "t5_bias": 0.5 * nrm(ks[6], (T5_BUCKETS, N_MOBA + N_DSA), f32),
        "w_gate": nrm(ks[7], (DEPTH, N_BRANCH, D_MODEL, D_MODEL), f32) * D_MODEL ** -0.5,
        "w_branch": nrm(ks[8], (DEPTH, N_BRANCH, BRANCH_W, D_MODEL), f32) * BRANCH_W ** -0.5,
        "w_out": nrm(ks[9], (DEPTH, D_MODEL, D_MODEL), f32) * D_MODEL ** -0.5,
        "w_up": nrm(ks[10], (DEPTH, D_MODEL, 2 * D_FF), f32) * D_MODEL ** -0.5,
        "conv_w": nrm(ks[11], (DEPTH, CONV_W, 2 * D_FF), f32) * CONV_W ** -0.5,
        "conv_b": 0.01 * nrm(ks[12], (DEPTH, 2 * D_FF), f32),
        "w_down": nrm(ks[13], (DEPTH, D_FF, D_MODEL), f32) * D_FF ** -0.5,
    }


def reference(x, norm_mix_g, norm_ffn_g, norm_final_g, w_in, fox_b_f, t5_bias,
              w_gate, w_branch, w_out, w_up, conv_w, conv_b, w_down):
    for l in range(DEPTH):
        x = x + token_mixer(rmsnorm(x, norm_mix_g[l]), w_in[l], fox_b_f[l], t5_bias,
                            w_gate[l], w_branch[l], w_out[l])
        x = x + conv_ffn(rmsnorm(x, norm_ffn_g[l]), w_up[l], conv_w[l], conv_b[l], w_down[l])
    return rmsnorm(x, norm_final_g)
```

```python
import math
from contextlib import ExitStack
import numpy as np
import concourse.bass as bass
import concourse.mybir as mybir
from concourse.bass_utils import run_bass_kernel_spmd

F32 = mybir.dt.float32
BF16 = mybir.dt.bfloat16
AF = mybir.ActivationFunctionType
ALU = mybir.AluOpType
AX = mybir.AxisListType

D = 1024
HD = 64
PIN = 2988
DFF = 2816
NJ = DFF // 128
C_SB, C_MB, C_FX, C_FXF, C_DQ, C_DK, C_DV, C_IQ, C_IK, C_IW = 0, 768, 1536, 2304, 2308, 2564, 2628, 2692, 2948, 2980
NEG = -30000.0
NIT = 18
SAME_ENGINE_SYNC = True


class Sem:
    def __init__(self, h):
        self.h = h
        self.cnt = 0


class Eng:
    def __init__(self, e, sem, name):
        self.e = e
        self.sem = sem
        self.seen = {}
        self.name = name


class Reg:
    __slots__ = ("w", "r", "name")

    def __init__(self, name=""):
        self.w = None
        self.r = {}
        self.name = name


class KB:
    def __init__(self, nc, es):
        self.nc = nc
        self.es = es
        mk = lambda n: Sem(es.enter_context(nc.semaphore(n)))
        self.pe = Eng(nc.tensor, mk("s_pe"), "pe")
        self.act = Eng(nc.scalar, mk("s_act"), "act")
        self.dve = Eng(nc.vector, mk("s_dve"), "dve")
        self.pool = Eng(nc.gpsimd, mk("s_pool"), "pool")
        self.sp = Eng(nc.sync, mk("s_sp"), "sp")
        self.engs = [self.pe, self.act, self.dve, self.pool, self.sp]
        self.dsems = {"pool": [mk("d_pool%d" % i) for i in range(12)],
                      "sp": [mk("d_sp%d" % i) for i in range(12)]}
        self.drr = {"pool": 0, "sp": 0}
        self.allsems = [e.sem for e in self.engs] + self.dsems["pool"] + self.dsems["sp"]

    def _wait(self, E, deps):
        need = {}
        for (sem, val) in deps:
            if sem is E.sem and (E.name == "pe" or not SAME_ENGINE_SYNC):
                continue
            if need.get(sem, 0) < val:
                need[sem] = val
        for sem, val in need.items():
            if E.seen.get(sem, 0) < val:
                E.e.wait_ge(sem.h, val)
                E.seen[sem] = val

    def _deps(self, reads, writes):
        deps = []
        for r in reads:
            if r.w is not None:
                deps.append(r.w)
        for w in writes:
            if w.w is not None:
                deps.append(w.w)
            deps.extend(w.r.items())
        return deps

    def _record(self, tok, reads, writes):
        sem, val = tok
        for r in reads:
            if r.r.get(sem, 0) < val:
                r.r[sem] = val
        for w in writes:
            w.w = tok
            w.r = {}

    def op(self, E, reads, writes, fn, inc=True):
        self._wait(E, self._deps(reads, writes))
        ins = fn()
        if inc:
            E.sem.cnt += 1
            ins.then_inc(E.sem.h, 1)
            tok = (E.sem, E.sem.cnt)
        else:
            tok = (E.sem, E.sem.cnt + 1)
        self._record(tok, reads, writes)
        return tok

    def dma(self, Q, out, in_, reads, writes):
        sems = self.dsems[Q.name]
        s = sems[self.drr[Q.name] % len(sems)]
        self.drr[Q.name] += 1
        deps = self._deps(reads, writes)
        if s.cnt > 0:
            deps.append((s, s.cnt))
        self._wait(Q, deps)
        Q.e.dma_start(out=out, in_=in_).then_inc(s.h, 16)
        s.cnt += 16
        tok = (s, s.cnt)
        self._record(tok, reads, writes)
        return tok

    def barrier(self):
        for E in self.engs:
            for s in self.allsems:
                if s is E.sem or s.cnt == 0:
                    continue
                if E.seen.get(s, 0) < s.cnt:
                    E.e.wait_ge(s.h, s.cnt)
                    E.seen[s] = s.cnt


def t5_bucket_np(n):
    n = np.maximum(n, 0)
    nf = np.maximum(n, 1).astype(np.float32)
    large = 16 + (np.log(nf / np.float32(16)) / np.float32(math.log(128 / 16)) * np.float32(16)).astype(np.int32)
    large = np.minimum(large, 31)
    return np.where(n < 16, n, large)


def host_consts(S):
    c = {}
    i = np.arange(128)
    c["c_ident"] = np.eye(128, dtype=np.float32)
    c["c_ident4"] = np.tile(np.eye(128, dtype=np.float32), (1, 4))
    c["c_uinclneg"] = -(i[:, None] >= i[None, :]).astype(np.float32)
    c["c_onesneg"] = -np.ones((128, 128), np.float32)
    c["c_cmstrict"] = np.where(i[:, None] >= i[None, :], NEG, 0.0).astype(np.float32)
    c["c_cmincl"] = np.where(i[:, None] > i[None, :], NEG, 0.0).astype(np.float32)
    c["c_adm"] = np.where(i[None, :] <= i[:, None], 0.0, -1e30).astype(np.float32)
    dd = np.arange(256)[None, :] - i[:, None]
    c["c_buck"] = np.where(dd >= 0, t5_bucket_np(dd), 99).astype(np.float32)
    c["c_tmask"] = np.where(dd >= 0, 0.0, NEG).astype(np.float32)
    c["c_blk"] = (np.arange(S)[None, :] // 256 == np.arange(8)[:, None]).astype(np.float32)
    c["c_ones"] = np.ones((8, S), np.float32)
    c["c_pow"] = np.tile((0.5 ** (np.arange(NIT) + 1)).astype(np.float32)[None, :], (128, 1))
    return c


def build(S, DEPTH, NSEQ, dbg=False):
    NT = S // 128
    NCH = S // 512
    NB = S // 256
    TOPK = min(3, NB)
    KSEL = min(256, S // 4)
    TOKH = min(S, 1024)
    nc = bass.Bass("TRN2", target_bir_lowering=False)
    es = ExitStack()
    with es:
        def din(name, shape):
            return nc.dram_tensor(name, list(shape), F32, kind="ExternalInput").ap()
        x_d = din("x", [NSEQ, S, D])
        y_d = nc.dram_tensor("y", [NSEQ, S, D], F32, kind="ExternalOutput").ap()
        gmix_d = din("norm_mix_g", [DEPTH, D])
        gffn_d = din("norm_ffn_g", [DEPTH, D])
        gfin_d = din("norm_final_g", [1, D])
        win_d = din("w_in", [DEPTH, D, PIN])
        fxb_d = din("fox_b_f", [DEPTH, 4])
        t5_d = din("t5_bias", [1, 256])
        wg_d = din("w_gate", [DEPTH, 4, D, D])
        wb_d = din("w_branch", [DEPTH, 4, 256, D])
        wo_d = din("w_out", [DEPTH, D, D])
        wup_d = din("w_up", [DEPTH, D, 2 * DFF])
        cw_d = din("conv_w", [DEPTH, 3, 2 * DFF])
        cb_d = din("conv_b", [DEPTH, 2 * DFF])
        wdn_d = din("w_down", [DEPTH, DFF, D])
        hc = host_consts(S)
        cst = {k: din(k, v.shape) for k, v in hc.items()}
        dbg_d = nc.dram_tensor("dbg", [6, S, D], F32, kind="ExternalOutput").ap() if dbg else None

        K = KB(nc, es)
        pe, act, dve, pool, sp = K.pe, K.act, K.dve, K.pool, K.sp

        uniq = [0]

        def sb(name, shape, dt, st=None):
            uniq[0] += 1
            return (st or es).enter_context(nc.sbuf_tensor("%s_%d" % (name, uniq[0]), list(shape), dt))

        PS = [es.enter_context(nc.psum_tensor("ps%d" % i, [128, 512], F32)) for i in range(7)]
        PSR = [Reg("ps%d" % i) for i in range(7)]
        PSB = es.enter_context(nc.psum_tensor("psb", [128, 1024], BF16))
        PSBR = Reg("psb")

        ident_f = sb("ident_f", [128, 128], F32)
        ident_b = sb("ident_b", [128, 128], BF16)
        ident4 = sb("ident4", [128, 512], BF16)
        uinclneg = sb("uinclneg", [128, 128], BF16)
        onesneg = sb("onesneg", [128, 128], BF16)
        cmstrict = sb("cmstrict", [128, 128], BF16)
        cmincl = sb("cmincl", [128, 128], BF16)
        adm = sb("adm", [128, 128], F32)
        buck = sb("buck", [128, 256], F32)
        tmask = sb("tmask", [128, 256], F32)
        powt = sb("powt", [128, NIT], F32)
        t5bc = sb("t5bc", [128, 256], F32)
        tbias = sb("tbias", [128, 8, 256], BF16)
        CR = Reg("const")
        for (t, k, q) in [(ident_f, "c_ident", sp), (ident_b, "c_ident", pool), (ident4, "c_ident4", pool),
                          (uinclneg, "c_uinclneg", pool), (onesneg, "c_onesneg", pool), (cmstrict, "c_cmstrict", pool),
                          (cmincl, "c_cmincl", pool), (adm, "c_adm", sp), (buck, "c_buck", sp), (tmask, "c_tmask", sp),
                          (powt, "c_pow", sp)]:
            K.dma(q, t[:], cst[k], [], [CR])
        K.dma(sp, t5bc[:], bass.AP(tensor=t5_d.tensor, offset=0, ap=[[0, 128], [1, 256]]), [], [CR])
        with ExitStack() as st:
            tacc = sb("tacc", [128, 256], F32, st)
            ttmp = sb("ttmp", [128, 256], F32, st)
            TR = Reg("tb")
            for h in range(8):
                for b in range(32):
                    col = t5bc[:, b * 8 + h:b * 8 + h + 1]
                    if b == 0:
                        K.op(dve, [CR], [TR], lambda: nc.vector.tensor_scalar(out=tacc[:], in0=buck[:], scalar1=float(b), scalar2=col, op0=ALU.is_equal, op1=ALU.mult))
                    else:
                        K.op(dve, [CR], [TR], lambda: nc.vector.tensor_scalar(out=ttmp[:], in0=buck[:], scalar1=float(b), scalar2=col, op0=ALU.is_equal, op1=ALU.mult))
                        K.op(dve, [TR], [TR], lambda: nc.vector.tensor_tensor(out=tacc[:], in0=tacc[:], in1=ttmp[:], op=ALU.add))
                c31 = t5bc[:, 31 * 8 + h:31 * 8 + h + 1]
                K.op(dve, [TR, CR], [CR], lambda: nc.vector.scalar_tensor_tensor(out=tbias[:, h, :], in0=tacc[:], scalar=c31, in1=tmask[:], op0=ALU.subtract, op1=ALU.add))
            K.barrier()

        xnT = sb("xnT", [128, 8, S], BF16)
        XNR = [Reg("xnT%d" % t) for t in range(NT)]
        gbc = sb("gbc", [128, D], F32)
        GBR = Reg("gbc")

        def load_g(src_row_ap):
            K.dma(sp, gbc[:], bass.AP(tensor=src_row_ap.tensor, offset=src_row_ap.offset, ap=[[0, 128], [1, D]]), [], [GBR])

        YR = [[Reg("y%d_%d" % (s, t)) for t in range(NT)] for s in range(NSEQ)]

        class NormBufs:
            pass

        def norm_tile(NBF, seq, t, src_ap, delta_banks, final, slot):
            xt = NBF.xt[slot]
            XTR = NBF.xtr[slot]
            K.dma(sp, xt[:], src_ap, [YR[seq][t]], [XTR])
            if delta_banks is not None:
                for hb, (bk, bkr) in enumerate(delta_banks):
                    K.op(dve, [XTR, bkr], [XTR], lambda: nc.vector.tensor_tensor(out=xt[:, hb * 512:(hb + 1) * 512], in0=xt[:, hb * 512:(hb + 1) * 512], in1=bk[:, :], op=ALU.add))
                K.dma(sp, y_d[seq, t * 128:(t + 1) * 128, :], xt[:], [XTR], [YR[seq][t]])
            K.op(act, [XTR], [NBF.sqr[slot]], lambda: nc.scalar.activation(out=NBF.sq[slot][:], in_=xt[:], func=AF.Square, accum_out=NBF.ss[slot][:]))
            K.op(act, [NBF.sqr[slot]], [NBF.sqr[slot]], lambda: nc.scalar.activation(out=NBF.rs[slot][:], in_=NBF.ss[slot][:], func=AF.Sqrt, scale=1.0 / D, bias=NBF.eps[:]))
            K.op(dve, [NBF.sqr[slot]], [NBF.sqr[slot]], lambda: nc.vector.reciprocal(out=NBF.rs[slot][:], in_=NBF.rs[slot][:]))
            if final:
                K.op(dve, [XTR, NBF.sqr[slot], GBR], [XTR], lambda: nc.vector.scalar_tensor_tensor(out=xt[:], in0=xt[:], scalar=NBF.rs[slot][:], in1=gbc[:], op0=ALU.mult, op1=ALU.mult))
                K.dma(sp, y_d[seq, t * 128:(t + 1) * 128, :], xt[:], [XTR], [YR[seq][t]])
                return
            xb = NBF.xb[slot]
            XBR = NBF.xbr[slot]
            K.op(dve, [XTR, NBF.sqr[slot], GBR], [XBR], lambda: nc.vector.scalar_tensor_tensor(out=xb[:], in0=xt[:], scalar=NBF.rs[slot][:], in1=gbc[:], op0=ALU.mult, op1=ALU.mult))
            for c in range(8):
                K.op(pe, [XBR, CR], [PSBR], lambda: nc.tensor.transpose(PSB[:, c * 128:(c + 1) * 128], xb[:, c * 128:(c + 1) * 128], ident_b[:]), inc=(c == 7))
            K.op(act, [PSBR], [XNR[t]], lambda: nc.scalar.copy(out=xnT[:, :, t * 128:(t + 1) * 128], in_=PSB[:, :].rearrange("p (c n) -> p c n", n=128)))

        def alloc_normbufs(st):
            NBF = NormBufs()
            NBF.xt = [sb("nb_xt%d" % i, [128, D], F32, st) for i in range(2)]
            NBF.xtr = [Reg() for i in range(2)]
            NBF.sq = [sb("nb_sq%d" % i, [128, D], BF16, st) for i in range(2)]
            NBF.ss = [sb("nb_ss%d" % i, [128, 1], F32, st) for i in range(2)]
            NBF.rs = [sb("nb_rs%d" % i, [128, 1], F32, st) for i in range(2)]
            NBF.sqr = [Reg() for i in range(2)]
            NBF.xb = [sb("nb_xb%d" % i, [128, D], BF16, st) for i in range(2)]
            NBF.xbr = [Reg() for i in range(2)]
            NBF.eps = sb("nb_eps", [128, 1], F32, st)
            K.op(dve, [], [NBF.sqr[0], NBF.sqr[1]], lambda: nc.vector.memset(NBF.eps[:], 1e-6))
            return NBF

        bank_rr = [0]

        def next_bank(lo=0, hi=7):
            b = lo + bank_rr[0] % (hi - lo)
            bank_rr[0] += 1
            return b

        def proj_fm(W, WR, wcol, M, dst_fn, dst_reg, scale=None, evac_eng=None):
            for n in range(NCH):
                b = next_bank()
                for c in range(8):
                    K.op(pe, [WR] + XNR[n * 4:(n + 1) * 4], [PSR[b]],
                         lambda: nc.tensor.matmul(PS[b][0:M, :], lhsT=W[:, c, wcol:wcol + M], rhs=xnT[:, c, n * 512:(n + 1) * 512], start=(c == 0), stop=(c == 7)), inc=(c == 7))
                dst = dst_fn(n)
                if scale is None:
                    K.op(dve, [PSR[b]], [dst_reg], lambda: nc.vector.tensor_copy(out=dst, in_=PS[b][0:M, :]))
                else:
                    K.op(dve, [PSR[b]], [dst_reg], lambda: nc.vector.tensor_scalar(out=dst, in0=PS[b][0:M, :], scalar1=float(scale), scalar2=None, op0=ALU.mult))

        def proj_tm(W, WR, wcol, ncol, evac_fn):
            for t in range(NT):
                b = next_bank()
                for c in range(8):
                    K.op(pe, [WR, XNR[t]], [PSR[b]],
                         lambda: nc.tensor.matmul(PS[b][:, 0:ncol], lhsT=xnT[:, c, t * 128:(t + 1) * 128], rhs=W[:, c, wcol:wcol + ncol], start=(c == 0), stop=(c == 7)), inc=(c == 7))
                evac_fn(t, PS[b], PSR[b])

        def load_win(l, W, WR, c0, c1, dcol=0):
            K.dma(pool, W[:, :, dcol:dcol + (c1 - c0)], win_d[l][:, c0:c1].rearrange("(c p) n -> p c n", p=128), [], [WR])

        for seq in range(NSEQ):
            with ExitStack() as st:
                NBF = alloc_normbufs(st)
                load_g(gmix_d[0])
                for t in range(NT):
                    slot = t % 2
                    xt = NBF.xt[slot]
                    K.dma(sp, xt[:], x_d[seq, t * 128:(t + 1) * 128, :], [], [NBF.xtr[slot]])
                    K.dma(sp, y_d[seq, t * 128:(t + 1) * 128, :], xt[:], [NBF.xtr[slot]], [YR[seq][t]])
                    norm_tile(NBF, seq, t, y_d[seq, t * 128:(t + 1) * 128, :], None, False, slot)
                K.barrier()

            for l in range(DEPTH):
                with ExitStack() as mst:
                    ast = ExitStack()
                    oT = sb("oT", [128, 4, 2, S], BF16, mst)
                    OTR = [Reg("oT%d" % m) for m in range(4)]
                    QT = [sb("QT%d" % h, [72, S], BF16, ast) for h in range(4)]
                    KT = [sb("KT%d" % h, [72, S], BF16, ast) for h in range(4)]
                    QTR = [Reg("QT%d" % h) for h in range(4)]
                    QAR = [Reg("QA%d" % h) for h in range(4)]
                    KTR = [Reg("KT%d" % h) for h in range(4)]
                    V = sb("V", [128, NT, 4, 65], BF16, ast)
                    VR = Reg("V")
                    OTOK = sb("OTOK", [128, NT, 256], BF16, ast)
                    OKR = Reg("OTOK")
                    W = [sb("Wm%d" % i, [128, 8, 776], BF16, ast) for i in range(2)]
                    WR = [Reg("Wm%d" % i) for i in range(2)]
                    PT = [sb("PT%d" % i, [128, 512], BF16, ast) for i in range(2)]
                    PTR = [Reg("PT%d" % i) for i in range(2)]
                    orec = sb("orec", [128, 4], F32, ast)
                    ORR = Reg("orec")
                    K.op(dve, [], [VR], lambda: nc.vector.memset(V[:], 1.0))

                    def v_evac(nheads):
                        def f(t, ps, psr):
                            K.op(dve, [psr], [VR], lambda: nc.vector.tensor_copy(out=V[:, t, 0:nheads, 0:64], in_=ps[:, 0:nheads * 64].rearrange("p (h d) -> p h d", d=64)))
                        return f

                    def qk_proj(Wt, WRt, base, with_scale=True):
                        for h in range(4):
                            proj_fm(Wt, WRt, base + h * 64, 64, (lambda n, h=h: QT[h][0:64, n * 512:(n + 1) * 512]), QTR[h], scale=0.125)
                            proj_fm(Wt, WRt, base + 256 + h * 64, 64, (lambda n, h=h: KT[h][0:64, n * 512:(n + 1) * 512]), KTR[h])

                    def finish_o(ob, obr, tq0, nsub, h, normalize):
                        o3 = PS[ob][:, 0:nsub * 65].rearrange("p (s d) -> p s d", d=65)
                        dst = OTOK[:, tq0:tq0 + nsub, h * 64:(h + 1) * 64]
                        if normalize:
                            K.op(dve, [obr], [ORR], lambda: nc.vector.reciprocal(out=orec[:, 0:nsub], in_=o3[:, :, 64]))
                            for s_ in range(nsub):
                                K.op(dve, [obr, ORR], [OKR], lambda: nc.vector.tensor_scalar(out=OTOK[:, tq0 + s_, h * 64:(h + 1) * 64], in0=o3[:, s_, 0:64], scalar1=orec[:, s_:s_ + 1], scalar2=None, op0=ALU.mult))
                        else:
                            K.op(dve, [obr], [OKR], lambda: nc.vector.tensor_copy(out=dst, in_=o3[:, :, 0:64]))

                    def otok_to_oT(m):
                        if dbg and seq == 0 and l == 0:
                            K.dma(pool, dbg_d[m, :, 0:256].rearrange("(t p) c -> p t c", p=128), OTOK[:], [OKR], [])
                        for t in range(NT):
                            for k in range(2):
                                K.op(pe, [OKR, CR], [PSBR], lambda: nc.tensor.transpose(PSB[:, k * 128:(k + 1) * 128], OTOK[:, t, k * 128:(k + 1) * 128], ident_b[:]), inc=(k == 1))
                            K.op(act, [PSBR], [OTR[m]], lambda: nc.scalar.copy(out=oT[:, m, :, t * 128:(t + 1) * 128], in_=PSB[:, 0:256].rearrange("p (c n) -> p c n", n=128)))

                    def attn_softmax(h, kdim, near_bias, diag_mask):
                        for cq in range(NCH):
                            ob = 6
                            K.op(dve, [], [PSR[ob]], lambda: nc.vector.memset(PS[ob][:, 0:260], 0.0))
                            t0 = cq * 512
                            na = 4 * cq + 4

                            def stA(a):
                                r = a - 4 * cq
                                off = max(0, r) * 128
                                xb = a % 2
                                extra = []
                                if near_bias is not None:
                                    if r >= 0:
                                        w_ = min(256, 512 - off)
                                        extra.append((ident_b, tbias[:, near_bias, 0:w_], off, w_))
                                    elif r == -1:
                                        extra.append((ident_b, tbias[:, near_bias, 128:256], 0, 128))
                                elif diag_mask is not None and r >= 0:
                                    extra.append((ident_b, diag_mask[:, :], off, 128))
                                K.op(pe, [KTR[h], QTR[h], QAR[h]], [PSR[xb]], lambda: nc.tensor.matmul(PS[xb][:, off:512], lhsT=KT[h][0:kdim, a * 128:(a + 1) * 128], rhs=QT[h][0:kdim, t0 + off:t0 + 512], start=True, stop=(len(extra) == 0), skip_group_check=True), inc=(len(extra) == 0))
                                for ei, (lt, rh, eo, ew) in enumerate(extra):
                                    K.op(pe, [CR], [PSR[xb]], lambda: nc.tensor.matmul(PS[xb][:, eo:eo + ew], lhsT=lt[:, :], rhs=rh, start=False, stop=(ei == len(extra) - 1), skip_group_check=True), inc=(ei == len(extra) - 1))

                            def stB(a):
                                r = a - 4 * cq
                                off = max(0, r) * 128
                                xb = a % 2
                                pt, ptr = PT[a % 2], PTR[a % 2]
                                K.op(act, [PSR[xb]], [ptr], lambda: nc.scalar.activation(out=pt[:, off:512], in_=PS[xb][:, off:512], func=AF.Exp))
                                for s_ in range(max(0, r), 4):
                                    K.op(pe, [ptr, VR], [PSR[ob]], lambda: nc.tensor.matmul(PS[ob][:, s_ * 65:(s_ + 1) * 65], lhsT=pt[:, s_ * 128:(s_ + 1) * 128], rhs=V[:, a, h, :], start=False, stop=False, skip_group_check=True), inc=(s_ == 3))
                            stA(0)
                            for a in range(na):
                                if a + 1 < na:
                                    stA(a + 1)
                                stB(a)
                            finish_o(ob, PSR[ob], cq * 4, 4, h, True)

                    load_win(l, W[0], WR[0], C_SB, C_SB + 768)
                    load_win(l, W[1], WR[1], C_MB, C_MB + 768)
                    qk_proj(W[0], WR[0], 0)
                    proj_tm(W[0], WR[0], 512, 256, v_evac(4))
                    with ExitStack() as st:
                        E1 = sb("E1", [128, 512], F32, st)
                        SPb = [sb("SPb%d" % i, [128, 512], BF16, st) for i in range(2)]
                        SACC = sb("SACC", [128, 512], F32, st)
                        SACCB = sb("SACCB", [128, 512], BF16, st)
                        E1R, SAR, SBR = Reg(), Reg(), Reg()
                        SPR = [Reg(), Reg()]
                        for h in range(4):
                            for cq in range(NCH):
                                ob = 6
                                K.op(dve, [], [PSR[ob]], lambda: nc.vector.memset(PS[ob][:, 0:260], 0.0))
                                K.op(dve, [], [SAR], lambda: nc.vector.memset(SACC[:], 0.0))
                                t0 = cq * 512
                                atop = 4 * cq + 3

                                def offs(a):
                                    return max(0, a - 4 * cq) * 128

                                def stA(a):
                                    r = a - 4 * cq
                                    off = offs(a)
                                    xb = a % 2
                                    spb, spr = SPb[a % 2], SPR[a % 2]
                                    K.op(pe, [KTR[h], QTR[h]], [PSR[xb]], lambda: nc.tensor.matmul(PS[xb][:, off:512], lhsT=KT[h][0:64, a * 128:(a + 1) * 128], rhs=QT[h][0:64, t0 + off:t0 + 512], start=True, stop=False, skip_group_check=True), inc=(r < 0))
                                    if r >= 0:
                                        K.op(pe, [CR], [PSR[xb]], lambda: nc.tensor.matmul(PS[xb][:, off:off + 128], lhsT=ident_b[:, :], rhs=cmstrict[:, :], start=False, stop=False, skip_group_check=True))
                                    K.op(act, [PSR[xb]], [E1R], lambda: nc.scalar.activation(out=E1[:, off:512], in_=PS[xb][:, off:512], func=AF.Exp))
                                    K.op(act, [E1R], [spr], lambda: nc.scalar.activation(out=spb[:, off:512], in_=E1[:, off:512], func=AF.Ln, bias=1.0))

                                def stB(a):
                                    r = a - 4 * cq
                                    off = offs(a)
                                    xb = a % 2
                                    spb, spr = SPb[a % 2], SPR[a % 2]
                                    pt, ptr = PT[a % 2], PTR[a % 2]
                                    first = (a == atop)
                                    prev_off = 512 if first else offs(a + 1)
                                    K.op(pe, [spr, CR], [PSR[xb]], lambda: nc.tensor.matmul(PS[xb][:, off:512], lhsT=uinclneg[:, :], rhs=spb[:, off:512], start=False, stop=first, skip_group_check=True), inc=first)
                                    if not first:
                                        K.op(pe, [SBR, CR], [PSR[xb]], lambda: nc.tensor.matmul(PS[xb][:, prev_off:512], lhsT=onesneg[:, :], rhs=SACCB[:, prev_off:512], start=False, stop=True, skip_group_check=True))
                                    if a > 0:
                                        K.op(dve, [spr, SAR], [SAR], lambda: nc.vector.tensor_tensor(out=SACC[:, off:512], in0=SACC[:, off:512], in1=spb[:, off:512], op=ALU.add))
                                        K.op(dve, [SAR], [SBR], lambda: nc.vector.tensor_copy(out=SACCB[:, off:512], in_=SACC[:, off:512]))
                                    K.op(act, [PSR[xb]], [ptr], lambda: nc.scalar.activation(out=pt[:, off:512], in_=PS[xb][:, off:512], func=AF.Exp))
                                    for s_ in range(max(0, r), 4):
                                        K.op(pe, [ptr, VR], [PSR[ob]], lambda: nc.tensor.matmul(PS[ob][:, s_ * 65:s_ * 65 + 64], lhsT=pt[:, s_ * 128:(s_ + 1) * 128], rhs=V[:, a, h, 0:64], start=False, stop=False, skip_group_check=True), inc=(s_ == 3))
                                stA(atop)
                                for a in range(atop, -1, -1):
                                    if a - 1 >= 0:
                                        stA(a - 1)
                                    stB(a)
                                finish_o(ob, PSR[ob], cq * 4, 4, h, False)
                        otok_to_oT(0)
                        K.barrier()

                    load_win(l, W[0], WR[0], C_FX, C_FX + 772)
                    qk_proj(W[1], WR[1], 0)
                    proj_tm(W[1], WR[1], 512, 256, v_evac(4))
                    with ExitStack() as st:
                        kmean = sb("kmean", [64, 4, 8], F32, st)
                        kmb = sb("kmb", [64, 4, 8], BF16, st)
                        KMR = Reg()
                        NSET = 2
                        G8s = [sb("G8_%d" % i_, [128, 8], F32, st) for i_ in range(NSET)]
                        M8s = [sb("M8_%d" % i_, [128, 8], F32, st) for i_ in range(NSET)]
                        thrs = [sb("thr_%d" % i_, [128, 1], F32, st) for i_ in range(NSET)]
                        SELTs = [sb("SELT_%d" % i_, [128, 72], F32, st) for i_ in range(NSET)]
                        GRs = [Reg() for i_ in range(NSET)]
                        SELRs = [Reg() for i_ in range(NSET)]
                        PSBRs = [Reg() for i_ in range(NSET)]
                        for i_ in range(NSET):
                            K.op(dve, [], [SELRs[i_]], lambda: nc.vector.memset(SELTs[i_][:], 0.0))
                        for h in range(4):
                            K.dma(pool, KT[h][64:72, :], cst["c_blk"], [], [KTR[h]])
                            K.op(dve, [KTR[h]], [KMR], lambda: nc.vector.tensor_reduce(out=kmean[:, h, 0:NB], in_=KT[h][0:64, :].rearrange("p (n k) -> p n k", k=256), axis=AX.X, op=ALU.add))
                        K.op(dve, [KMR], [KMR], lambda: nc.vector.tensor_scalar(out=kmb[:], in0=kmean[:], scalar1=1.0 / 256, scalar2=None, op0=ALU.mult))
                        chain = 0
                        for h in range(4):
                            for t in range(NT):
                                own = t // 2
                                k_ = chain % NSET
                                chain += 1
                                G8, M8, thr, SELT, GR, SELR = G8s[k_], M8s[k_], thrs[k_], SELTs[k_], GRs[k_], SELRs[k_]
                                K.op(dve, [], [SELR], lambda: nc.vector.memset(SELT[:, 64:72], 0.0))
                                if own > 0:
                                    b = 2 + k_
                                    K.op(pe, [QTR[h], KMR], [PSR[b]], lambda: nc.tensor.matmul(PS[b][:, 0:8], lhsT=QT[h][0:64, t * 128:(t + 1) * 128], rhs=kmb[:, h, :], start=True, stop=True))
                                    K.op(dve, [], [GR], lambda: nc.vector.memset(G8[:], -1e30))
                                    K.op(dve, [PSR[b]], [GR], lambda: nc.vector.tensor_copy(out=G8[:, 0:own], in_=PS[b][:, 0:own]))
                                    K.op(dve, [GR], [GR], lambda: nc.vector.max(out=M8[:], in_=G8[:]))
                                    K.op(dve, [GR], [GR], lambda: nc.vector.tensor_scalar(out=thr[:], in0=M8[:, TOPK - 1:TOPK], scalar1=-1e29, scalar2=None, op0=ALU.max))
                                    K.op(dve, [GR], [SELR], lambda: nc.vector.tensor_scalar(out=SELT[:, 64:64 + own], in0=G8[:, 0:own], scalar1=thr[:], scalar2=NEG, op0=ALU.is_lt, op1=ALU.mult))
                                tb_ = 4 + k_
                                K.op(pe, [SELR, CR], [PSR[tb_]], lambda: nc.tensor.transpose(PS[tb_][0:72, 0:128], SELT[:, :], ident_f[:]))
                                K.op(act, [PSR[tb_]], [QAR[h]], lambda: nc.scalar.copy(out=QT[h][64:72, t * 128:(t + 1) * 128], in_=PS[tb_][64:72, 0:128]))
                        K.barrier()
                        for h in range(4):
                            attn_softmax(h, 72, h, None)
                        otok_to_oT(1)
                        K.barrier()

                    load_win(l, W[1], WR[1], C_DQ, C_DQ + 680)
                    load_win(l, W[1], WR[1], C_IK, C_IK + 32, dcol=680)
                    load_win(l, W[1], WR[1], C_IK, C_IK + 32, dcol=712)
                    qk_proj(W[0], WR[0], 0)
                    proj_tm(W[0], WR[0], 512, 256, v_evac(4))
                    with ExitStack() as st:
                        fb = sb("fb", [4, 1], F32, st)
                        ef = sb("ef", [4, S], F32, st)
                        spf = sb("spf", [4, S], F32, st)
                        ones4 = sb("ones4", [4, S], F32, st)
                        cc = sb("cc", [4, S], F32, st)
                        chi = sb("chi", [4, S], BF16, st)
                        clo = sb("clo", [4, S], BF16, st)
                        nchi = sb("nchi", [4, S], BF16, st)
                        nclo = sb("nclo", [4, S], BF16, st)
                        FR = Reg()
                        K.dma(sp, fb[:], fxb_d[l].rearrange("(p o) -> p o", o=1), [], [FR])
                        K.op(dve, [FR], [FR], lambda: nc.vector.tensor_scalar(out=fb[:], in0=fb[:], scalar1=-1.0, scalar2=None, op0=ALU.mult))
                        K.op(dve, [], [FR], lambda: nc.vector.memset(ones4[:], 1.0))
                        for n in range(NCH):
                            b = next_bank()
                            for c in range(8):
                                K.op(pe, [WR[0]] + XNR[n * 4:(n + 1) * 4], [PSR[b]], lambda: nc.tensor.matmul(PS[b][0:4, :], lhsT=W[0][:, c, 768:772], rhs=xnT[:, c, n * 512:(n + 1) * 512], start=(c == 0), stop=(c == 7)), inc=(c == 7))
                            K.op(act, [PSR[b], FR], [FR], lambda: nc.scalar.activation(out=ef[:, n * 512:(n + 1) * 512], in_=PS[b][0:4, :], func=AF.Exp, scale=-1.0, bias=fb[:]))
                        K.op(act, [FR], [FR], lambda: nc.scalar.activation(out=spf[:], in_=ef[:], func=AF.Ln, bias=1.0))
                        K.op(dve, [FR], [FR], lambda: nc.vector.tensor_tensor_scan(out=cc[:], data0=ones4[:], data1=spf[:], initial=0.0, op0=ALU.mult, op1=ALU.subtract))
                        K.op(dve, [FR], [FR], lambda: nc.vector.tensor_copy(out=chi[:], in_=cc[:]))
                        K.op(dve, [FR], [FR], lambda: nc.vector.tensor_tensor(out=clo[:], in0=cc[:], in1=chi[:], op=ALU.subtract))
                        K.op(dve, [FR], [FR], lambda: nc.vector.tensor_scalar(out=nchi[:], in0=chi[:], scalar1=-1.0, scalar2=None, op0=ALU.mult))
                        K.op(dve, [FR], [FR], lambda: nc.vector.tensor_scalar(out=nclo[:], in0=clo[:], scalar1=-1.0, scalar2=None, op0=ALU.mult))
                        for h in range(4):
                            K.dma(sp, QT[h][64:65, :], chi[h:h + 1, :], [FR], [QTR[h]])
                            K.dma(sp, QT[h][65:66, :], clo[h:h + 1, :], [FR], [QTR[h]])
                            K.dma(pool, QT[h][66:68, :], cst["c_ones"][0:2, :], [], [QTR[h]])
                            K.dma(pool, KT[h][64:66, :], cst["c_ones"][0:2, :], [], [KTR[h]])
                            K.dma(sp, KT[h][66:67, :], nchi[h:h + 1, :], [FR], [KTR[h]])
                            K.dma(sp, KT[h][67:68, :], nclo[h:h + 1, :], [FR], [KTR[h]])
                        for h in range(4):
                            attn_softmax(h, 68, None, cmincl)
                        otok_to_oT(2)
                        K.barrier()

                    for h in range(4):
                        proj_fm(W[1], WR[1], h * 64, 64, (lambda n, h=h: QT[h][0:64, n * 512:(n + 1) * 512]), QTR[h], scale=0.125)
                    proj_fm(W[1], WR[1], 256, 64, (lambda n: KT[0][0:64, n * 512:(n + 1) * 512]), KTR[0])
                    proj_tm(W[1], WR[1], 320, 64, v_evac(1))
                    with ExitStack() as st:
                        QI = [sb("QI%d" % i, [64, S], BF16, st) for i in range(4)]
                        KI = sb("KI", [64, S], BF16, st)
                        QIR, KIR = Reg(), Reg()
                        WI = sb("WI", [128, NT, 8], F32, st)
                        WA = sb("WA", [128, NT, 8], F32, st)
                        WS = sb("WS", [128, NT, 8], F32, st)
                        WIR = Reg()
                        ISC = sb("ISC", [128, S], F32, st)
                        ISR = Reg()
                        RT = [sb("RT%d" % i, [128, 512], BF16, st) for i in range(2)]
                        DG = sb("DG", [128, 8, 128], BF16, st)
                        DGR = Reg()
                        acc_rr = [0]
                        RTR = [Reg(), Reg()]
                        MNB = [sb("MNEG%d" % i_, [128, S], BF16, st) for i_ in range(2)]
                        MNRB = [Reg(), Reg()]
                        MZ = sb("MZ", [128, 128], BF16, st)
                        MZR = Reg()
                        lo = sb("lo", [128, 1], F32, st)
                        hi_ = sb("hi_", [128, 1], F32, st)
                        hw = sb("hw", [128, NIT], F32, st)
                        mid = sb("mid", [128, 1], F32, st)
                        cnt = sb("cnt", [128, 1], F32, st)
                        tmp1 = sb("tmp1", [128, 1], F32, st)
                        BR = Reg()
                        for i in range(4):
                            proj_fm(W[1], WR[1], 384 + i * 64, 64, (lambda n, i=i: QI[i][0:64, n * 512:(n + 1) * 512]), QIR)
                        proj_fm(W[1], WR[1], 680, 64, (lambda n: KI[0:64, n * 512:(n + 1) * 512]), KIR)

                        def wi_evac(t, ps, psr):
                            K.op(dve, [psr], [WIR], lambda: nc.vector.tensor_copy(out=WI[:, t, :], in_=ps[:, 0:8]))
                        proj_tm(W[1], WR[1], 672, 8, wi_evac)
                        K.op(act, [WIR], [WIR], lambda: nc.scalar.activation(out=WA[:], in_=WI[:], func=AF.Abs))
                        K.op(act, [WIR], [WIR], lambda: nc.scalar.activation(out=WS[:], in_=WI[:], func=AF.Sign))
                        K.op(dve, [], [MZR], lambda: nc.vector.memset(MZ[:], 0.0))
                        def sel(i):
                            t1 = (i + 1) * 128
                            MNEG, MNR = MNB[i % 2], MNRB[i % 2]
                            for hi in range(8):
                                K.op(dve, [WIR, CR], [DGR], lambda: nc.vector.tensor_scalar(out=DG[:, hi, :], in0=ident_b[:, :], scalar1=WS[:, i, hi:hi + 1], scalar2=None, op0=ALU.mult))
                            for kc in range((t1 + 511) // 512):
                                k0 = kc * 512
                                kw = min(512, t1 - k0)
                                ab = 4 + (acc_rr[0] % 2)
                                acc_rr[0] += 1

                                def zmm(hi):
                                    b = 2 + (hi % 2)
                                    rt, rtr = RT[hi % 2], RTR[hi % 2]
                                    K.op(pe, [QIR, KIR], [PSR[b]], lambda: nc.tensor.matmul(PS[b][:, 0:kw], lhsT=QI[hi // 2][(hi % 2) * 32:(hi % 2) * 32 + 32, i * 128:(i + 1) * 128], rhs=KI[(hi % 2) * 32:(hi % 2) * 32 + 32, k0:k0 + kw], start=True, stop=True))
                                    K.op(act, [PSR[b], WIR], [rtr], lambda: nc.scalar.activation(out=rt[:, 0:kw], in_=PS[b][:, 0:kw], func=AF.Relu, scale=WA[:, i, hi:hi + 1]))

                                def amm(hi):
                                    rt, rtr = RT[hi % 2], RTR[hi % 2]
                                    K.op(pe, [rtr, DGR], [PSR[ab]], lambda: nc.tensor.matmul(PS[ab][:, 0:kw], lhsT=DG[:, hi, :], rhs=rt[:, 0:kw], start=(hi == 0), stop=(hi == 7)), inc=(hi == 7))
                                zmm(0)
                                for hi in range(8):
                                    if hi + 1 < 8:
                                        zmm(hi + 1)
                                    amm(hi)
                                K.op(act, [PSR[ab]], [ISR], lambda: nc.scalar.copy(out=ISC[:, k0:k0 + kw], in_=PS[ab][:, 0:kw]))
                            K.op(dve, [ISR], [BR], lambda: nc.vector.tensor_reduce(out=lo[:], in_=ISC[:, 0:t1], axis=AX.X, op=ALU.min))
                            K.op(dve, [ISR], [BR], lambda: nc.vector.tensor_reduce(out=hi_[:], in_=ISC[:, 0:t1], axis=AX.X, op=ALU.max))
                            K.op(dve, [ISR, CR], [ISR], lambda: nc.vector.tensor_tensor(out=ISC[:, i * 128:t1], in0=ISC[:, i * 128:t1], in1=adm[:, :], op=ALU.add))
                            K.op(dve, [BR], [BR], lambda: nc.vector.tensor_tensor(out=hi_[:], in0=hi_[:], in1=lo[:], op=ALU.subtract))
                            K.op(dve, [BR, CR], [BR], lambda: nc.vector.tensor_scalar(out=hw[:], in0=powt[:], scalar1=hi_[:], scalar2=None, op0=ALU.mult))
                            K.op(dve, [BR], [BR], lambda: nc.vector.tensor_tensor(out=mid[:], in0=lo[:], in1=hw[:, 0:1], op=ALU.add))
                            for it in range(NIT):
                                K.op(dve, [BR, ISR], [BR, MNR], lambda: nc.vector.tensor_scalar(out=MNEG[:, 0:t1], in0=ISC[:, 0:t1], scalar1=mid[:], scalar2=0.0, op0=ALU.is_ge, op1=ALU.add, accum_out=cnt[:]))
                                K.op(dve, [BR], [BR], lambda: nc.vector.tensor_scalar(out=tmp1[:], in0=cnt[:], scalar1=float(KSEL) - 0.5, scalar2=hw[:, it:it + 1], op0=ALU.is_ge, op1=ALU.mult))
                                nxt = it + 1 if it + 1 < NIT else it
                                K.op(dve, [BR], [BR], lambda: nc.vector.tensor_scalar(out=mid[:], in0=mid[:], scalar1=hw[:, nxt:nxt + 1], scalar2=tmp1[:], op0=ALU.subtract, op1=ALU.add))
                            K.op(dve, [BR], [BR], lambda: nc.vector.tensor_copy(out=lo[:], in_=mid[:]))
                            K.op(dve, [BR, ISR], [MNR], lambda: nc.vector.tensor_scalar(out=MNEG[:, 0:t1], in0=ISC[:, 0:t1], scalar1=lo[:], scalar2=NEG, op0=ALU.is_lt, op1=ALU.mult))
                        def attn(i):
                            t1 = (i + 1) * 128
                            need_sel = t1 > KSEL
                            MNEG, MNR = MNB[i % 2], MNRB[i % 2]
                            ob = 6
                            def dA(a):
                                xb = a % 2
                                K.op(pe, [MNR if need_sel else MZR, CR], [PSR[xb]], lambda: nc.tensor.matmul(PS[xb][:, :], lhsT=(MNEG[:, a * 128:(a + 1) * 128] if need_sel else MZ[:, :]), rhs=ident4[:, :], start=True, stop=False, skip_group_check=True), inc=False)
                                for h in range(4):
                                    lastmm = (h == 3) and (a < i - 1)
                                    K.op(pe, [KTR[0], QTR[h]], [PSR[xb]], lambda: nc.tensor.matmul(PS[xb][:, h * 128:(h + 1) * 128], lhsT=KT[0][0:64, a * 128:(a + 1) * 128], rhs=QT[h][0:64, i * 128:(i + 1) * 128], start=False, stop=lastmm, skip_group_check=True), inc=lastmm)
                                if a >= i - 1:
                                    for h in range(4):
                                        tb = tbias[:, 4 + h, 0:128] if a == i else tbias[:, 4 + h, 128:256]
                                        K.op(pe, [CR], [PSR[xb]], lambda: nc.tensor.matmul(PS[xb][:, h * 128:(h + 1) * 128], lhsT=ident_b[:, :], rhs=tb, start=False, stop=(h == 3), skip_group_check=True), inc=(h == 3))
                            def dB(a):
                                xb = a % 2
                                pt, ptr = PT[a % 2], PTR[a % 2]
                                K.op(act, [PSR[xb]], [ptr], lambda: nc.scalar.activation(out=pt[:, :], in_=PS[xb][:, :], func=AF.Exp))
                                for h in range(4):
                                    K.op(pe, [ptr, VR], [PSR[ob]], lambda: nc.tensor.matmul(PS[ob][:, h * 65:(h + 1) * 65], lhsT=pt[:, h * 128:(h + 1) * 128], rhs=V[:, a, 0, :], start=False, stop=False, skip_group_check=True), inc=(h == 3))
                            dA(0)
                            for a in range(i + 1):
                                if a + 1 <= i:
                                    dA(a + 1)
                                dB(a)
                            o3 = PS[ob][:, 0:260].rearrange("p (s d) -> p s d", d=65)
                            K.op(dve, [PSR[ob]], [ORR], lambda: nc.vector.reciprocal(out=orec[:, 0:4], in_=o3[:, :, 64]))
                            for h in range(4):
                                K.op(dve, [PSR[ob], ORR], [OKR], lambda: nc.vector.tensor_scalar(out=OTOK[:, i, h * 64:(h + 1) * 64], in0=o3[:, h, 0:64], scalar1=orec[:, h:h + 1], scalar2=None, op0=ALU.mult))
                        for i in range(NT):
                            K.op(dve, [], [PSR[6]], lambda: nc.vector.memset(PS[6][:, 0:260], 0.0))
                            if (i + 2) * 128 > KSEL and i + 1 < NT:
                                sel(i + 1)
                            attn(i)
                        otok_to_oT(3)
                        K.barrier()

                    ast.close()
                    with ExitStack() as st:
                        mixT = sb("mixT", [128, 8, S], BF16, st)
                        MXR = Reg()
                        WG = [sb("WG%d" % i, [128, 4, 8, 128], BF16, st) for i in range(2)]
                        WB = [sb("WB%d" % i, [128, 4, 2, 128], BF16, st) for i in range(2)]
                        WGR = [Reg(), Reg()]
                        SG = [sb("SG%d" % i, [128, 512], F32, st) for i in range(2)]
                        SGR = [Reg(), Reg()]
                        ACC = sb("ACCg", [128, 512], F32, st)
                        TMP = sb("TMPg", [128, 512], F32, st)
                        ACR = Reg()

                        def load_gw(f):
                            s_ = f % 2
                            for i in range(4):
                                K.dma(pool, WG[s_][:, i, :, :], wg_d[l, i][:, f * 128:(f + 1) * 128].rearrange("(c p) n -> p c n", p=128), [], [WGR[s_]])
                                K.dma(pool, WB[s_][:, i, :, :], wb_d[l, i][:, f * 128:(f + 1) * 128].rearrange("(c p) n -> p c n", p=128), [], [WGR[s_]])
                        load_gw(0)
                        for f in range(8):
                            if f + 1 < 8:
                                load_gw(f + 1)
                            s_ = f % 2
                            for n in range(NCH):
                                for i in range(4):
                                    gb = next_bank(0, 3)
                                    bb = 3 + next_bank(0, 3)
                                    for c in range(8):
                                        K.op(pe, [WGR[s_]] + XNR[n * 4:(n + 1) * 4], [PSR[gb]], lambda: nc.tensor.matmul(PS[gb][:, :], lhsT=WG[s_][:, i, c, :], rhs=xnT[:, c, n * 512:(n + 1) * 512], start=(c == 0), stop=(c == 7)), inc=(c == 7))
                                    for k in range(2):
                                        K.op(pe, [WGR[s_], OTR[i]], [PSR[bb]], lambda: nc.tensor.matmul(PS[bb][:, :], lhsT=WB[s_][:, i, k, :], rhs=oT[:, i, k, n * 512:(n + 1) * 512], start=(k == 0), stop=(k == 1)), inc=(k == 1))
                                    sg, sgr = SG[i % 2], SGR[i % 2]
                                    K.op(act, [PSR[gb]], [sgr], lambda: nc.scalar.activation(out=sg[:], in_=PS[gb][:, :], func=AF.Sigmoid))
                                    if i == 0:
                                        K.op(dve, [sgr, PSR[bb]], [ACR], lambda: nc.vector.tensor_tensor(out=ACC[:], in0=sg[:], in1=PS[bb][:, :], op=ALU.mult))
                                    else:
                                        K.op(dve, [sgr, PSR[bb]], [ACR], lambda: nc.vector.tensor_tensor(out=TMP[:], in0=sg[:], in1=PS[bb][:, :], op=ALU.mult))
                                        if i < 3:
                                            K.op(dve, [ACR], [ACR], lambda: nc.vector.tensor_tensor(out=ACC[:], in0=ACC[:], in1=TMP[:], op=ALU.add))
                                        else:
                                            K.op(dve, [ACR], [MXR], lambda: nc.vector.tensor_tensor(out=mixT[:, f, n * 512:(n + 1) * 512], in0=ACC[:], in1=TMP[:], op=ALU.add))
                        WO = sb("WO", [128, 8, D], BF16, st)
                        WOR = Reg()
                        K.dma(pool, WO[:], wo_d[l].rearrange("(c p) n -> p c n", p=128), [], [WOR])
                        NBF = alloc_normbufs(st)
                        load_g(gffn_d[l])
                        def wo_mm(t):
                            banks = []
                            for hb in range(2):
                                b = (t % 2) * 2 + hb
                                for c in range(8):
                                    K.op(pe, [MXR, WOR], [PSR[b]], lambda: nc.tensor.matmul(PS[b][:, :], lhsT=mixT[:, c, t * 128:(t + 1) * 128], rhs=WO[:, c, hb * 512:(hb + 1) * 512], start=(c == 0), stop=(c == 7)), inc=(c == 7))
                                banks.append((PS[b], PSR[b]))
                            return banks
                        nxt_b = wo_mm(0)
                        for t in range(NT):
                            cur_b = nxt_b
                            if t + 1 < NT:
                                nxt_b = wo_mm(t + 1)
                            norm_tile(NBF, seq, t, y_d[seq, t * 128:(t + 1) * 128, :], cur_b, False, t % 2)
                        K.barrier()
                        if dbg and seq == 0 and l == 0:
                            K.dma(sp, dbg_d[4], y_d[0], [], [])
                            K.barrier()

                with ExitStack() as fst:
                    NH = S // TOKH
                    NCF = TOKH // 512
                    actT = sb("actT", [128, NJ, TOKH], BF16, fst)
                    ATR = Reg()
                    WD = sb("WD", [128, NJ, D], BF16, fst)
                    WDR = Reg()
                    K.dma(pool, WD[:, 0:11, :], wdn_d[l][0:11 * 128, :].rearrange("(c p) n -> p c n", p=128), [], [WDR])
                    K.dma(pool, WD[:, 11:22, :], wdn_d[l][11 * 128:22 * 128, :].rearrange("(c p) n -> p c n", p=128), [], [WDR])
                    HB = [[sb("HB%d%d" % (i, g), [128, TOKH + 2], F32, fst) for g in range(2)] for i in range(2)]
                    HBR = [[Reg(), Reg()], [Reg(), Reg()]]
                    HALO = sb("HALO", [128, NJ, 2, 2], F32, fst)
                    HLR = Reg()
                    WU = [sb("WU%d" % i, [128, 2, 8, 256], BF16, fst) for i in range(2)]
                    WUR = [Reg(), Reg()]
                    CWt = sb("CWt", [128, 4, 2 * NJ], F32, fst)
                    CWc = sb("CWc", [2 * NJ, 4, 128], F32, fst)
                    CWR = Reg()
                    for j_ in range(3):
                        K.dma(sp, CWc[:, j_, :], cw_d[l, j_].rearrange("(c p) -> c p", p=128), [], [CWR])
                    K.dma(sp, CWc[:, 3, :], cb_d[l].rearrange("(c p) -> c p", p=128), [], [CWR])
                    for j_ in range(4):
                        K.op(pe, [CWR, CR], [PSR[6]], lambda: nc.tensor.transpose(PS[6][:, j_ * 2 * NJ:(j_ + 1) * 2 * NJ], CWc[:, j_, :], ident_f[0:2 * NJ, 0:2 * NJ]), inc=(j_ == 3))
                    K.op(dve, [PSR[6]], [CWR], lambda: nc.vector.tensor_copy(out=CWt[:], in_=PS[6][:, 0:8 * NJ].rearrange("p (a c) -> p a c", c=2 * NJ)))
                    CA2 = [[[sb("CA%d_%d_%d" % (p_, g, n_), [128, 512], F32, fst) for n_ in range(TOKH // 512)] for g in range(2)] for p_ in range(2)]
                    CAR2 = [[[Reg() for n_ in range(TOKH // 512)] for g in range(2)] for p_ in range(2)]
                    GL2 = [sb("GL%d" % i_, [128, 512], F32, fst) for i_ in range(2)]
                    GLR2 = [Reg(), Reg()]
                    NBF = alloc_normbufs(fst)
                    final = (l == DEPTH - 1)
                    load_g(gfin_d[0] if final else gmix_d[l + 1])

                    def load_wu(jp):
                        s_ = jp % 2
                        for g in range(2):
                            K.dma(pool, WU[s_][:, g, :, :], wup_d[l][:, g * DFF + jp * 256:g * DFF + (jp + 1) * 256].rearrange("(c p) n -> p c n", p=128), [], [WUR[s_]])

                    for half in range(NH):
                        tk0 = half * TOKH
                        load_wu(0)

                        def st1(j):
                            if j % 2 == 0 and j // 2 + 1 < NJ // 2:
                                load_wu(j // 2 + 1)
                            s_ = j % 2
                            ws_ = (j // 2) % 2
                            wo_ = (j % 2) * 128
                            CA, CAR = CA2[s_], CAR2[s_]
                            for g in range(2):
                                hbuf, hbr = HB[s_][g], HBR[s_][g]
                                if half == 0:
                                    K.op(dve, [], [hbr], lambda: nc.vector.memset(hbuf[:, 0:2], 0.0))
                                else:
                                    K.op(dve, [HLR], [hbr], lambda: nc.vector.tensor_copy(out=hbuf[:, 0:2], in_=HALO[:, j, g, :]))
                                for n in range(NCF):
                                    b = next_bank(0, 6)
                                    tn = (tk0 + n * 512) // 128
                                    for c in range(8):
                                        K.op(pe, [WUR[ws_]] + XNR[tn:tn + 4], [PSR[b]], lambda: nc.tensor.matmul(PS[b][:, :], lhsT=WU[ws_][:, g, c, wo_:wo_ + 128], rhs=xnT[:, c, tk0 + n * 512:tk0 + (n + 1) * 512], start=(c == 0), stop=(c == 7)), inc=(c == 7))
                                    K.op(act, [PSR[b]], [hbr], lambda: nc.scalar.copy(out=hbuf[:, 2 + n * 512:2 + (n + 1) * 512], in_=PS[b][:, :]))
                                    ch_ = g * NJ + j
                                    K.op(act, [PSR[b], CWR], [CAR[g][n]], lambda: nc.scalar.activation(out=CA[g][n][:], in_=PS[b][:, :], func=AF.Identity, scale=CWt[:, 2, ch_:ch_ + 1], bias=CWt[:, 3, ch_:ch_ + 1]))
                                if half + 1 < NH:
                                    K.op(dve, [hbr], [HLR], lambda: nc.vector.tensor_copy(out=HALO[:, j, g, :], in_=hbuf[:, TOKH:TOKH + 2]))

                        def st2(j):
                            s_ = j % 2
                            CA, CAR = CA2[s_], CAR2[s_]
                            for n in range(NCF):
                                for g in range(2):
                                    hbuf, hbr = HB[s_][g], HBR[s_][g]
                                    ch = g * NJ + j
                                    ca, car = CA[g][n], CAR[g][n]
                                    K.op(dve, [hbr, CWR, car], [car], lambda: nc.vector.scalar_tensor_tensor(out=ca[:], in0=hbuf[:, 1 + n * 512:1 + (n + 1) * 512], scalar=CWt[:, 1, ch:ch + 1], in1=ca[:], op0=ALU.mult, op1=ALU.add))
                                    K.op(dve, [hbr, CWR, car], [car], lambda: nc.vector.scalar_tensor_tensor(out=ca[:], in0=hbuf[:, 0 + n * 512:0 + (n + 1) * 512], scalar=CWt[:, 0, ch:ch + 1], in1=ca[:], op0=ALU.mult, op1=ALU.add))
                                gl, glr = GL2[n % 2], GLR2[n % 2]
                                K.op(act, [CAR[0][n]], [glr], lambda: nc.scalar.activation(out=gl[:], in_=CA[0][n][:], func=AF.Gelu))
                                K.op(dve, [glr, CAR[1][n]], [ATR], lambda: nc.vector.tensor_tensor(out=actT[:, j, n * 512:(n + 1) * 512], in0=gl[:], in1=CA[1][n][:], op=ALU.mult))
                        st1(0)
                        for j in range(NJ):
                            if j + 1 < NJ:
                                st1(j + 1)
                            st2(j)
                        def dn_mm(tt):
                            t = tk0 // 128 + tt
                            banks = []
                            for hb in range(2):
                                b = (t % 2) * 2 + hb
                                for j in range(NJ):
                                    K.op(pe, [ATR, WDR], [PSR[b]], lambda: nc.tensor.matmul(PS[b][:, :], lhsT=actT[:, j, tt * 128:(tt + 1) * 128], rhs=WD[:, j, hb * 512:(hb + 1) * 512], start=(j == 0), stop=(j == NJ - 1)), inc=(j == NJ - 1))
                                banks.append((PS[b], PSR[b]))
                            return banks
                        nxt_b = dn_mm(0)
                        for tt in range(TOKH // 128):
                            t = tk0 // 128 + tt
                            cur_b = nxt_b
                            if tt + 1 < TOKH // 128:
                                nxt_b = dn_mm(tt + 1)
                            norm_tile(NBF, seq, t, y_d[seq, t * 128:(t + 1) * 128, :], cur_b, False, t % 2)
                        K.barrier()
                    if final:
                        for t in range(NT):
                            norm_tile(NBF, seq, t, y_d[seq, t * 128:(t + 1) * 128, :], None, True, t % 2)
                    K.barrier()
        K.barrier()
    return nc, hc


_CACHE = {}


def kernel(**inputs):
    S, DEPTH, NSEQ, NCORE = 2048, 4, 2, 8
    if "prog" not in _CACHE:
        _CACHE["prog"] = build(S, DEPTH, NSEQ)
    nc, hc = _CACHE["prog"]
    f = lambda a: np.ascontiguousarray(np.asarray(a, dtype=np.float32))
    x = f(inputs["x"])
    shared = {k: f(inputs[k]) for k in ["norm_mix_g", "norm_ffn_g", "w_in", "fox_b_f", "w_gate", "w_branch", "w_out", "w_up", "conv_w", "conv_b", "w_down"]}
    shared["norm_final_g"] = f(inputs["norm_final_g"]).reshape(1, D)
    shared["t5_bias"] = f(inputs["t5_bias"]).reshape(1, 256)
    shared.update(hc)
    in_maps = []
    for c in range(NCORE):
        m = dict(shared)
        m["x"] = np.ascontiguousarray(x[c * NSEQ:(c + 1) * NSEQ])
        in_maps.append(m)
    res = run_bass_kernel_spmd(nc, in_maps, core_ids=list(range(NCORE)))
    return np.concatenate([r["y"] for r in res.results], axis=0).astype(np.float32)
```

```python
import math
from contextlib import ExitStack
import numpy as np
import concourse.bass as bass
import concourse.mybir as mybir
from concourse.bass_utils import run_bass_kernel_spmd

F32 = mybir.dt.float32
BF16 = mybir.dt.bfloat16
AF = mybir.ActivationFunctionType
ALU = mybir.AluOpType
AX = mybir.AxisListType

D = 1024
HD = 64
PIN = 2988
DFF = 2816
NJ = DFF // 128
C_SB, C_MB, C_FX, C_FXF, C_DQ, C_DK, C_DV, C_IQ, C_IK, C_IW = 0, 768, 1536, 2304, 2308, 2564, 2628, 2692, 2948, 2980
NEG = -30000.0
NIT = 18
SAME_ENGINE_SYNC = True


class Sem:
    def __init__(self, h):
        self.h = h
        self.cnt = 0


class Eng:
    def __init__(self, e, sem, name):
        self.e = e
        self.sem = sem
        self.seen = {}
        self.name = name


class Reg:
    __slots__ = ("w", "r", "name")

    def __init__(self, name=""):
        self.w = None
        self.r = {}
        self.name = name


class KB:
    def __init__(self, nc, es):
        self.nc = nc
        self.es = es
        mk = lambda n: Sem(es.enter_context(nc.semaphore(n)))
        self.pe = Eng(nc.tensor, mk("s_pe"), "pe")
        self.act = Eng(nc.scalar, mk("s_act"), "act")
        self.dve = Eng(nc.vector, mk("s_dve"), "dve")
        self.pool = Eng(nc.gpsimd, mk("s_pool"), "pool")
        self.sp = Eng(nc.sync, mk("s_sp"), "sp")
        self.engs = [self.pe, self.act, self.dve, self.pool, self.sp]
        self.dsems = {"pool": [mk("d_pool%d" % i) for i in range(12)],
                      "sp": [mk("d_sp%d" % i) for i in range(12)]}
        self.drr = {"pool": 0, "sp": 0}
        self.allsems = [e.sem for e in self.engs] + self.dsems["pool"] + self.dsems["sp"]

    def _wait(self, E, deps):
        need = {}
        for (sem, val) in deps:
            if sem is E.sem and (E.name == "pe" or not SAME_ENGINE_SYNC):
                continue
            if need.get(sem, 0) < val:
                need[sem] = val
        for sem, val in need.items():
            if E.seen.get(sem, 0) < val:
                E.e.wait_ge(sem.h, val)
                E.seen[sem] = val

    def _deps(self, reads, writes):
        deps = []
        for r in reads:
            if r.w is not None:
                deps.append(r.w)
        for w in writes:
            if w.w is not None:
                deps.append(w.w)
            deps.extend(w.r.items())
        return deps

    def _record(self, tok, reads, writes):
        sem, val = tok
        for r in reads:
            if r.r.get(sem, 0) < val:
                r.r[sem] = val
        for w in writes:
            w.w = tok
            w.r = {}

    def op(self, E, reads, writes, fn, inc=True):
        self._wait(E, self._deps(reads, writes))
        ins = fn()
        if inc:
            E.sem.cnt += 1
            ins.then_inc(E.sem.h, 1)
            tok = (E.sem, E.sem.cnt)
        else:
            tok = (E.sem, E.sem.cnt + 1)
        self._record(tok, reads, writes)
        return tok

    def dma(self, Q, out, in_, reads, writes):
        sems = self.dsems[Q.name]
        s = sems[self.drr[Q.name] % len(sems)]
        self.drr[Q.name] += 1
        deps = self._deps(reads, writes)
        if s.cnt > 0:
            deps.append((s, s.cnt))
        self._wait(Q, deps)
        Q.e.dma_start(out=out, in_=in_).then_inc(s.h, 16)
        s.cnt += 16
        tok = (s, s.cnt)
        self._record(tok, reads, writes)
        return tok

    def barrier(self):
        for E in self.engs:
            for s in self.allsems:
                if s is E.sem or s.cnt == 0:
                    continue
                if E.seen.get(s, 0) < s.cnt:
                    E.e.wait_ge(s.h, s.cnt)
                    E.seen[s] = s.cnt


def t5_bucket_np(n):
    n = np.maximum(n, 0)
    nf = np.maximum(n, 1).astype(np.float32)
    large = 16 + (np.log(nf / np.float32(16)) / np.float32(math.log(128 / 16)) * np.float32(16)).astype(np.int32)
    large = np.minimum(large, 31)
    return np.where(n < 16, n, large)


def host_consts(S):
    c = {}
    i = np.arange(128)
    c["c_ident"] = np.eye(128, dtype=np.float32)
    c["c_ident4"] = np.tile(np.eye(128, dtype=np.float32), (1, 4))
    c["c_uinclneg"] = -(i[:, None] >= i[None, :]).astype(np.float32)
    c["c_onesneg"] = -np.ones((128, 128), np.float32)
    c["c_cmstrict"] = np.where(i[:, None] >= i[None, :], NEG, 0.0).astype(np.float32)
    c["c_cmincl"] = np.where(i[:, None] > i[None, :], NEG, 0.0).astype(np.float32)
    c["c_adm"] = np.where(i[None, :] <= i[:, None], 0.0, -1e30).astype(np.float32)
    dd = np.arange(256)[None, :] - i[:, None]
    c["c_buck"] = np.where(dd >= 0, t5_bucket_np(dd), 99).astype(np.float32)
    c["c_tmask"] = np.where(dd >= 0, 0.0, NEG).astype(np.float32)
    c["c_blk"] = (np.arange(S)[None, :] // 256 == np.arange(8)[:, None]).astype(np.float32)
    c["c_ones"] = np.ones((8, S), np.float32)
    c["c_pow"] = np.tile((0.5 ** (np.arange(NIT) + 1)).astype(np.float32)[None, :], (128, 1))
    return c


def build(S, DEPTH, NSEQ, dbg=False):
    NT = S // 128
    NCH = S // 512
    NB = S // 256
    TOPK = min(3, NB)
    KSEL = min(256, S // 4)
    TOKH = min(S, 1024)
    nc = bass.Bass("TRN2", target_bir_lowering=False)
    es = ExitStack()
    with es:
        def din(name, shape):
            return nc.dram_tensor(name, list(shape), F32, kind="ExternalInput").ap()
        x_d = din("x", [NSEQ, S, D])
        y_d = nc.dram_tensor("y", [NSEQ, S, D], F32, kind="ExternalOutput").ap()
        gmix_d = din("norm_mix_g", [DEPTH, D])
        gffn_d = din("norm_ffn_g", [DEPTH, D])
        gfin_d = din("norm_final_g", [1, D])
        win_d = din("w_in", [DEPTH, D, PIN])
        fxb_d = din("fox_b_f", [DEPTH, 4])
        t5_d = din("t5_bias", [1, 256])
        wg_d = din("w_gate", [DEPTH, 4, D, D])
        wb_d = din("w_branch", [DEPTH, 4, 256, D])
        wo_d = din("w_out", [DEPTH, D, D])
        wup_d = din("w_up", [DEPTH, D, 2 * DFF])
        cw_d = din("conv_w", [DEPTH, 3, 2 * DFF])
        cb_d = din("conv_b", [DEPTH, 2 * DFF])
        wdn_d = din("w_down", [DEPTH, DFF, D])
        hc = host_consts(S)
        cst = {k: din(k, v.shape) for k, v in hc.items()}
        dbg_d = nc.dram_tensor("dbg", [6, S, D], F32, kind="ExternalOutput").ap() if dbg else None

        K = KB(nc, es)
        pe, act, dve, pool, sp = K.pe, K.act, K.dve, K.pool, K.sp

        uniq = [0]

        def sb(name, shape, dt, st=None):
            uniq[0] += 1
            return (st or es).enter_context(nc.sbuf_tensor("%s_%d" % (name, uniq[0]), list(shape), dt))

        PS = [es.enter_context(nc.psum_tensor("ps%d" % i, [128, 512], F32)) for i in range(7)]
        PSR = [Reg("ps%d" % i) for i in range(7)]
        PSB = es.enter_context(nc.psum_tensor("psb", [128, 1024], BF16))
        PSBR = Reg("psb")

        ident_f = sb("ident_f", [128, 128], F32)
        ident_b = sb("ident_b", [128, 128], BF16)
        ident4 = sb("ident4", [128, 512], BF16)
        uinclneg = sb("uinclneg", [128, 128], BF16)
        onesneg = sb("onesneg", [128, 128], BF16)
        cmstrict = sb("cmstrict", [128, 128], BF16)
        cmincl = sb("cmincl", [128, 128], BF16)
        adm = sb("adm", [128, 128], F32)
        buck = sb("buck", [128, 256], F32)
        tmask = sb("tmask", [128, 256], F32)
        powt = sb("powt", [128, NIT], F32)
        t5bc = sb("t5bc", [128, 256], F32)
        tbias = sb("tbias", [128, 8, 256], BF16)
        CR = Reg("const")
        for (t, k, q) in [(ident_f, "c_ident", sp), (ident_b, "c_ident", pool), (ident4, "c_ident4", pool),
                          (uinclneg, "c_uinclneg", pool), (onesneg, "c_onesneg", pool), (cmstrict, "c_cmstrict", pool),
                          (cmincl, "c_cmincl", pool), (adm, "c_adm", sp), (buck, "c_buck", sp), (tmask, "c_tmask", sp),
                          (powt, "c_pow", sp)]:
            K.dma(q, t[:], cst[k], [], [CR])
        K.dma(sp, t5bc[:], bass.AP(tensor=t5_d.tensor, offset=0, ap=[[0, 128], [1, 256]]), [], [CR])
        with ExitStack() as st:
            tacc = sb("tacc", [128, 256], F32, st)
            ttmp = sb("ttmp", [128, 256], F32, st)
            TR = Reg("tb")
            for h in range(8):
                for b in range(32):
                    col = t5bc[:, b * 8 + h:b * 8 + h + 1]
                    if b == 0:
                        K.op(dve, [CR], [TR], lambda: nc.vector.tensor_scalar(out=tacc[:], in0=buck[:], scalar1=float(b), scalar2=col, op0=ALU.is_equal, op1=ALU.mult))
                    else:
                        K.op(dve, [CR], [TR], lambda: nc.vector.tensor_scalar(out=ttmp[:], in0=buck[:], scalar1=float(b), scalar2=col, op0=ALU.is_equal, op1=ALU.mult))
                        K.op(dve, [TR], [TR], lambda: nc.vector.tensor_tensor(out=tacc[:], in0=tacc[:], in1=ttmp[:], op=ALU.add))
                c31 = t5bc[:, 31 * 8 + h:31 * 8 + h + 1]
                K.op(dve, [TR, CR], [CR], lambda: nc.vector.scalar_tensor_tensor(out=tbias[:, h, :], in0=tacc[:], scalar=c31, in1=tmask[:], op0=ALU.subtract, op1=ALU.add))
            K.barrier()

        xnT = sb("xnT", [128, 8, S], BF16)
        XNR = [Reg("xnT%d" % t) for t in range(NT)]
        gbc = sb("gbc", [128, D], F32)
        GBR = Reg("gbc")

        def load_g(src_row_ap):
            K.dma(sp, gbc[:], bass.AP(tensor=src_row_ap.tensor, offset=src_row_ap.offset, ap=[[0, 128], [1, D]]), [], [GBR])

        YR = [[Reg("y%d_%d" % (s, t)) for t in range(NT)] for s in range(NSEQ)]

        class NormBufs:
            pass

        def norm_tile(NBF, seq, t, src_ap, delta_banks, final, slot):
            xt = NBF.xt[slot]
            XTR = NBF.xtr[slot]
            K.dma(sp, xt[:], src_ap, [YR[seq][t]], [XTR])
            if delta_banks is not None:
                for hb, (bk, bkr) in enumerate(delta_banks):
                    K.op(dve, [XTR, bkr], [XTR], lambda: nc.vector.tensor_tensor(out=xt[:, hb * 512:(hb + 1) * 512], in0=xt[:, hb * 512:(hb + 1) * 512], in1=bk[:, :], op=ALU.add))
                K.dma(sp, y_d[seq, t * 128:(t + 1) * 128, :], xt[:], [XTR], [YR[seq][t]])
            K.op(act, [XTR], [NBF.sqr[slot]], lambda: nc.scalar.activation(out=NBF.sq[slot][:], in_=xt[:], func=AF.Square, accum_out=NBF.ss[slot][:]))
            K.op(act, [NBF.sqr[slot]], [NBF.sqr[slot]], lambda: nc.scalar.activation(out=NBF.rs[slot][:], in_=NBF.ss[slot][:], func=AF.Sqrt, scale=1.0 / D, bias=NBF.eps[:]))
            K.op(dve, [NBF.sqr[slot]], [NBF.sqr[slot]], lambda: nc.vector.reciprocal(out=NBF.rs[slot][:], in_=NBF.rs[slot][:]))
            if final:
                K.op(dve, [XTR, NBF.sqr[slot], GBR], [XTR], lambda: nc.vector.scalar_tensor_tensor(out=xt[:], in0=xt[:], scalar=NBF.rs[slot][:], in1=gbc[:], op0=ALU.mult, op1=ALU.mult))
                K.dma(sp, y_d[seq, t * 128:(t + 1) * 128, :], xt[:], [XTR], [YR[seq][t]])
                return
            xb = NBF.xb[slot]
            XBR = NBF.xbr[slot]
            K.op(dve, [XTR, NBF.sqr[slot], GBR], [XBR], lambda: nc.vector.scalar_tensor_tensor(out=xb[:], in0=xt[:], scalar=NBF.rs[slot][:], in1=gbc[:], op0=ALU.mult, op1=ALU.mult))
            for c in range(8):
                K.op(pe, [XBR, CR], [PSBR], lambda: nc.tensor.transpose(PSB[:, c * 128:(c + 1) * 128], xb[:, c * 128:(c + 1) * 128], ident_b[:]), inc=(c == 7))
            K.op(act, [PSBR], [XNR[t]], lambda: nc.scalar.copy(out=xnT[:, :, t * 128:(t + 1) * 128], in_=PSB[:, :].rearrange("p (c n) -> p c n", n=128)))

        def alloc_normbufs(st):
            NBF = NormBufs()
            NBF.xt = [sb("nb_xt%d" % i, [128, D], F32, st) for i in range(2)]
            NBF.xtr = [Reg() for i in range(2)]
            NBF.sq = [sb("nb_sq%d" % i, [128, D], BF16, st) for i in range(2)]
            NBF.ss = [sb("nb_ss%d" % i, [128, 1], F32, st) for i in range(2)]
            NBF.rs = [sb("nb_rs%d" % i, [128, 1], F32, st) for i in range(2)]
            NBF.sqr = [Reg() for i in range(2)]
            NBF.xb = [sb("nb_xb%d" % i, [128, D], BF16, st) for i in range(2)]
            NBF.xbr = [Reg() for i in range(2)]
            NBF.eps = sb("nb_eps", [128, 1], F32, st)
            K.op(dve, [], [NBF.sqr[0], NBF.sqr[1]], lambda: nc.vector.memset(NBF.eps[:], 1e-6))
            return NBF

        bank_rr = [0]

        def next_bank(lo=0, hi=7):
            b = lo + bank_rr[0] % (hi - lo)
            bank_rr[0] += 1
            return b

        def proj_fm(W, WR, wcol, M, dst_fn, dst_reg, scale=None, evac_eng=None):
            for n in range(NCH):
                b = next_bank()
                for c in range(8):
                    K.op(pe, [WR] + XNR[n * 4:(n + 1) * 4], [PSR[b]],
                         lambda: nc.tensor.matmul(PS[b][0:M, :], lhsT=W[:, c, wcol:wcol + M], rhs=xnT[:, c, n * 512:(n + 1) * 512], start=(c == 0), stop=(c == 7)), inc=(c == 7))
                dst = dst_fn(n)
                if scale is None:
                    K.op(dve, [PSR[b]], [dst_reg], lambda: nc.vector.tensor_copy(out=dst, in_=PS[b][0:M, :]))
                else:
                    K.op(dve, [PSR[b]], [dst_reg], lambda: nc.vector.tensor_scalar(out=dst, in0=PS[b][0:M, :], scalar1=float(scale), scalar2=None, op0=ALU.mult))

        def proj_tm(W, WR, wcol, ncol, evac_fn):
            for t in range(NT):
                b = next_bank()
                for c in range(8):
                    K.op(pe, [WR, XNR[t]], [PSR[b]],
                         lambda: nc.tensor.matmul(PS[b][:, 0:ncol], lhsT=xnT[:, c, t * 128:(t + 1) * 128], rhs=W[:, c, wcol:wcol + ncol], start=(c == 0), stop=(c == 7)), inc=(c == 7))
                evac_fn(t, PS[b], PSR[b])

        def load_win(l, W, WR, c0, c1, dcol=0):
            K.dma(pool, W[:, :, dcol:dcol + (c1 - c0)], win_d[l][:, c0:c1].rearrange("(c p) n -> p c n", p=128), [], [WR])

        for seq in range(NSEQ):
            with ExitStack() as st:
                NBF = alloc_normbufs(st)
                load_g(gmix_d[0])
                for t in range(NT):
                    slot = t % 2
                    xt = NBF.xt[slot]
                    K.dma(sp, xt[:], x_d[seq, t * 128:(t + 1) * 128, :], [], [NBF.xtr[slot]])
                    K.dma(sp, y_d[seq, t * 128:(t + 1) * 128, :], xt[:], [NBF.xtr[slot]], [YR[seq][t]])
                    norm_tile(NBF, seq, t, y_d[seq, t * 128:(t + 1) * 128, :], None, False, slot)
                K.barrier()

            for l in range(DEPTH):
                with ExitStack() as mst:
                    ast = ExitStack()
                    oT = sb("oT", [128, 4, 2, S], BF16, mst)
                    OTR = [Reg("oT%d" % m) for m in range(4)]
                    QT = [sb("QT%d" % h, [72, S], BF16, ast) for h in range(4)]
                    KT = [sb("KT%d" % h, [72, S], BF16, ast) for h in range(4)]
                    QTR = [Reg("QT%d" % h) for h in range(4)]
                    QAR = [Reg("QA%d" % h) for h in range(4)]
                    KTR = [Reg("KT%d" % h) for h in range(4)]
                    V = sb("V", [128, NT, 4, 65], BF16, ast)
                    VR = Reg("V")
                    OTOK = sb("OTOK", [128, NT, 256], BF16, ast)
                    OKR = Reg("OTOK")
                    W = [sb("Wm%d" % i, [128, 8, 776], BF16, ast) for i in range(2)]
                    WR = [Reg("Wm%d" % i) for i in range(2)]
                    PT = [sb("PT%d" % i, [128, 512], BF16, ast) for i in range(2)]
                    PTR = [Reg("PT%d" % i) for i in range(2)]
                    orec = sb("orec", [128, 4], F32, ast)
                    ORR = Reg("orec")
                    K.op(dve, [], [VR], lambda: nc.vector.memset(V[:], 1.0))

                    def v_evac(nheads):
                        def f(t, ps, psr):
                            K.op(dve, [psr], [VR], lambda: nc.vector.tensor_copy(out=V[:, t, 0:nheads, 0:64], in_=ps[:, 0:nheads * 64].rearrange("p (h d) -> p h d", d=64)))
                        return f

                    def qk_proj(Wt, WRt, base, with_scale=True):
                        for h in range(4):
                            proj_fm(Wt, WRt, base + h * 64, 64, (lambda n, h=h: QT[h][0:64, n * 512:(n + 1) * 512]), QTR[h], scale=0.125)
                            proj_fm(Wt, WRt, base + 256 + h * 64, 64, (lambda n, h=h: KT[h][0:64, n * 512:(n + 1) * 512]), KTR[h])

                    def finish_o(ob, obr, tq0, nsub, h, normalize):
                        o3 = PS[ob][:, 0:nsub * 65].rearrange("p (s d) -> p s d", d=65)
                        dst = OTOK[:, tq0:tq0 + nsub, h * 64:(h + 1) * 64]
                        if normalize:
                            K.op(dve, [obr], [ORR], lambda: nc.vector.reciprocal(out=orec[:, 0:nsub], in_=o3[:, :, 64]))
                            for s_ in range(nsub):
                                K.op(dve, [obr, ORR], [OKR], lambda: nc.vector.tensor_scalar(out=OTOK[:, tq0 + s_, h * 64:(h + 1) * 64], in0=o3[:, s_, 0:64], scalar1=orec[:, s_:s_ + 1], scalar2=None, op0=ALU.mult))
                        else:
                            K.op(dve, [obr], [OKR], lambda: nc.vector.tensor_copy(out=dst, in_=o3[:, :, 0:64]))

                    def otok_to_oT(m):
                        if dbg and seq == 0 and l == 0:
                            K.dma(pool, dbg_d[m, :, 0:256].rearrange("(t p) c -> p t c", p=128), OTOK[:], [OKR], [])
                        for t in range(NT):
                            for k in range(2):
                                K.op(pe, [OKR, CR], [PSBR], lambda: nc.tensor.transpose(PSB[:, k * 128:(k + 1) * 128], OTOK[:, t, k * 128:(k + 1) * 128], ident_b[:]), inc=(k == 1))
                            K.op(act, [PSBR], [OTR[m]], lambda: nc.scalar.copy(out=oT[:, m, :, t * 128:(t + 1) * 128], in_=PSB[:, 0:256].rearrange("p (c n) -> p c n", n=128)))

                    def attn_softmax(h, kdim, near_bias, diag_mask):
                        for cq in range(NCH):
                            ob = 6
                            K.op(dve, [], [PSR[ob]], lambda: nc.vector.memset(PS[ob][:, 0:260], 0.0))
                            t0 = cq * 512
                            na = 4 * cq + 4

                            def stA(a):
                                r = a - 4 * cq
                                off = max(0, r) * 128
                                xb = a % 2
                                extra = []
                                if near_bias is not None:
                                    if r >= 0:
                                        w_ = min(256, 512 - off)
                                        extra.append((ident_b, tbias[:, near_bias, 0:w_], off, w_))
                                    elif r == -1:
                                        extra.append((ident_b, tbias[:, near_bias, 128:256], 0, 128))
                                elif diag_mask is not None and r >= 0:
                                    extra.append((ident_b, diag_mask[:, :], off, 128))
                                K.op(pe, [KTR[h], QTR[h], QAR[h]], [PSR[xb]], lambda: nc.tensor.matmul(PS[xb][:, off:512], lhsT=KT[h][0:kdim, a * 128:(a + 1) * 128], rhs=QT[h][0:kdim, t0 + off:t0 + 512], start=True, stop=(len(extra) == 0), skip_group_check=True), inc=(len(extra) == 0))
                                for ei, (lt, rh, eo, ew) in enumerate(extra):
                                    K.op(pe, [CR], [PSR[xb]], lambda: nc.tensor.matmul(PS[xb][:, eo:eo + ew], lhsT=lt[:, :], rhs=rh, start=False, stop=(ei == len(extra) - 1), skip_group_check=True), inc=(ei == len(extra) - 1))

                            def stB(a):
                                r = a - 4 * cq
                                off = max(0, r) * 128
                                xb = a % 2
                                pt, ptr = PT[a % 2], PTR[a % 2]
                                K.op(act, [PSR[xb]], [ptr], lambda: nc.scalar.activation(out=pt[:, off:512], in_=PS[xb][:, off:512], func=AF.Exp))
                                for s_ in range(max(0, r), 4):
                                    K.op(pe, [ptr, VR], [PSR[ob]], lambda: nc.tensor.matmul(PS[ob][:, s_ * 65:(s_ + 1) * 65], lhsT=pt[:, s_ * 128:(s_ + 1) * 128], rhs=V[:, a, h, :], start=False, stop=False, skip_group_check=True), inc=(s_ == 3))
                            stA(0)
                            for a in range(na):
                                if a + 1 < na:
                                    stA(a + 1)
                                stB(a)
                            finish_o(ob, PSR[ob], cq * 4, 4, h, True)

                    load_win(l, W[0], WR[0], C_SB, C_SB + 768)
                    load_win(l, W[1], WR[1], C_MB, C_MB + 768)
                    qk_proj(W[0], WR[0], 0)
                    proj_tm(W[0], WR[0], 512, 256, v_evac(4))
                    with ExitStack() as st:
                        E1 = sb("E1", [128, 512], F32, st)
                        SPb = [sb("SPb%d" % i, [128, 512], BF16, st) for i in range(2)]
                        SACC = sb("SACC", [128, 512], F32, st)
                        SACCB = sb("SACCB", [128, 512], BF16, st)
                        E1R, SAR, SBR = Reg(), Reg(), Reg()
                        SPR = [Reg(), Reg()]
                        for h in range(4):
                            for cq in range(NCH):
                                ob = 6
                                K.op(dve, [], [PSR[ob]], lambda: nc.vector.memset(PS[ob][:, 0:260], 0.0))
                                K.op(dve, [], [SAR], lambda: nc.vector.memset(SACC[:], 0.0))
                                t0 = cq * 512
                                atop = 4 * cq + 3

                                def offs(a):
                                    return max(0, a - 4 * cq) * 128

                                def stA(a):
                                    r = a - 4 * cq
                                    off = offs(a)
                                    xb = a % 3
                                    spb, spr = SPb[a % 2], SPR[a % 2]
                                    K.op(pe, [KTR[h], QTR[h]], [PSR[xb]], lambda: nc.tensor.matmul(PS[xb][:, off:512], lhsT=KT[h][0:64, a * 128:(a + 1) * 128], rhs=QT[h][0:64, t0 + off:t0 + 512], start=True, stop=False, skip_group_check=True), inc=(r < 0))
                                    if r >= 0:
                                        K.op(pe, [CR], [PSR[xb]], lambda: nc.tensor.matmul(PS[xb][:, off:off + 128], lhsT=ident_b[:, :], rhs=cmstrict[:, :], start=False, stop=False, skip_group_check=True))
                                    K.op(act, [PSR[xb]], [E1R], lambda: nc.scalar.activation(out=E1[:, off:512], in_=PS[xb][:, off:512], func=AF.Exp))
                                    K.op(act, [E1R], [spr], lambda: nc.scalar.activation(out=spb[:, off:512], in_=E1[:, off:512], func=AF.Ln, bias=1.0))

                                def stB1(a):
                                    off = offs(a)
                                    xb = a % 3
                                    spb, spr = SPb[a % 2], SPR[a % 2]
                                    first = (a == atop)
                                    prev_off = 512 if first else offs(a + 1)
                                    K.op(pe, [spr, CR], [PSR[xb]], lambda: nc.tensor.matmul(PS[xb][:, off:512], lhsT=uinclneg[:, :], rhs=spb[:, off:512], start=False, stop=first, skip_group_check=True), inc=first)
                                    if not first:
                                        K.op(pe, [SBR, CR], [PSR[xb]], lambda: nc.tensor.matmul(PS[xb][:, prev_off:512], lhsT=onesneg[:, :], rhs=SACCB[:, prev_off:512], start=False, stop=True, skip_group_check=True))
                                    if a > 0:
                                        K.op(dve, [spr, SAR], [SAR], lambda: nc.vector.tensor_tensor(out=SACC[:, off:512], in0=SACC[:, off:512], in1=spb[:, off:512], op=ALU.add))
                                        K.op(dve, [SAR], [SBR], lambda: nc.vector.tensor_copy(out=SACCB[:, off:512], in_=SACC[:, off:512]))

                                def stB2(a):
                                    r = a - 4 * cq
                                    off = offs(a)
                                    xb = a % 3
                                    pt, ptr = PT[a % 2], PTR[a % 2]
                                    K.op(act, [PSR[xb]], [ptr], lambda: nc.scalar.activation(out=pt[:, off:512], in_=PS[xb][:, off:512], func=AF.Exp))
                                    for s_ in range(max(0, r), 4):
                                        K.op(pe, [ptr, VR], [PSR[ob]], lambda: nc.tensor.matmul(PS[ob][:, s_ * 65:s_ * 65 + 64], lhsT=pt[:, s_ * 128:(s_ + 1) * 128], rhs=V[:, a, h, 0:64], start=False, stop=False, skip_group_check=True), inc=(s_ == 3))
                                T_ = list(range(atop, -1, -1))
                                for k_ in range(len(T_) + 2):
                                    if k_ < len(T_):
                                        stA(T_[k_])
                                    if 0 <= k_ - 1 < len(T_):
                                        stB1(T_[k_ - 1])
                                    if 0 <= k_ - 2 < len(T_):
                                        stB2(T_[k_ - 2])
                                finish_o(ob, PSR[ob], cq * 4, 4, h, False)
                        otok_to_oT(0)
                        K.barrier()

                    load_win(l, W[0], WR[0], C_FX, C_FX + 772)
                    qk_proj(W[1], WR[1], 0)
                    proj_tm(W[1], WR[1], 512, 256, v_evac(4))
                    with ExitStack() as st:
                        kmean = sb("kmean", [64, 4, 8], F32, st)
                        kmb = sb("kmb", [64, 4, 8], BF16, st)
                        KMR = Reg()
                        NSET = 2
                        G8s = [sb("G8_%d" % i_, [128, 8], F32, st) for i_ in range(NSET)]
                        M8s = [sb("M8_%d" % i_, [128, 8], F32, st) for i_ in range(NSET)]
                        thrs = [sb("thr_%d" % i_, [128, 1], F32, st) for i_ in range(NSET)]
                        SELTs = [sb("SELT_%d" % i_, [128, 72], F32, st) for i_ in range(NSET)]
                        GRs = [Reg() for i_ in range(NSET)]
                        SELRs = [Reg() for i_ in range(NSET)]
                        PSBRs = [Reg() for i_ in range(NSET)]
                        for i_ in range(NSET):
                            K.op(dve, [], [SELRs[i_]], lambda: nc.vector.memset(SELTs[i_][:], 0.0))
                        for h in range(4):
                            K.dma(pool, KT[h][64:72, :], cst["c_blk"], [], [KTR[h]])
                            K.op(dve, [KTR[h]], [KMR], lambda: nc.vector.tensor_reduce(out=kmean[:, h, 0:NB], in_=KT[h][0:64, :].rearrange("p (n k) -> p n k", k=256), axis=AX.X, op=ALU.add))
                        K.op(dve, [KMR], [KMR], lambda: nc.vector.tensor_scalar(out=kmb[:], in0=kmean[:], scalar1=1.0 / 256, scalar2=None, op0=ALU.mult))
                        chain = 0
                        for h in range(4):
                            for t in range(NT):
                                own = t // 2
                                k_ = chain % NSET
                                chain += 1
                                G8, M8, thr, SELT, GR, SELR = G8s[k_], M8s[k_], thrs[k_], SELTs[k_], GRs[k_], SELRs[k_]
                                K.op(dve, [], [SELR], lambda: nc.vector.memset(SELT[:, 64:72], 0.0))
                                if own > 0:
                                    b = 2 + k_
                                    K.op(pe, [QTR[h], KMR], [PSR[b]], lambda: nc.tensor.matmul(PS[b][:, 0:8], lhsT=QT[h][0:64, t * 128:(t + 1) * 128], rhs=kmb[:, h, :], start=True, stop=True))
                                    K.op(dve, [], [GR], lambda: nc.vector.memset(G8[:], -1e30))
                                    K.op(dve, [PSR[b]], [GR], lambda: nc.vector.tensor_copy(out=G8[:, 0:own], in_=PS[b][:, 0:own]))
                                    K.op(dve, [GR], [GR], lambda: nc.vector.max(out=M8[:], in_=G8[:]))
                                    K.op(dve, [GR], [GR], lambda: nc.vector.tensor_scalar(out=thr[:], in0=M8[:, TOPK - 1:TOPK], scalar1=-1e29, scalar2=None, op0=ALU.max))
                                    K.op(dve, [GR], [SELR], lambda: nc.vector.tensor_scalar(out=SELT[:, 64:64 + own], in0=G8[:, 0:own], scalar1=thr[:], scalar2=NEG, op0=ALU.is_lt, op1=ALU.mult))
                                tb_ = 4 + k_
                                K.op(pe, [SELR, CR], [PSR[tb_]], lambda: nc.tensor.transpose(PS[tb_][0:72, 0:128], SELT[:, :], ident_f[:]))
                                K.op(act, [PSR[tb_]], [QAR[h]], lambda: nc.scalar.copy(out=QT[h][64:72, t * 128:(t + 1) * 128], in_=PS[tb_][64:72, 0:128]))
                        K.barrier()
                        for h in range(4):
                            attn_softmax(h, 72, h, None)
                        otok_to_oT(1)
                        K.barrier()

                    load_win(l, W[1], WR[1], C_DQ, C_DQ + 680)
                    load_win(l, W[1], WR[1], C_IK, C_IK + 32, dcol=680)
                    load_win(l, W[1], WR[1], C_IK, C_IK + 32, dcol=712)
                    qk_proj(W[0], WR[0], 0)
                    proj_tm(W[0], WR[0], 512, 256, v_evac(4))
                    with ExitStack() as st:
                        fb = sb("fb", [4, 1], F32, st)
                        ef = sb("ef", [4, S], F32, st)
                        spf = sb("spf", [4, S], F32, st)
                        ones4 = sb("ones4", [4, S], F32, st)
                        cc = sb("cc", [4, S], F32, st)
                        chi = sb("chi", [4, S], BF16, st)
                        clo = sb("clo", [4, S], BF16, st)
                        nchi = sb("nchi", [4, S], BF16, st)
                        nclo = sb("nclo", [4, S], BF16, st)
                        FR = Reg()
                        K.dma(sp, fb[:], fxb_d[l].rearrange("(p o) -> p o", o=1), [], [FR])
                        K.op(dve, [FR], [FR], lambda: nc.vector.tensor_scalar(out=fb[:], in0=fb[:], scalar1=-1.0, scalar2=None, op0=ALU.mult))
                        K.op(dve, [], [FR], lambda: nc.vector.memset(ones4[:], 1.0))
                        for n in range(NCH):
                            b = next_bank()
                            for c in range(8):
                                K.op(pe, [WR[0]] + XNR[n * 4:(n + 1) * 4], [PSR[b]], lambda: nc.tensor.matmul(PS[b][0:4, :], lhsT=W[0][:, c, 768:772], rhs=xnT[:, c, n * 512:(n + 1) * 512], start=(c == 0), stop=(c == 7)), inc=(c == 7))
                            K.op(act, [PSR[b], FR], [FR], lambda: nc.scalar.activation(out=ef[:, n * 512:(n + 1) * 512], in_=PS[b][0:4, :], func=AF.Exp, scale=-1.0, bias=fb[:]))
                        K.op(act, [FR], [FR], lambda: nc.scalar.activation(out=spf[:], in_=ef[:], func=AF.Ln, bias=1.0))
                        K.op(dve, [FR], [FR], lambda: nc.vector.tensor_tensor_scan(out=cc[:], data0=ones4[:], data1=spf[:], initial=0.0, op0=ALU.mult, op1=ALU.subtract))
                        K.op(dve, [FR], [FR], lambda: nc.vector.tensor_copy(out=chi[:], in_=cc[:]))
                        K.op(dve, [FR], [FR], lambda: nc.vector.tensor_tensor(out=clo[:], in0=cc[:], in1=chi[:], op=ALU.subtract))
                        K.op(dve, [FR], [FR], lambda: nc.vector.tensor_scalar(out=nchi[:], in0=chi[:], scalar1=-1.0, scalar2=None, op0=ALU.mult))
                        K.op(dve, [FR], [FR], lambda: nc.vector.tensor_scalar(out=nclo[:], in0=clo[:], scalar1=-1.0, scalar2=None, op0=ALU.mult))
                        for h in range(4):
                            K.dma(sp, QT[h][64:65, :], chi[h:h + 1, :], [FR], [QTR[h]])
                            K.dma(sp, QT[h][65:66, :], clo[h:h + 1, :], [FR], [QTR[h]])
                            K.dma(pool, QT[h][66:68, :], cst["c_ones"][0:2, :], [], [QTR[h]])
                            K.dma(pool, KT[h][64:66, :], cst["c_ones"][0:2, :], [], [KTR[h]])
                            K.dma(sp, KT[h][66:67, :], nchi[h:h + 1, :], [FR], [KTR[h]])
                            K.dma(sp, KT[h][67:68, :], nclo[h:h + 1, :], [FR], [KTR[h]])
                        for h in range(4):
                            attn_softmax(h, 68, None, cmincl)
                        otok_to_oT(2)
                        K.barrier()

                    for h in range(4):
                        proj_fm(W[1], WR[1], h * 64, 64, (lambda n, h=h: QT[h][0:64, n * 512:(n + 1) * 512]), QTR[h], scale=0.125)
                    proj_fm(W[1], WR[1], 256, 64, (lambda n: KT[0][0:64, n * 512:(n + 1) * 512]), KTR[0])
                    proj_tm(W[1], WR[1], 320, 64, v_evac(1))
                    with ExitStack() as st:
                        QI = [sb("QI%d" % i, [64, S], BF16, st) for i in range(4)]
                        KI = sb("KI", [64, S], BF16, st)
                        QIR, KIR = Reg(), Reg()
                        WI = sb("WI", [128, NT, 8], F32, st)
                        WA = sb("WA", [128, NT, 8], F32, st)
                        WS = sb("WS", [128, NT, 8], F32, st)
                        WIR = Reg()
                        ISC = sb("ISC", [128, S], F32, st)
                        ISR = Reg()
                        RT = [sb("RT%d" % i, [128, 512], F32, st) for i in range(2)]
                        RTR = [Reg(), Reg()]
                        MNB = [sb("MNEG%d" % i_, [128, S], BF16, st) for i_ in range(2)]
                        MNRB = [Reg(), Reg()]
                        MZ = sb("MZ", [128, 128], BF16, st)
                        MZR = Reg()
                        lo = sb("lo", [128, 1], F32, st)
                        hi_ = sb("hi_", [128, 1], F32, st)
                        hw = sb("hw", [128, NIT], F32, st)
                        mid = sb("mid", [128, 1], F32, st)
                        cnt = sb("cnt", [128, 1], F32, st)
                        tmp1 = sb("tmp1", [128, 1], F32, st)
                        BR = Reg()
                        for i in range(4):
                            proj_fm(W[1], WR[1], 384 + i * 64, 64, (lambda n, i=i: QI[i][0:64, n * 512:(n + 1) * 512]), QIR)
                        proj_fm(W[1], WR[1], 680, 64, (lambda n: KI[0:64, n * 512:(n + 1) * 512]), KIR)

                        def wi_evac(t, ps, psr):
                            K.op(dve, [psr], [WIR], lambda: nc.vector.tensor_copy(out=WI[:, t, :], in_=ps[:, 0:8]))
                        proj_tm(W[1], WR[1], 672, 8, wi_evac)
                        K.op(act, [WIR], [WIR], lambda: nc.scalar.activation(out=WA[:], in_=WI[:], func=AF.Abs))
                        K.op(act, [WIR], [WIR], lambda: nc.scalar.activation(out=WS[:], in_=WI[:], func=AF.Sign))
                        K.op(dve, [], [MZR], lambda: nc.vector.memset(MZ[:], 0.0))
                        def sel(i):
                            t1 = (i + 1) * 128
                            MNEG, MNR = MNB[i % 2], MNRB[i % 2]
                            for kc in range((t1 + 511) // 512):
                                k0 = kc * 512
                                kw = min(512, t1 - k0)
                                for hi in range(8):
                                    b = next_bank(2, 6)
                                    rt, rtr = RT[hi % 2], RTR[hi % 2]
                                    K.op(pe, [QIR, KIR], [PSR[b]], lambda: nc.tensor.matmul(PS[b][:, 0:kw], lhsT=QI[hi // 2][(hi % 2) * 32:(hi % 2) * 32 + 32, i * 128:(i + 1) * 128], rhs=KI[(hi % 2) * 32:(hi % 2) * 32 + 32, k0:k0 + kw], start=True, stop=True))
                                    K.op(act, [PSR[b], WIR], [rtr], lambda: nc.scalar.activation(out=rt[:, 0:kw], in_=PS[b][:, 0:kw], func=AF.Relu, scale=WA[:, i, hi:hi + 1]))
                                    if hi == 0:
                                        K.op(dve, [rtr, WIR], [ISR], lambda: nc.vector.tensor_scalar(out=ISC[:, k0:k0 + kw], in0=rt[:, 0:kw], scalar1=WS[:, i, hi:hi + 1], scalar2=None, op0=ALU.mult))
                                    else:
                                        K.op(dve, [rtr, WIR, ISR], [ISR], lambda: nc.vector.scalar_tensor_tensor(out=ISC[:, k0:k0 + kw], in0=rt[:, 0:kw], scalar=WS[:, i, hi:hi + 1], in1=ISC[:, k0:k0 + kw], op0=ALU.mult, op1=ALU.add))
                            K.op(dve, [ISR], [BR], lambda: nc.vector.tensor_reduce(out=lo[:], in_=ISC[:, 0:t1], axis=AX.X, op=ALU.min))
                            K.op(dve, [ISR], [BR], lambda: nc.vector.tensor_reduce(out=hi_[:], in_=ISC[:, 0:t1], axis=AX.X, op=ALU.max))
                            K.op(dve, [ISR, CR], [ISR], lambda: nc.vector.tensor_tensor(out=ISC[:, i * 128:t1], in0=ISC[:, i * 128:t1], in1=adm[:, :], op=ALU.add))
                            K.op(dve, [BR], [BR], lambda: nc.vector.tensor_tensor(out=hi_[:], in0=hi_[:], in1=lo[:], op=ALU.subtract))
                            K.op(dve, [BR, CR], [BR], lambda: nc.vector.tensor_scalar(out=hw[:], in0=powt[:], scalar1=hi_[:], scalar2=None, op0=ALU.mult))
                            K.op(dve, [BR], [BR], lambda: nc.vector.tensor_tensor(out=mid[:], in0=lo[:], in1=hw[:, 0:1], op=ALU.add))
                            for it in range(NIT):
                                K.op(dve, [BR, ISR], [BR, MNR], lambda: nc.vector.tensor_scalar(out=MNEG[:, 0:t1], in0=ISC[:, 0:t1], scalar1=mid[:], scalar2=0.0, op0=ALU.is_ge, op1=ALU.add, accum_out=cnt[:]))
                                K.op(dve, [BR], [BR], lambda: nc.vector.tensor_scalar(out=tmp1[:], in0=cnt[:], scalar1=float(KSEL) - 0.5, scalar2=hw[:, it:it + 1], op0=ALU.is_ge, op1=ALU.mult))
                                nxt = it + 1 if it + 1 < NIT else it
                                K.op(dve, [BR], [BR], lambda: nc.vector.tensor_scalar(out=mid[:], in0=mid[:], scalar1=hw[:, nxt:nxt + 1], scalar2=tmp1[:], op0=ALU.subtract, op1=ALU.add))
                            K.op(dve, [BR], [BR], lambda: nc.vector.tensor_copy(out=lo[:], in_=mid[:]))
                            K.op(dve, [BR, ISR], [MNR], lambda: nc.vector.tensor_scalar(out=MNEG[:, 0:t1], in0=ISC[:, 0:t1], scalar1=lo[:], scalar2=NEG, op0=ALU.is_lt, op1=ALU.mult))
                        def attn(i):
                            t1 = (i + 1) * 128
                            need_sel = t1 > KSEL
                            MNEG, MNR = MNB[i % 2], MNRB[i % 2]
                            ob = 6
                            def dA(a):
                                xb = a % 2
                                K.op(pe, [MNR if need_sel else MZR, CR], [PSR[xb]], lambda: nc.tensor.matmul(PS[xb][:, :], lhsT=(MNEG[:, a * 128:(a + 1) * 128] if need_sel else MZ[:, :]), rhs=ident4[:, :], start=True, stop=False, skip_group_check=True), inc=False)
                                for h in range(4):
                                    lastmm = (h == 3) and (a < i - 1)
                                    K.op(pe, [KTR[0], QTR[h]], [PSR[xb]], lambda: nc.tensor.matmul(PS[xb][:, h * 128:(h + 1) * 128], lhsT=KT[0][0:64, a * 128:(a + 1) * 128], rhs=QT[h][0:64, i * 128:(i + 1) * 128], start=False, stop=lastmm, skip_group_check=True), inc=lastmm)
                                if a >= i - 1:
                                    for h in range(4):
                                        tb = tbias[:, 4 + h, 0:128] if a == i else tbias[:, 4 + h, 128:256]
                                        K.op(pe, [CR], [PSR[xb]], lambda: nc.tensor.matmul(PS[xb][:, h * 128:(h + 1) * 128], lhsT=ident_b[:, :], rhs=tb, start=False, stop=(h == 3), skip_group_check=True), inc=(h == 3))
                            def dB(a):
                                xb = a % 2
                                pt, ptr = PT[a % 2], PTR[a % 2]
                                K.op(act, [PSR[xb]], [ptr], lambda: nc.scalar.activation(out=pt[:, :], in_=PS[xb][:, :], func=AF.Exp))
                                for h in range(4):
                                    K.op(pe, [ptr, VR], [PSR[ob]], lambda: nc.tensor.matmul(PS[ob][:, h * 65:(h + 1) * 65], lhsT=pt[:, h * 128:(h + 1) * 128], rhs=V[:, a, 0, :], start=False, stop=False, skip_group_check=True), inc=(h == 3))
                            dA(0)
                            for a in range(i + 1):
                                if a + 1 <= i:
                                    dA(a + 1)
                                dB(a)
                            o3 = PS[ob][:, 0:260].rearrange("p (s d) -> p s d", d=65)
                            K.op(dve, [PSR[ob]], [ORR], lambda: nc.vector.reciprocal(out=orec[:, 0:4], in_=o3[:, :, 64]))
                            for h in range(4):
                                K.op(dve, [PSR[ob], ORR], [OKR], lambda: nc.vector.tensor_scalar(out=OTOK[:, i, h * 64:(h + 1) * 64], in0=o3[:, h, 0:64], scalar1=orec[:, h:h + 1], scalar2=None, op0=ALU.mult))
                        for i in range(NT):
                            K.op(dve, [], [PSR[6]], lambda: nc.vector.memset(PS[6][:, 0:260], 0.0))
                            if (i + 2) * 128 > KSEL and i + 1 < NT:
                                sel(i + 1)
                            attn(i)
                        otok_to_oT(3)
                        K.barrier()

                    ast.close()
                    with ExitStack() as st:
                        mixT = sb("mixT", [128, 8, S], BF16, st)
                        MXR = Reg()
                        WG = [sb("WG%d" % i, [128, 4, 8, 128], BF16, st) for i in range(2)]
                        WB = [sb("WB%d" % i, [128, 4, 2, 128], BF16, st) for i in range(2)]
                        WGR = [Reg(), Reg()]
                        SG = [sb("SG%d" % i, [128, 512], F32, st) for i in range(2)]
                        SGR = [Reg(), Reg()]
                        ACC = sb("ACCg", [128, 512], F32, st)
                        TMP = sb("TMPg", [128, 512], F32, st)
                        ACR = Reg()

                        def load_gw(f):
                            s_ = f % 2
                            for i in range(4):
                                K.dma(pool, WG[s_][:, i, :, :], wg_d[l, i][:, f * 128:(f + 1) * 128].rearrange("(c p) n -> p c n", p=128), [], [WGR[s_]])
                                K.dma(pool, WB[s_][:, i, :, :], wb_d[l, i][:, f * 128:(f + 1) * 128].rearrange("(c p) n -> p c n", p=128), [], [WGR[s_]])
                        load_gw(0)
                        for f in range(8):
                            if f + 1 < 8:
                                load_gw(f + 1)
                            s_ = f % 2
                            for n in range(NCH):
                                for i in range(4):
                                    gb = next_bank(0, 3)
                                    bb = 3 + next_bank(0, 3)
                                    for c in range(8):
                                        K.op(pe, [WGR[s_]] + XNR[n * 4:(n + 1) * 4], [PSR[gb]], lambda: nc.tensor.matmul(PS[gb][:, :], lhsT=WG[s_][:, i, c, :], rhs=xnT[:, c, n * 512:(n + 1) * 512], start=(c == 0), stop=(c == 7)), inc=(c == 7))
                                    for k in range(2):
                                        K.op(pe, [WGR[s_], OTR[i]], [PSR[bb]], lambda: nc.tensor.matmul(PS[bb][:, :], lhsT=WB[s_][:, i, k, :], rhs=oT[:, i, k, n * 512:(n + 1) * 512], start=(k == 0), stop=(k == 1)), inc=(k == 1))
                                    sg, sgr = SG[i % 2], SGR[i % 2]
                                    K.op(act, [PSR[gb]], [sgr], lambda: nc.scalar.activation(out=sg[:], in_=PS[gb][:, :], func=AF.Sigmoid))
                                    if i == 0:
                                        K.op(dve, [sgr, PSR[bb]], [ACR], lambda: nc.vector.tensor_tensor(out=ACC[:], in0=sg[:], in1=PS[bb][:, :], op=ALU.mult))
                                    else:
                                        K.op(dve, [sgr, PSR[bb]], [ACR], lambda: nc.vector.tensor_tensor(out=TMP[:], in0=sg[:], in1=PS[bb][:, :], op=ALU.mult))
                                        if i < 3:
                                            K.op(dve, [ACR], [ACR], lambda: nc.vector.tensor_tensor(out=ACC[:], in0=ACC[:], in1=TMP[:], op=ALU.add))
                                        else:
                                            K.op(dve, [ACR], [MXR], lambda: nc.vector.tensor_tensor(out=mixT[:, f, n * 512:(n + 1) * 512], in0=ACC[:], in1=TMP[:], op=ALU.add))
                        WO = sb("WO", [128, 8, D], BF16, st)
                        WOR = Reg()
                        K.dma(pool, WO[:], wo_d[l].rearrange("(c p) n -> p c n", p=128), [], [WOR])
                        NBF = alloc_normbufs(st)
                        load_g(gffn_d[l])
                        def wo_mm(t):
                            banks = []
                            for hb in range(2):
                                b = (t % 2) * 2 + hb
                                for c in range(8):
                                    K.op(pe, [MXR, WOR], [PSR[b]], lambda: nc.tensor.matmul(PS[b][:, :], lhsT=mixT[:, c, t * 128:(t + 1) * 128], rhs=WO[:, c, hb * 512:(hb + 1) * 512], start=(c == 0), stop=(c == 7)), inc=(c == 7))
                                banks.append((PS[b], PSR[b]))
                            return banks
                        nxt_b = wo_mm(0)
                        for t in range(NT):
                            cur_b = nxt_b
                            if t + 1 < NT:
                                nxt_b = wo_mm(t + 1)
                            norm_tile(NBF, seq, t, y_d[seq, t * 128:(t + 1) * 128, :], cur_b, False, t % 2)
                        K.barrier()
                        if dbg and seq == 0 and l == 0:
                            K.dma(sp, dbg_d[4], y_d[0], [], [])
                            K.barrier()

                with ExitStack() as fst:
                    NH = S // TOKH
                    NCF = TOKH // 512
                    actT = sb("actT", [128, NJ, TOKH], BF16, fst)
                    ATR = Reg()
                    WD = sb("WD", [128, NJ, D], BF16, fst)
                    WDR = Reg()
                    K.dma(pool, WD[:, 0:11, :], wdn_d[l][0:11 * 128, :].rearrange("(c p) n -> p c n", p=128), [], [WDR])
                    K.dma(pool, WD[:, 11:22, :], wdn_d[l][11 * 128:22 * 128, :].rearrange("(c p) n -> p c n", p=128), [], [WDR])
                    HB = [[sb("HB%d%d" % (i, g), [128, TOKH + 2], F32, fst) for g in range(2)] for i in range(2)]
                    HBR = [[Reg(), Reg()], [Reg(), Reg()]]
                    HALO = sb("HALO", [128, NJ, 2, 2], F32, fst)
                    HLR = Reg()
                    WU = [sb("WU%d" % i, [128, 2, 8, 256], BF16, fst) for i in range(2)]
                    WUR = [Reg(), Reg()]
                    CWt = sb("CWt", [128, 4, 2 * NJ], F32, fst)
                    CWc = sb("CWc", [2 * NJ, 4, 128], F32, fst)
                    CWR = Reg()
                    for j_ in range(3):
                        K.dma(sp, CWc[:, j_, :], cw_d[l, j_].rearrange("(c p) -> c p", p=128), [], [CWR])
                    K.dma(sp, CWc[:, 3, :], cb_d[l].rearrange("(c p) -> c p", p=128), [], [CWR])
                    for j_ in range(4):
                        K.op(pe, [CWR, CR], [PSR[6]], lambda: nc.tensor.transpose(PS[6][:, j_ * 2 * NJ:(j_ + 1) * 2 * NJ], CWc[:, j_, :], ident_f[0:2 * NJ, 0:2 * NJ]), inc=(j_ == 3))
                    K.op(dve, [PSR[6]], [CWR], lambda: nc.vector.tensor_copy(out=CWt[:], in_=PS[6][:, 0:8 * NJ].rearrange("p (a c) -> p a c", c=2 * NJ)))
                    CA2 = [[[sb("CA%d_%d_%d" % (p_, g, n_), [128, 512], F32, fst) for n_ in range(TOKH // 512)] for g in range(2)] for p_ in range(2)]
                    CAR2 = [[[Reg() for n_ in range(TOKH // 512)] for g in range(2)] for p_ in range(2)]
                    GL2 = [sb("GL%d" % i_, [128, 512], F32, fst) for i_ in range(2)]
                    GLR2 = [Reg(), Reg()]
                    NBF = alloc_normbufs(fst)
                    final = (l == DEPTH - 1)
                    load_g(gfin_d[0] if final else gmix_d[l + 1])

                    def load_wu(jp):
                        s_ = jp % 2
                        for g in range(2):
                            K.dma(pool, WU[s_][:, g, :, :], wup_d[l][:, g * DFF + jp * 256:g * DFF + (jp + 1) * 256].rearrange("(c p) n -> p c n", p=128), [], [WUR[s_]])

                    for half in range(NH):
                        tk0 = half * TOKH
                        load_wu(0)

                        def st1(j):
                            if j % 2 == 0 and j // 2 + 1 < NJ // 2:
                                load_wu(j // 2 + 1)
                            s_ = j % 2
                            ws_ = (j // 2) % 2
                            wo_ = (j % 2) * 128
                            CA, CAR = CA2[s_], CAR2[s_]
                            for g in range(2):
                                hbuf, hbr = HB[s_][g], HBR[s_][g]
                                if half == 0:
                                    K.op(dve, [], [hbr], lambda: nc.vector.memset(hbuf[:, 0:2], 0.0))
                                else:
                                    K.op(dve, [HLR], [hbr], lambda: nc.vector.tensor_copy(out=hbuf[:, 0:2], in_=HALO[:, j, g, :]))
                                for n in range(NCF):
                                    b = next_bank(0, 6)
                                    tn = (tk0 + n * 512) // 128
                                    for c in range(8):
                                        K.op(pe, [WUR[ws_]] + XNR[tn:tn + 4], [PSR[b]], lambda: nc.tensor.matmul(PS[b][:, :], lhsT=WU[ws_][:, g, c, wo_:wo_ + 128], rhs=xnT[:, c, tk0 + n * 512:tk0 + (n + 1) * 512], start=(c == 0), stop=(c == 7)), inc=(c == 7))
                                    K.op(act, [PSR[b]], [hbr], lambda: nc.scalar.copy(out=hbuf[:, 2 + n * 512:2 + (n + 1) * 512], in_=PS[b][:, :]))
                                    ch_ = g * NJ + j
                                    K.op(act, [PSR[b], CWR], [CAR[g][n]], lambda: nc.scalar.activation(out=CA[g][n][:], in_=PS[b][:, :], func=AF.Identity, scale=CWt[:, 2, ch_:ch_ + 1], bias=CWt[:, 3, ch_:ch_ + 1]))
                                if half + 1 < NH:
                                    K.op(dve, [hbr], [HLR], lambda: nc.vector.tensor_copy(out=HALO[:, j, g, :], in_=hbuf[:, TOKH:TOKH + 2]))

                        def st2(j):
                            s_ = j % 2
                            CA, CAR = CA2[s_], CAR2[s_]
                            for n in range(NCF):
                                for g in range(2):
                                    hbuf, hbr = HB[s_][g], HBR[s_][g]
                                    ch = g * NJ + j
                                    ca, car = CA[g][n], CAR[g][n]
                                    K.op(dve, [hbr, CWR, car], [car], lambda: nc.vector.scalar_tensor_tensor(out=ca[:], in0=hbuf[:, 1 + n * 512:1 + (n + 1) * 512], scalar=CWt[:, 1, ch:ch + 1], in1=ca[:], op0=ALU.mult, op1=ALU.add))
                                    K.op(dve, [hbr, CWR, car], [car], lambda: nc.vector.scalar_tensor_tensor(out=ca[:], in0=hbuf[:, 0 + n * 512:0 + (n + 1) * 512], scalar=CWt[:, 0, ch:ch + 1], in1=ca[:], op0=ALU.mult, op1=ALU.add))
                                gl, glr = GL2[n % 2], GLR2[n % 2]
                                K.op(act, [CAR[0][n]], [glr], lambda: nc.scalar.activation(out=gl[:], in_=CA[0][n][:], func=AF.Gelu))
                                K.op(dve, [glr, CAR[1][n]], [ATR], lambda: nc.vector.tensor_tensor(out=actT[:, j, n * 512:(n + 1) * 512], in0=gl[:], in1=CA[1][n][:], op=ALU.mult))
                        st1(0)
                        for j in range(NJ):
                            if j + 1 < NJ:
                                st1(j + 1)
                            st2(j)
                        def dn_mm(tt):
                            t = tk0 // 128 + tt
                            banks = []
                            for hb in range(2):
                                b = (t % 2) * 2 + hb
                                for j in range(NJ):
                                    K.op(pe, [ATR, WDR], [PSR[b]], lambda: nc.tensor.matmul(PS[b][:, :], lhsT=actT[:, j, tt * 128:(tt + 1) * 128], rhs=WD[:, j, hb * 512:(hb + 1) * 512], start=(j == 0), stop=(j == NJ - 1)), inc=(j == NJ - 1))
                                banks.append((PS[b], PSR[b]))
                            return banks
                        nxt_b = dn_mm(0)
                        for tt in range(TOKH // 128):
                            t = tk0 // 128 + tt
                            cur_b = nxt_b
                            if tt + 1 < TOKH // 128:
                                nxt_b = dn_mm(tt + 1)
                            norm_tile(NBF, seq, t, y_d[seq, t * 128:(t + 1) * 128, :], cur_b, False, t % 2)
                        K.barrier()
                    if final:
                        for t in range(NT):
                            norm_tile(NBF, seq, t, y_d[seq, t * 128:(t + 1) * 128, :], None, True, t % 2)
                    K.barrier()
        K.barrier()
    return nc, hc


_CACHE = {}


def kernel(**inputs):
    S, DEPTH, NSEQ, NCORE = 2048, 4, 2, 8
    if "prog" not in _CACHE:
        _CACHE["prog"] = build(S, DEPTH, NSEQ)
    nc, hc = _CACHE["prog"]
    f = lambda a: np.ascontiguousarray(np.asarray(a, dtype=np.float32))
    x = f(inputs["x"])
    shared = {k: f(inputs[k]) for k in ["norm_mix_g", "norm_ffn_g", "w_in", "fox_b_f", "w_gate", "w_branch", "w_out", "w_up", "conv_w", "conv_b", "w_down"]}
    shared["norm_final_g"] = f(inputs["norm_final_g"]).reshape(1, D)
    shared["t5_bias"] = f(inputs["t5_bias"]).reshape(1, 256)
    shared.update(hc)
    in_maps = []
    for c in range(NCORE):
        m = dict(shared)
        m["x"] = np.ascontiguousarray(x[c * NSEQ:(c + 1) * NSEQ])
        in_maps.append(m)
    res = run_bass_kernel_spmd(nc, in_maps, core_ids=list(range(NCORE)))
    return np.concatenate([r["y"] for r in res.results], axis=0).astype(np.float32)
```

```python
import math
from contextlib import ExitStack
import numpy as np
import concourse.bass as bass
import concourse.mybir as mybir
from concourse.bass_utils import run_bass_kernel_spmd

F32 = mybir.dt.float32
BF16 = mybir.dt.bfloat16
AF = mybir.ActivationFunctionType
ALU = mybir.AluOpType
AX = mybir.AxisListType

D = 1024
HD = 64
PIN = 2988
DFF = 2816
NJ = DFF // 128
C_SB, C_MB, C_FX, C_FXF, C_DQ, C_DK, C_DV, C_IQ, C_IK, C_IW = 0, 768, 1536, 2304, 2308, 2564, 2628, 2692, 2948, 2980
NEG = -30000.0
NIT = 16
SAME_ENGINE_SYNC = True


class Sem:
    def __init__(self, h):
        self.h = h
        self.cnt = 0


class Eng:
    def __init__(self, e, sem, name):
        self.e = e
        self.sem = sem
        self.seen = {}
        self.name = name


class Reg:
    __slots__ = ("w", "r", "name")

    def __init__(self, name=""):
        self.w = None
        self.r = {}
        self.name = name


class KB:
    def __init__(self, nc, es):
        self.nc = nc
        self.es = es
        mk = lambda n: Sem(es.enter_context(nc.semaphore(n)))
        self.pe = Eng(nc.tensor, mk("s_pe"), "pe")
        self.act = Eng(nc.scalar, mk("s_act"), "act")
        self.dve = Eng(nc.vector, mk("s_dve"), "dve")
        self.pool = Eng(nc.gpsimd, mk("s_pool"), "pool")
        self.sp = Eng(nc.sync, mk("s_sp"), "sp")
        self.engs = [self.pe, self.act, self.dve, self.pool, self.sp]
        self.dsems = {"pool": [mk("d_pool%d" % i) for i in range(12)],
                      "sp": [mk("d_sp%d" % i) for i in range(12)]}
        self.drr = {"pool": 0, "sp": 0}
        self.allsems = [e.sem for e in self.engs] + self.dsems["pool"] + self.dsems["sp"]

    def _wait(self, E, deps):
        need = {}
        for (sem, val) in deps:
            if sem is E.sem and (E.name == "pe" or not SAME_ENGINE_SYNC):
                continue
            if need.get(sem, 0) < val:
                need[sem] = val
        for sem, val in need.items():
            if E.seen.get(sem, 0) < val:
                E.e.wait_ge(sem.h, val)
                E.seen[sem] = val

    def _deps(self, reads, writes):
        deps = []
        for r in reads:
            if r.w is not None:
                deps.append(r.w)
        for w in writes:
            if w.w is not None:
                deps.append(w.w)
            deps.extend(w.r.items())
        return deps

    def _record(self, tok, reads, writes):
        sem, val = tok
        for r in reads:
            if r.r.get(sem, 0) < val:
                r.r[sem] = val
        for w in writes:
            w.w = tok
            w.r = {}

    def op(self, E, reads, writes, fn, inc=True):
        self._wait(E, self._deps(reads, writes))
        ins = fn()
        if inc:
            E.sem.cnt += 1
            ins.then_inc(E.sem.h, 1)
            tok = (E.sem, E.sem.cnt)
        else:
            tok = (E.sem, E.sem.cnt + 1)
        self._record(tok, reads, writes)
        return tok

    def dma(self, Q, out, in_, reads, writes):
        sems = self.dsems[Q.name]
        s = sems[self.drr[Q.name] % len(sems)]
        self.drr[Q.name] += 1
        deps = self._deps(reads, writes)
        if s.cnt > 0:
            deps.append((s, s.cnt))
        self._wait(Q, deps)
        Q.e.dma_start(out=out, in_=in_).then_inc(s.h, 16)
        s.cnt += 16
        tok = (s, s.cnt)
        self._record(tok, reads, writes)
        return tok

    def barrier(self):
        for E in self.engs:
            for s in self.allsems:
                if s is E.sem or s.cnt == 0:
                    continue
                if E.seen.get(s, 0) < s.cnt:
                    E.e.wait_ge(s.h, s.cnt)
                    E.seen[s] = s.cnt


def t5_bucket_np(n):
    n = np.maximum(n, 0)
    nf = np.maximum(n, 1).astype(np.float32)
    large = 16 + (np.log(nf / np.float32(16)) / np.float32(math.log(128 / 16)) * np.float32(16)).astype(np.int32)
    large = np.minimum(large, 31)
    return np.where(n < 16, n, large)


def host_consts(S):
    c = {}
    i = np.arange(128)
    c["c_ident"] = np.eye(128, dtype=np.float32)
    c["c_ident4"] = np.tile(np.eye(128, dtype=np.float32), (1, 4))
    c["c_uinclneg"] = -(i[:, None] >= i[None, :]).astype(np.float32)
    c["c_onesneg"] = -np.ones((128, 128), np.float32)
    c["c_cmstrict"] = np.where(i[:, None] >= i[None, :], NEG, 0.0).astype(np.float32)
    c["c_cmincl"] = np.where(i[:, None] > i[None, :], NEG, 0.0).astype(np.float32)
    c["c_adm"] = np.where(i[None, :] <= i[:, None], 0.0, -1e30).astype(np.float32)
    dd = np.arange(256)[None, :] - i[:, None]
    c["c_buck"] = np.where(dd >= 0, t5_bucket_np(dd), 99).astype(np.float32)
    c["c_tmask"] = np.where(dd >= 0, 0.0, NEG).astype(np.float32)
    c["c_blk"] = (np.arange(S)[None, :] // 256 == np.arange(8)[:, None]).astype(np.float32)
    c["c_ones"] = np.ones((8, S), np.float32)
    c["c_pow"] = np.tile((0.5 ** (np.arange(NIT) + 1)).astype(np.float32)[None, :], (128, 1))
    return c


def build(S, DEPTH, NSEQ, dbg=False):
    NT = S // 128
    NCH = S // 512
    NB = S // 256
    TOPK = min(3, NB)
    KSEL = min(256, S // 4)
    TOKH = min(S, 1024)
    nc = bass.Bass("TRN2", target_bir_lowering=False)
    es = ExitStack()
    with es:
        def din(name, shape):
            return nc.dram_tensor(name, list(shape), F32, kind="ExternalInput").ap()
        x_d = din("x", [NSEQ, S, D])
        y_d = nc.dram_tensor("y", [NSEQ, S, D], F32, kind="ExternalOutput").ap()
        gmix_d = din("norm_mix_g", [DEPTH, D])
        gffn_d = din("norm_ffn_g", [DEPTH, D])
        gfin_d = din("norm_final_g", [1, D])
        win_d = din("w_in", [DEPTH, D, PIN])
        fxb_d = din("fox_b_f", [DEPTH, 4])
        t5_d = din("t5_bias", [1, 256])
        wg_d = din("w_gate", [DEPTH, 4, D, D])
        wb_d = din("w_branch", [DEPTH, 4, 256, D])
        wo_d = din("w_out", [DEPTH, D, D])
        wup_d = din("w_up", [DEPTH, D, 2 * DFF])
        cw_d = din("conv_w", [DEPTH, 3, 2 * DFF])
        cb_d = din("conv_b", [DEPTH, 2 * DFF])
        wdn_d = din("w_down", [DEPTH, DFF, D])
        hc = host_consts(S)
        cst = {k: din(k, v.shape) for k, v in hc.items()}
        dbg_d = nc.dram_tensor("dbg", [6, S, D], F32, kind="ExternalOutput").ap() if dbg else None

        K = KB(nc, es)
        pe, act, dve, pool, sp = K.pe, K.act, K.dve, K.pool, K.sp

        uniq = [0]

        def sb(name, shape, dt, st=None):
            uniq[0] += 1
            return (st or es).enter_context(nc.sbuf_tensor("%s_%d" % (name, uniq[0]), list(shape), dt))

        PS = [es.enter_context(nc.psum_tensor("ps%d" % i, [128, 512], F32)) for i in range(7)]
        PSR = [Reg("ps%d" % i) for i in range(7)]
        PSB = es.enter_context(nc.psum_tensor("psb", [128, 1024], BF16))
        PSBR = Reg("psb")

        ident_f = sb("ident_f", [128, 128], F32)
        ident_b = sb("ident_b", [128, 128], BF16)
        ident4 = sb("ident4", [128, 512], BF16)
        uinclneg = sb("uinclneg", [128, 128], BF16)
        onesneg = sb("onesneg", [128, 128], BF16)
        cmstrict = sb("cmstrict", [128, 128], BF16)
        cmincl = sb("cmincl", [128, 128], BF16)
        adm = sb("adm", [128, 128], F32)
        buck = sb("buck", [128, 256], F32)
        tmask = sb("tmask", [128, 256], F32)
        powt = sb("powt", [128, NIT], F32)
        t5bc = sb("t5bc", [128, 256], F32)
        tbias = sb("tbias", [128, 8, 256], BF16)
        CR = Reg("const")
        for (t, k, q) in [(ident_f, "c_ident", sp), (ident_b, "c_ident", pool), (ident4, "c_ident4", pool),
                          (uinclneg, "c_uinclneg", pool), (onesneg, "c_onesneg", pool), (cmstrict, "c_cmstrict", pool),
                          (cmincl, "c_cmincl", pool), (adm, "c_adm", sp), (buck, "c_buck", sp), (tmask, "c_tmask", sp),
                          (powt, "c_pow", sp)]:
            K.dma(q, t[:], cst[k], [], [CR])
        K.dma(sp, t5bc[:], bass.AP(tensor=t5_d.tensor, offset=0, ap=[[0, 128], [1, 256]]), [], [CR])
        with ExitStack() as st:
            tacc = sb("tacc", [128, 256], F32, st)
            ttmp = sb("ttmp", [128, 256], F32, st)
            TR = Reg("tb")
            for h in range(8):
                for b in range(32):
                    col = t5bc[:, b * 8 + h:b * 8 + h + 1]
                    if b == 0:
                        K.op(dve, [CR], [TR], lambda: nc.vector.tensor_scalar(out=tacc[:], in0=buck[:], scalar1=float(b), scalar2=col, op0=ALU.is_equal, op1=ALU.mult))
                    else:
                        K.op(dve, [CR], [TR], lambda: nc.vector.tensor_scalar(out=ttmp[:], in0=buck[:], scalar1=float(b), scalar2=col, op0=ALU.is_equal, op1=ALU.mult))
                        K.op(dve, [TR], [TR], lambda: nc.vector.tensor_tensor(out=tacc[:], in0=tacc[:], in1=ttmp[:], op=ALU.add))
                c31 = t5bc[:, 31 * 8 + h:31 * 8 + h + 1]
                K.op(dve, [TR, CR], [CR], lambda: nc.vector.scalar_tensor_tensor(out=tbias[:, h, :], in0=tacc[:], scalar=c31, in1=tmask[:], op0=ALU.subtract, op1=ALU.add))
            K.barrier()

        xnT = sb("xnT", [128, 8, S], BF16)
        XNR = [Reg("xnT%d" % t) for t in range(NT)]
        gbc = sb("gbc", [128, D], F32)
        GBR = Reg("gbc")

        def load_g(src_row_ap):
            K.dma(sp, gbc[:], bass.AP(tensor=src_row_ap.tensor, offset=src_row_ap.offset, ap=[[0, 128], [1, D]]), [], [GBR])

        YR = [[Reg("y%d_%d" % (s, t)) for t in range(NT)] for s in range(NSEQ)]

        class NormBufs:
            pass

        def norm_tile(NBF, seq, t, src_ap, delta_banks, final, slot):
            xt = NBF.xt[slot]
            XTR = NBF.xtr[slot]
            K.dma(sp, xt[:], src_ap, [YR[seq][t]], [XTR])
            if delta_banks is not None:
                for hb, (bk, bkr) in enumerate(delta_banks):
                    K.op(dve, [XTR, bkr], [XTR], lambda: nc.vector.tensor_tensor(out=xt[:, hb * 512:(hb + 1) * 512], in0=xt[:, hb * 512:(hb + 1) * 512], in1=bk[:, :], op=ALU.add))
                K.dma(sp, y_d[seq, t * 128:(t + 1) * 128, :], xt[:], [XTR], [YR[seq][t]])
            K.op(act, [XTR], [NBF.sqr[slot]], lambda: nc.scalar.activation(out=NBF.sq[slot][:], in_=xt[:], func=AF.Square, accum_out=NBF.ss[slot][:]))
            K.op(act, [NBF.sqr[slot]], [NBF.sqr[slot]], lambda: nc.scalar.activation(out=NBF.rs[slot][:], in_=NBF.ss[slot][:], func=AF.Sqrt, scale=1.0 / D, bias=NBF.eps[:]))
            K.op(dve, [NBF.sqr[slot]], [NBF.sqr[slot]], lambda: nc.vector.reciprocal(out=NBF.rs[slot][:], in_=NBF.rs[slot][:]))
            if final:
                K.op(dve, [XTR, NBF.sqr[slot], GBR], [XTR], lambda: nc.vector.scalar_tensor_tensor(out=xt[:], in0=xt[:], scalar=NBF.rs[slot][:], in1=gbc[:], op0=ALU.mult, op1=ALU.mult))
                K.dma(sp, y_d[seq, t * 128:(t + 1) * 128, :], xt[:], [XTR], [YR[seq][t]])
                return
            xb = NBF.xb[slot]
            XBR = NBF.xbr[slot]
            K.op(dve, [XTR, NBF.sqr[slot], GBR], [XBR], lambda: nc.vector.scalar_tensor_tensor(out=xb[:], in0=xt[:], scalar=NBF.rs[slot][:], in1=gbc[:], op0=ALU.mult, op1=ALU.mult))
            for c in range(8):
                K.op(pe, [XBR, CR], [PSBR], lambda: nc.tensor.transpose(PSB[:, c * 128:(c + 1) * 128], xb[:, c * 128:(c + 1) * 128], ident_b[:]), inc=(c == 7))
            K.op(act, [PSBR], [XNR[t]], lambda: nc.scalar.copy(out=xnT[:, :, t * 128:(t + 1) * 128], in_=PSB[:, :].rearrange("p (c n) -> p c n", n=128)))

        def alloc_normbufs(st):
            NBF = NormBufs()
            NBF.xt = [sb("nb_xt%d" % i, [128, D], F32, st) for i in range(2)]
            NBF.xtr = [Reg() for i in range(2)]
            NBF.sq = [sb("nb_sq%d" % i, [128, D], BF16, st) for i in range(2)]
            NBF.ss = [sb("nb_ss%d" % i, [128, 1], F32, st) for i in range(2)]
            NBF.rs = [sb("nb_rs%d" % i, [128, 1], F32, st) for i in range(2)]
            NBF.sqr = [Reg() for i in range(2)]
            NBF.xb = [sb("nb_xb%d" % i, [128, D], BF16, st) for i in range(2)]
            NBF.xbr = [Reg() for i in range(2)]
            NBF.eps = sb("nb_eps", [128, 1], F32, st)
            K.op(dve, [], [NBF.sqr[0], NBF.sqr[1]], lambda: nc.vector.memset(NBF.eps[:], 1e-6))
            return NBF

        bank_rr = [0]

        def next_bank(lo=0, hi=7):
            b = lo + bank_rr[0] % (hi - lo)
            bank_rr[0] += 1
            return b

        def proj_fm(W, WR, wcol, M, dst_fn, dst_reg, scale=None, evac_eng=None):
            for n in range(NCH):
                b = next_bank()
                for c in range(8):
                    K.op(pe, [WR] + XNR[n * 4:(n + 1) * 4], [PSR[b]],
                         lambda: nc.tensor.matmul(PS[b][0:M, :], lhsT=W[:, c, wcol:wcol + M], rhs=xnT[:, c, n * 512:(n + 1) * 512], start=(c == 0), stop=(c == 7)), inc=(c == 7))
                dst = dst_fn(n)
                if scale is None:
                    K.op(dve, [PSR[b]], [dst_reg], lambda: nc.vector.tensor_copy(out=dst, in_=PS[b][0:M, :]))
                else:
                    K.op(dve, [PSR[b]], [dst_reg], lambda: nc.vector.tensor_scalar(out=dst, in0=PS[b][0:M, :], scalar1=float(scale), scalar2=None, op0=ALU.mult))

        def proj_tm(W, WR, wcol, ncol, evac_fn):
            for t in range(NT):
                b = next_bank()
                for c in range(8):
                    K.op(pe, [WR, XNR[t]], [PSR[b]],
                         lambda: nc.tensor.matmul(PS[b][:, 0:ncol], lhsT=xnT[:, c, t * 128:(t + 1) * 128], rhs=W[:, c, wcol:wcol + ncol], start=(c == 0), stop=(c == 7)), inc=(c == 7))
                evac_fn(t, PS[b], PSR[b])

        def load_win(l, W, WR, c0, c1, dcol=0):
            K.dma(pool, W[:, :, dcol:dcol + (c1 - c0)], win_d[l][:, c0:c1].rearrange("(c p) n -> p c n", p=128), [], [WR])

        for seq in range(NSEQ):
            with ExitStack() as st:
                NBF = alloc_normbufs(st)
                load_g(gmix_d[0])
                for t in range(NT):
                    slot = t % 2
                    xt = NBF.xt[slot]
                    K.dma(sp, xt[:], x_d[seq, t * 128:(t + 1) * 128, :], [], [NBF.xtr[slot]])
                    K.dma(sp, y_d[seq, t * 128:(t + 1) * 128, :], xt[:], [NBF.xtr[slot]], [YR[seq][t]])
                    norm_tile(NBF, seq, t, y_d[seq, t * 128:(t + 1) * 128, :], None, False, slot)
                K.barrier()

            for l in range(DEPTH):
                with ExitStack() as mst:
                    ast = ExitStack()
                    oT = sb("oT", [128, 4, 2, S], BF16, mst)
                    OTR = [Reg("oT%d" % m) for m in range(4)]
                    QT = [sb("QT%d" % h, [72, S], BF16, ast) for h in range(4)]
                    KT = [sb("KT%d" % h, [72, S], BF16, ast) for h in range(4)]
                    QTR = [Reg("QT%d" % h) for h in range(4)]
                    QAR = [Reg("QA%d" % h) for h in range(4)]
                    KTR = [Reg("KT%d" % h) for h in range(4)]
                    V = sb("V", [128, NT, 4, 65], BF16, ast)
                    VR = Reg("V")
                    OTOK = sb("OTOK", [128, NT, 256], BF16, ast)
                    OKR = Reg("OTOK")
                    W = [sb("Wm%d" % i, [128, 8, 776], BF16, ast) for i in range(2)]
                    WR = [Reg("Wm%d" % i) for i in range(2)]
                    PT = [sb("PT%d" % i, [128, 512], BF16, ast) for i in range(2)]
                    PTR = [Reg("PT%d" % i) for i in range(2)]
                    orec = sb("orec", [128, 4], F32, ast)
                    ORR = Reg("orec")
                    K.op(dve, [], [VR], lambda: nc.vector.memset(V[:], 1.0))

                    def v_evac(nheads):
                        def f(t, ps, psr):
                            K.op(dve, [psr], [VR], lambda: nc.vector.tensor_copy(out=V[:, t, 0:nheads, 0:64], in_=ps[:, 0:nheads * 64].rearrange("p (h d) -> p h d", d=64)))
                        return f

                    def qk_proj(Wt, WRt, base, with_scale=True):
                        for h in range(4):
                            proj_fm(Wt, WRt, base + h * 64, 64, (lambda n, h=h: QT[h][0:64, n * 512:(n + 1) * 512]), QTR[h], scale=0.125)
                            proj_fm(Wt, WRt, base + 256 + h * 64, 64, (lambda n, h=h: KT[h][0:64, n * 512:(n + 1) * 512]), KTR[h])

                    def finish_o(ob, obr, tq0, nsub, h, normalize):
                        o3 = PS[ob][:, 0:nsub * 65].rearrange("p (s d) -> p s d", d=65)
                        dst = OTOK[:, tq0:tq0 + nsub, h * 64:(h + 1) * 64]
                        if normalize:
                            K.op(dve, [obr], [ORR], lambda: nc.vector.reciprocal(out=orec[:, 0:nsub], in_=o3[:, :, 64]))
                            for s_ in range(nsub):
                                K.op(dve, [obr, ORR], [OKR], lambda: nc.vector.tensor_scalar(out=OTOK[:, tq0 + s_, h * 64:(h + 1) * 64], in0=o3[:, s_, 0:64], scalar1=orec[:, s_:s_ + 1], scalar2=None, op0=ALU.mult))
                        else:
                            K.op(dve, [obr], [OKR], lambda: nc.vector.tensor_copy(out=dst, in_=o3[:, :, 0:64]))

                    def otok_to_oT(m):
                        if dbg and seq == 0 and l == 0:
                            K.dma(pool, dbg_d[m, :, 0:256].rearrange("(t p) c -> p t c", p=128), OTOK[:], [OKR], [])
                        for t in range(NT):
                            for k in range(2):
                                K.op(pe, [OKR, CR], [PSBR], lambda: nc.tensor.transpose(PSB[:, k * 128:(k + 1) * 128], OTOK[:, t, k * 128:(k + 1) * 128], ident_b[:]), inc=(k == 1))
                            K.op(act, [PSBR], [OTR[m]], lambda: nc.scalar.copy(out=oT[:, m, :, t * 128:(t + 1) * 128], in_=PSB[:, 0:256].rearrange("p (c n) -> p c n", n=128)))

                    def attn_softmax(h, kdim, near_bias, diag_mask):
                        for cq in range(NCH):
                            ob = 6
                            K.op(dve, [], [PSR[ob]], lambda: nc.vector.memset(PS[ob][:, 0:260], 0.0))
                            t0 = cq * 512
                            na = 4 * cq + 4

                            def stA(a):
                                r = a - 4 * cq
                                off = max(0, r) * 128
                                xb = a % 2
                                extra = []
                                if near_bias is not None:
                                    if r >= 0:
                                        w_ = min(256, 512 - off)
                                        extra.append((ident_b, tbias[:, near_bias, 0:w_], off, w_))
                                    elif r == -1:
                                        extra.append((ident_b, tbias[:, near_bias, 128:256], 0, 128))
                                elif diag_mask is not None and r >= 0:
                                    extra.append((ident_b, diag_mask[:, :], off, 128))
                                K.op(pe, [KTR[h], QTR[h], QAR[h]], [PSR[xb]], lambda: nc.tensor.matmul(PS[xb][:, off:512], lhsT=KT[h][0:kdim, a * 128:(a + 1) * 128], rhs=QT[h][0:kdim, t0 + off:t0 + 512], start=True, stop=(len(extra) == 0), skip_group_check=True), inc=(len(extra) == 0))
                                for ei, (lt, rh, eo, ew) in enumerate(extra):
                                    K.op(pe, [CR], [PSR[xb]], lambda: nc.tensor.matmul(PS[xb][:, eo:eo + ew], lhsT=lt[:, :], rhs=rh, start=False, stop=(ei == len(extra) - 1), skip_group_check=True), inc=(ei == len(extra) - 1))

                            def stB(a):
                                r = a - 4 * cq
                                off = max(0, r) * 128
                                xb = a % 2
                                pt, ptr = PT[a % 2], PTR[a % 2]
                                K.op(act, [PSR[xb]], [ptr], lambda: nc.scalar.activation(out=pt[:, off:512], in_=PS[xb][:, off:512], func=AF.Exp))
                                for s_ in range(max(0, r), 4):
                                    K.op(pe, [ptr, VR], [PSR[ob]], lambda: nc.tensor.matmul(PS[ob][:, s_ * 65:(s_ + 1) * 65], lhsT=pt[:, s_ * 128:(s_ + 1) * 128], rhs=V[:, a, h, :], start=False, stop=False, skip_group_check=True), inc=(s_ == 3))
                            stA(0)
                            for a in range(na):
                                if a + 1 < na:
                                    stA(a + 1)
                                stB(a)
                            finish_o(ob, PSR[ob], cq * 4, 4, h, True)

                    load_win(l, W[0], WR[0], C_SB, C_SB + 768)
                    load_win(l, W[1], WR[1], C_MB, C_MB + 768)
                    proj_tm(W[0], WR[0], 512, 256, v_evac(4))
                    with ExitStack() as st:
                        QS = sb("QS", [128, 2, S], BF16, st)
                        KS = sb("KS", [128, 2, S], BF16, st)
                        QSR, KSR = Reg(), Reg()
                        for k2 in range(2):
                            proj_fm(W[0], WR[0], k2 * 128, 128, (lambda n, k2=k2: QS[:, k2, n * 512:(n + 1) * 512]), QSR, scale=0.125)
                            proj_fm(W[0], WR[0], 256 + k2 * 128, 128, (lambda n, k2=k2: KS[:, k2, n * 512:(n + 1) * 512]), KSR)
                        E1 = sb("E1", [128, 512], F32, st)
                        SPb = [sb("SPb%d" % i, [128, 512], BF16, st) for i in range(2)]
                        SACC = sb("SACC", [128, 512], F32, st)
                        SACCB = sb("SACCB", [128, 512], BF16, st)
                        E1R, SAR, SBR = Reg(), Reg(), Reg()
                        SPR = [Reg(), Reg()]
                        for h in range(4):
                            for cq in range(NCH):
                                ob = 6
                                K.op(dve, [], [PSR[ob]], lambda: nc.vector.memset(PS[ob][:, 0:260], 0.0))
                                K.op(dve, [], [SAR], lambda: nc.vector.memset(SACC[:], 0.0))
                                t0 = cq * 512
                                atop = 4 * cq + 3

                                def offs(a):
                                    return max(0, a - 4 * cq) * 128

                                def stA(a):
                                    r = a - 4 * cq
                                    off = offs(a)
                                    xb = a % 3
                                    spb, spr = SPb[a % 2], SPR[a % 2]
                                    K.op(pe, [KSR, QSR], [PSR[xb]], lambda: nc.tensor.matmul(PS[xb][:, off:512], lhsT=KS[(h % 2) * 64:(h % 2) * 64 + 64, h // 2, a * 128:(a + 1) * 128], rhs=QS[(h % 2) * 64:(h % 2) * 64 + 64, h // 2, t0 + off:t0 + 512], start=True, stop=False, skip_group_check=True), inc=(r < 0))
                                    if r >= 0:
                                        K.op(pe, [CR], [PSR[xb]], lambda: nc.tensor.matmul(PS[xb][:, off:off + 128], lhsT=ident_b[:, :], rhs=cmstrict[:, :], start=False, stop=False, skip_group_check=True))
                                    K.op(act, [PSR[xb]], [E1R], lambda: nc.scalar.activation(out=E1[:, off:512], in_=PS[xb][:, off:512], func=AF.Exp))
                                    K.op(act, [E1R], [spr], lambda: nc.scalar.activation(out=spb[:, off:512], in_=E1[:, off:512], func=AF.Ln, bias=1.0))

                                def stB1(a):
                                    off = offs(a)
                                    xb = a % 3
                                    spb, spr = SPb[a % 2], SPR[a % 2]
                                    first = (a == atop)
                                    prev_off = 512 if first else offs(a + 1)
                                    K.op(pe, [spr, CR], [PSR[xb]], lambda: nc.tensor.matmul(PS[xb][:, off:512], lhsT=uinclneg[:, :], rhs=spb[:, off:512], start=False, stop=first, skip_group_check=True), inc=first)
                                    if not first:
                                        K.op(pe, [SBR, CR], [PSR[xb]], lambda: nc.tensor.matmul(PS[xb][:, prev_off:512], lhsT=onesneg[:, :], rhs=SACCB[:, prev_off:512], start=False, stop=True, skip_group_check=True))
                                    if a > 0:
                                        K.op(dve, [spr, SAR], [SAR], lambda: nc.vector.tensor_tensor(out=SACC[:, off:512], in0=SACC[:, off:512], in1=spb[:, off:512], op=ALU.add))
                                        K.op(dve, [SAR], [SBR], lambda: nc.vector.tensor_copy(out=SACCB[:, off:512], in_=SACC[:, off:512]))

                                def stB2(a):
                                    r = a - 4 * cq
                                    off = offs(a)
                                    xb = a % 3
                                    pt, ptr = PT[a % 2], PTR[a % 2]
                                    K.op(act, [PSR[xb]], [ptr], lambda: nc.scalar.activation(out=pt[:, off:512], in_=PS[xb][:, off:512], func=AF.Exp))
                                    for s_ in range(max(0, r), 4):
                                        K.op(pe, [ptr, VR], [PSR[ob]], lambda: nc.tensor.matmul(PS[ob][:, s_ * 65:s_ * 65 + 64], lhsT=pt[:, s_ * 128:(s_ + 1) * 128], rhs=V[:, a, h, 0:64], start=False, stop=False, skip_group_check=True), inc=(s_ == 3))
                                T_ = list(range(atop, -1, -1))
                                for k_ in range(len(T_) + 2):
                                    if k_ < len(T_):
                                        stA(T_[k_])
                                    if 0 <= k_ - 1 < len(T_):
                                        stB1(T_[k_ - 1])
                                    if 0 <= k_ - 2 < len(T_):
                                        stB2(T_[k_ - 2])
                                finish_o(ob, PSR[ob], cq * 4, 4, h, False)
                        otok_to_oT(0)
                        K.barrier()

                    load_win(l, W[0], WR[0], C_FX, C_FX + 772)
                    qk_proj(W[1], WR[1], 0)
                    proj_tm(W[1], WR[1], 512, 256, v_evac(4))
                    with ExitStack() as st:
                        kmean = sb("kmean", [64, 4, 8], F32, st)
                        kmb = sb("kmb", [64, 4, 8], BF16, st)
                        KMR = Reg()
                        NSET = 2
                        G8s = [sb("G8_%d" % i_, [128, 8], F32, st) for i_ in range(NSET)]
                        M8s = [sb("M8_%d" % i_, [128, 8], F32, st) for i_ in range(NSET)]
                        thrs = [sb("thr_%d" % i_, [128, 1], F32, st) for i_ in range(NSET)]
                        SELTs = [sb("SELT_%d" % i_, [128, 72], F32, st) for i_ in range(NSET)]
                        GRs = [Reg() for i_ in range(NSET)]
                        SELRs = [Reg() for i_ in range(NSET)]
                        PSBRs = [Reg() for i_ in range(NSET)]
                        for i_ in range(NSET):
                            K.op(dve, [], [SELRs[i_]], lambda: nc.vector.memset(SELTs[i_][:], 0.0))
                        for h in range(4):
                            K.dma(pool, KT[h][64:72, :], cst["c_blk"], [], [KTR[h]])
                            K.op(dve, [KTR[h]], [KMR], lambda: nc.vector.tensor_reduce(out=kmean[:, h, 0:NB], in_=KT[h][0:64, :].rearrange("p (n k) -> p n k", k=256), axis=AX.X, op=ALU.add))
                        K.op(dve, [KMR], [KMR], lambda: nc.vector.tensor_scalar(out=kmb[:], in0=kmean[:], scalar1=1.0 / 256, scalar2=None, op0=ALU.mult))
                        chain = 0
                        for h in range(4):
                            for t in range(NT):
                                own = t // 2
                                k_ = chain % NSET
                                chain += 1
                                G8, M8, thr, SELT, GR, SELR = G8s[k_], M8s[k_], thrs[k_], SELTs[k_], GRs[k_], SELRs[k_]
                                K.op(dve, [], [SELR], lambda: nc.vector.memset(SELT[:, 64:72], 0.0))
                                if own > 0:
                                    b = 2 + k_
                                    K.op(pe, [QTR[h], KMR], [PSR[b]], lambda: nc.tensor.matmul(PS[b][:, 0:8], lhsT=QT[h][0:64, t * 128:(t + 1) * 128], rhs=kmb[:, h, :], start=True, stop=True))
                                    K.op(dve, [], [GR], lambda: nc.vector.memset(G8[:], -1e30))
                                    K.op(dve, [PSR[b]], [GR], lambda: nc.vector.tensor_copy(out=G8[:, 0:own], in_=PS[b][:, 0:own]))
                                    K.op(dve, [GR], [GR], lambda: nc.vector.max(out=M8[:], in_=G8[:]))
                                    K.op(dve, [GR], [GR], lambda: nc.vector.tensor_scalar(out=thr[:], in0=M8[:, TOPK - 1:TOPK], scalar1=-1e29, scalar2=None, op0=ALU.max))
                                    K.op(dve, [GR], [SELR], lambda: nc.vector.tensor_scalar(out=SELT[:, 64:64 + own], in0=G8[:, 0:own], scalar1=thr[:], scalar2=NEG, op0=ALU.is_lt, op1=ALU.mult))
                                tb_ = 4 + k_
                                K.op(pe, [SELR, CR], [PSR[tb_]], lambda: nc.tensor.transpose(PS[tb_][0:72, 0:128], SELT[:, :], ident_f[:]))
                                K.op(act, [PSR[tb_]], [QAR[h]], lambda: nc.scalar.copy(out=QT[h][64:72, t * 128:(t + 1) * 128], in_=PS[tb_][64:72, 0:128]))
                        K.barrier()
                        for h in range(4):
                            attn_softmax(h, 72, h, None)
                        otok_to_oT(1)
                        K.barrier()

                    load_win(l, W[1], WR[1], C_DQ, C_DQ + 680)
                    load_win(l, W[1], WR[1], C_IK, C_IK + 32, dcol=680)
                    load_win(l, W[1], WR[1], C_IK, C_IK + 32, dcol=712)
                    qk_proj(W[0], WR[0], 0)
                    proj_tm(W[0], WR[0], 512, 256, v_evac(4))
                    with ExitStack() as st:
                        fb = sb("fb", [4, 1], F32, st)
                        ef = sb("ef", [4, S], F32, st)
                        spf = sb("spf", [4, S], F32, st)
                        ones4 = sb("ones4", [4, S], F32, st)
                        cc = sb("cc", [4, S], F32, st)
                        chi = sb("chi", [4, S], BF16, st)
                        clo = sb("clo", [4, S], BF16, st)
                        nchi = sb("nchi", [4, S], BF16, st)
                        nclo = sb("nclo", [4, S], BF16, st)
                        FR = Reg()
                        K.dma(sp, fb[:], fxb_d[l].rearrange("(p o) -> p o", o=1), [], [FR])
                        K.op(dve, [FR], [FR], lambda: nc.vector.tensor_scalar(out=fb[:], in0=fb[:], scalar1=-1.0, scalar2=None, op0=ALU.mult))
                        K.op(dve, [], [FR], lambda: nc.vector.memset(ones4[:], 1.0))
                        for n in range(NCH):
                            b = next_bank()
                            for c in range(8):
                                K.op(pe, [WR[0]] + XNR[n * 4:(n + 1) * 4], [PSR[b]], lambda: nc.tensor.matmul(PS[b][0:4, :], lhsT=W[0][:, c, 768:772], rhs=xnT[:, c, n * 512:(n + 1) * 512], start=(c == 0), stop=(c == 7)), inc=(c == 7))
                            K.op(act, [PSR[b], FR], [FR], lambda: nc.scalar.activation(out=ef[:, n * 512:(n + 1) * 512], in_=PS[b][0:4, :], func=AF.Exp, scale=-1.0, bias=fb[:]))
                        K.op(act, [FR], [FR], lambda: nc.scalar.activation(out=spf[:], in_=ef[:], func=AF.Ln, bias=1.0))
                        K.op(dve, [FR], [FR], lambda: nc.vector.tensor_tensor_scan(out=cc[:], data0=ones4[:], data1=spf[:], initial=0.0, op0=ALU.mult, op1=ALU.subtract))
                        K.op(dve, [FR], [FR], lambda: nc.vector.tensor_copy(out=chi[:], in_=cc[:]))
                        K.op(dve, [FR], [FR], lambda: nc.vector.tensor_tensor(out=clo[:], in0=cc[:], in1=chi[:], op=ALU.subtract))
                        K.op(dve, [FR], [FR], lambda: nc.vector.tensor_scalar(out=nchi[:], in0=chi[:], scalar1=-1.0, scalar2=None, op0=ALU.mult))
                        K.op(dve, [FR], [FR], lambda: nc.vector.tensor_scalar(out=nclo[:], in0=clo[:], scalar1=-1.0, scalar2=None, op0=ALU.mult))
                        for h in range(4):
                            K.dma(sp, QT[h][64:65, :], chi[h:h + 1, :], [FR], [QTR[h]])
                            K.dma(sp, QT[h][65:66, :], clo[h:h + 1, :], [FR], [QTR[h]])
                            K.dma(pool, QT[h][66:68, :], cst["c_ones"][0:2, :], [], [QTR[h]])
                            K.dma(pool, KT[h][64:66, :], cst["c_ones"][0:2, :], [], [KTR[h]])
                            K.dma(sp, KT[h][66:67, :], nchi[h:h + 1, :], [FR], [KTR[h]])
                            K.dma(sp, KT[h][67:68, :], nclo[h:h + 1, :], [FR], [KTR[h]])
                        for h in range(4):
                            attn_softmax(h, 68, None, cmincl)
                        otok_to_oT(2)
                        K.barrier()

                    for h in range(4):
                        proj_fm(W[1], WR[1], h * 64, 64, (lambda n, h=h: QT[h][0:64, n * 512:(n + 1) * 512]), QTR[h], scale=0.125)
                    proj_fm(W[1], WR[1], 256, 64, (lambda n: KT[0][0:64, n * 512:(n + 1) * 512]), KTR[0])
                    proj_tm(W[1], WR[1], 320, 64, v_evac(1))
                    with ExitStack() as st:
                        QI = [sb("QI%d" % i, [64, S], BF16, st) for i in range(4)]
                        KI = sb("KI", [64, S], BF16, st)
                        QIR, KIR = Reg(), Reg()
                        WI = sb("WI", [128, NT, 8], F32, st)
                        WA = sb("WA", [128, NT, 8], F32, st)
                        WS = sb("WS", [128, NT, 8], F32, st)
                        WIR = Reg()
                        ISC = sb("ISC", [128, S], F32, st)
                        ISR = Reg()
                        RT = [sb("RT%d" % i, [128, 512], F32, st) for i in range(2)]
                        RTR = [Reg(), Reg()]
                        MNB = [sb("MNEG%d" % i_, [128, S], BF16, st) for i_ in range(2)]
                        MNRB = [Reg(), Reg()]
                        MZ = sb("MZ", [128, 128], BF16, st)
                        MZR = Reg()
                        lo = sb("lo", [128, 1], F32, st)
                        hi_ = sb("hi_", [128, 1], F32, st)
                        hw = sb("hw", [128, NIT], F32, st)
                        mid = sb("mid", [128, 1], F32, st)
                        cnt = sb("cnt", [128, 1], F32, st)
                        tmp1 = sb("tmp1", [128, 1], F32, st)
                        BR = Reg()
                        for i in range(4):
                            proj_fm(W[1], WR[1], 384 + i * 64, 64, (lambda n, i=i: QI[i][0:64, n * 512:(n + 1) * 512]), QIR)
                        proj_fm(W[1], WR[1], 680, 64, (lambda n: KI[0:64, n * 512:(n + 1) * 512]), KIR)

                        def wi_evac(t, ps, psr):
                            K.op(dve, [psr], [WIR], lambda: nc.vector.tensor_copy(out=WI[:, t, :], in_=ps[:, 0:8]))
                        proj_tm(W[1], WR[1], 672, 8, wi_evac)
                        K.op(act, [WIR], [WIR], lambda: nc.scalar.activation(out=WA[:], in_=WI[:], func=AF.Abs))
                        K.op(act, [WIR], [WIR], lambda: nc.scalar.activation(out=WS[:], in_=WI[:], func=AF.Sign))
                        K.op(dve, [], [MZR], lambda: nc.vector.memset(MZ[:], 0.0))
                        def sel(i):
                            t1 = (i + 1) * 128
                            MNEG, MNR = MNB[i % 2], MNRB[i % 2]
                            for kc in range((t1 + 511) // 512):
                                k0 = kc * 512
                                kw = min(512, t1 - k0)
                                for hi in range(8):
                                    b = next_bank(2, 6)
                                    rt, rtr = RT[hi % 2], RTR[hi % 2]
                                    K.op(pe, [QIR, KIR], [PSR[b]], lambda: nc.tensor.matmul(PS[b][:, 0:kw], lhsT=QI[hi // 2][(hi % 2) * 32:(hi % 2) * 32 + 32, i * 128:(i + 1) * 128], rhs=KI[(hi % 2) * 32:(hi % 2) * 32 + 32, k0:k0 + kw], start=True, stop=True))
                                    K.op(act, [PSR[b], WIR], [rtr], lambda: nc.scalar.activation(out=rt[:, 0:kw], in_=PS[b][:, 0:kw], func=AF.Relu, scale=WA[:, i, hi:hi + 1]))
                                    if hi == 0:
                                        K.op(dve, [rtr, WIR], [ISR], lambda: nc.vector.tensor_scalar(out=ISC[:, k0:k0 + kw], in0=rt[:, 0:kw], scalar1=WS[:, i, hi:hi + 1], scalar2=None, op0=ALU.mult))
                                    else:
                                        K.op(dve, [rtr, WIR, ISR], [ISR], lambda: nc.vector.scalar_tensor_tensor(out=ISC[:, k0:k0 + kw], in0=rt[:, 0:kw], scalar=WS[:, i, hi:hi + 1], in1=ISC[:, k0:k0 + kw], op0=ALU.mult, op1=ALU.add))
                            K.op(dve, [ISR], [BR], lambda: nc.vector.tensor_reduce(out=lo[:], in_=ISC[:, 0:t1], axis=AX.X, op=ALU.min))
                            K.op(dve, [ISR], [BR], lambda: nc.vector.tensor_reduce(out=hi_[:], in_=ISC[:, 0:t1], axis=AX.X, op=ALU.max))
                            K.op(dve, [ISR, CR], [ISR], lambda: nc.vector.tensor_tensor(out=ISC[:, i * 128:t1], in0=ISC[:, i * 128:t1], in1=adm[:, :], op=ALU.add))
                            K.op(dve, [BR], [BR], lambda: nc.vector.tensor_tensor(out=hi_[:], in0=hi_[:], in1=lo[:], op=ALU.subtract))
                            K.op(dve, [BR, CR], [BR], lambda: nc.vector.tensor_scalar(out=hw[:], in0=powt[:], scalar1=hi_[:], scalar2=None, op0=ALU.mult))
                            K.op(dve, [BR], [BR], lambda: nc.vector.tensor_tensor(out=mid[:], in0=lo[:], in1=hw[:, 0:1], op=ALU.add))
                            for it in range(NIT):
                                K.op(dve, [BR, ISR], [BR, MNR], lambda: nc.vector.tensor_scalar(out=MNEG[:, 0:t1], in0=ISC[:, 0:t1], scalar1=mid[:], scalar2=0.0, op0=ALU.is_ge, op1=ALU.add, accum_out=cnt[:]))
                                K.op(dve, [BR], [BR], lambda: nc.vector.tensor_scalar(out=tmp1[:], in0=cnt[:], scalar1=float(KSEL) - 0.5, scalar2=hw[:, it:it + 1], op0=ALU.is_ge, op1=ALU.mult))
                                nxt = it + 1 if it + 1 < NIT else it
                                K.op(dve, [BR], [BR], lambda: nc.vector.tensor_scalar(out=mid[:], in0=mid[:], scalar1=hw[:, nxt:nxt + 1], scalar2=tmp1[:], op0=ALU.subtract, op1=ALU.add))
                            K.op(dve, [BR], [BR], lambda: nc.vector.tensor_copy(out=lo[:], in_=mid[:]))
                            K.op(dve, [BR, ISR], [MNR], lambda: nc.vector.tensor_scalar(out=MNEG[:, 0:t1], in0=ISC[:, 0:t1], scalar1=lo[:], scalar2=NEG, op0=ALU.is_lt, op1=ALU.mult))
                        def attn(i):
                            t1 = (i + 1) * 128
                            need_sel = t1 > KSEL
                            MNEG, MNR = MNB[i % 2], MNRB[i % 2]
                            ob = 6
                            def dA(a):
                                xb = a % 2
                                K.op(pe, [MNR if need_sel else MZR, CR], [PSR[xb]], lambda: nc.tensor.matmul(PS[xb][:, :], lhsT=(MNEG[:, a * 128:(a + 1) * 128] if need_sel else MZ[:, :]), rhs=ident4[:, :], start=True, stop=False, skip_group_check=True), inc=False)
                                for h in range(4):
                                    lastmm = (h == 3) and (a < i - 1)
                                    K.op(pe, [KTR[0], QTR[h]], [PSR[xb]], lambda: nc.tensor.matmul(PS[xb][:, h * 128:(h + 1) * 128], lhsT=KT[0][0:64, a * 128:(a + 1) * 128], rhs=QT[h][0:64, i * 128:(i + 1) * 128], start=False, stop=lastmm, skip_group_check=True), inc=lastmm)
                                if a >= i - 1:
                                    for h in range(4):
                                        tb = tbias[:, 4 + h, 0:128] if a == i else tbias[:, 4 + h, 128:256]
                                        K.op(pe, [CR], [PSR[xb]], lambda: nc.tensor.matmul(PS[xb][:, h * 128:(h + 1) * 128], lhsT=ident_b[:, :], rhs=tb, start=False, stop=(h == 3), skip_group_check=True), inc=(h == 3))
                            def dB(a):
                                xb = a % 2
                                pt, ptr = PT[a % 2], PTR[a % 2]
                                K.op(act, [PSR[xb]], [ptr], lambda: nc.scalar.activation(out=pt[:, :], in_=PS[xb][:, :], func=AF.Exp))
                                for h in range(4):
                                    K.op(pe, [ptr, VR], [PSR[ob]], lambda: nc.tensor.matmul(PS[ob][:, h * 65:(h + 1) * 65], lhsT=pt[:, h * 128:(h + 1) * 128], rhs=V[:, a, 0, :], start=False, stop=False, skip_group_check=True), inc=(h == 3))
                            dA(0)
                            for a in range(i + 1):
                                if a + 1 <= i:
                                    dA(a + 1)
                                dB(a)
                            o3 = PS[ob][:, 0:260].rearrange("p (s d) -> p s d", d=65)
                            K.op(dve, [PSR[ob]], [ORR], lambda: nc.vector.reciprocal(out=orec[:, 0:4], in_=o3[:, :, 64]))
                            for h in range(4):
                                K.op(dve, [PSR[ob], ORR], [OKR], lambda: nc.vector.tensor_scalar(out=OTOK[:, i, h * 64:(h + 1) * 64], in0=o3[:, h, 0:64], scalar1=orec[:, h:h + 1], scalar2=None, op0=ALU.mult))
                        for i in range(NT):
                            K.op(dve, [], [PSR[6]], lambda: nc.vector.memset(PS[6][:, 0:260], 0.0))
                            if (i + 2) * 128 > KSEL and i + 1 < NT:
                                sel(i + 1)
                            attn(i)
                        otok_to_oT(3)
                        K.barrier()

                    ast.close()
                    with ExitStack() as st:
                        mixT = sb("mixT", [128, 8, S], BF16, st)
                        MXR = Reg()
                        WG = [sb("WG%d" % i, [128, 4, 8, 128], BF16, st) for i in range(2)]
                        WB = [sb("WB%d" % i, [128, 4, 2, 128], BF16, st) for i in range(2)]
                        WGR = [Reg(), Reg()]
                        SG = [sb("SG%d" % i, [128, 512], F32, st) for i in range(2)]
                        SGR = [Reg(), Reg()]
                        ACC = sb("ACCg", [128, 512], F32, st)
                        TMP = sb("TMPg", [128, 512], F32, st)
                        ACR = Reg()

                        def load_gw(f):
                            s_ = f % 2
                            for i in range(4):
                                K.dma(pool, WG[s_][:, i, :, :], wg_d[l, i][:, f * 128:(f + 1) * 128].rearrange("(c p) n -> p c n", p=128), [], [WGR[s_]])
                                K.dma(pool, WB[s_][:, i, :, :], wb_d[l, i][:, f * 128:(f + 1) * 128].rearrange("(c p) n -> p c n", p=128), [], [WGR[s_]])
                        load_gw(0)
                        for f in range(8):
                            if f + 1 < 8:
                                load_gw(f + 1)
                            s_ = f % 2
                            for n in range(NCH):
                                for i in range(4):
                                    gb = next_bank(0, 3)
                                    bb = 3 + next_bank(0, 3)
                                    for c in range(8):
                                        K.op(pe, [WGR[s_]] + XNR[n * 4:(n + 1) * 4], [PSR[gb]], lambda: nc.tensor.matmul(PS[gb][:, :], lhsT=WG[s_][:, i, c, :], rhs=xnT[:, c, n * 512:(n + 1) * 512], start=(c == 0), stop=(c == 7)), inc=(c == 7))
                                    for k in range(2):
                                        K.op(pe, [WGR[s_], OTR[i]], [PSR[bb]], lambda: nc.tensor.matmul(PS[bb][:, :], lhsT=WB[s_][:, i, k, :], rhs=oT[:, i, k, n * 512:(n + 1) * 512], start=(k == 0), stop=(k == 1)), inc=(k == 1))
                                    sg, sgr = SG[i % 2], SGR[i % 2]
                                    K.op(act, [PSR[gb]], [sgr], lambda: nc.scalar.activation(out=sg[:], in_=PS[gb][:, :], func=AF.Sigmoid))
                                    if i == 0:
                                        K.op(dve, [sgr, PSR[bb]], [ACR], lambda: nc.vector.tensor_tensor(out=ACC[:], in0=sg[:], in1=PS[bb][:, :], op=ALU.mult))
                                    else:
                                        K.op(dve, [sgr, PSR[bb]], [ACR], lambda: nc.vector.tensor_tensor(out=TMP[:], in0=sg[:], in1=PS[bb][:, :], op=ALU.mult))
                                        if i < 3:
                                            K.op(dve, [ACR], [ACR], lambda: nc.vector.tensor_tensor(out=ACC[:], in0=ACC[:], in1=TMP[:], op=ALU.add))
                                        else:
                                            K.op(dve, [ACR], [MXR], lambda: nc.vector.tensor_tensor(out=mixT[:, f, n * 512:(n + 1) * 512], in0=ACC[:], in1=TMP[:], op=ALU.add))
                        WO = sb("WO", [128, 8, D], BF16, st)
                        WOR = Reg()
                        K.dma(pool, WO[:], wo_d[l].rearrange("(c p) n -> p c n", p=128), [], [WOR])
                        NBF = alloc_normbufs(st)
                        load_g(gffn_d[l])
                        def wo_mm(t):
                            banks = []
                            for hb in range(2):
                                b = (t % 2) * 2 + hb
                                for c in range(8):
                                    K.op(pe, [MXR, WOR], [PSR[b]], lambda: nc.tensor.matmul(PS[b][:, :], lhsT=mixT[:, c, t * 128:(t + 1) * 128], rhs=WO[:, c, hb * 512:(hb + 1) * 512], start=(c == 0), stop=(c == 7)), inc=(c == 7))
                                banks.append((PS[b], PSR[b]))
                            return banks
                        nxt_b = wo_mm(0)
                        for t in range(NT):
                            cur_b = nxt_b
                            if t + 1 < NT:
                                nxt_b = wo_mm(t + 1)
                            norm_tile(NBF, seq, t, y_d[seq, t * 128:(t + 1) * 128, :], cur_b, False, t % 2)
                        K.barrier()
                        if dbg and seq == 0 and l == 0:
                            K.dma(sp, dbg_d[4], y_d[0], [], [])
                            K.barrier()

                with ExitStack() as fst:
                    NH = S // TOKH
                    NCF = TOKH // 512
                    actT = sb("actT", [128, NJ, TOKH], BF16, fst)
                    ATR = Reg()
                    WD = sb("WD", [128, NJ, D], BF16, fst)
                    WDR = Reg()
                    K.dma(pool, WD[:, 0:11, :], wdn_d[l][0:11 * 128, :].rearrange("(c p) n -> p c n", p=128), [], [WDR])
                    K.dma(pool, WD[:, 11:22, :], wdn_d[l][11 * 128:22 * 128, :].rearrange("(c p) n -> p c n", p=128), [], [WDR])
                    HB = [[sb("HB%d%d" % (i, g), [128, TOKH + 2], F32, fst) for g in range(2)] for i in range(2)]
                    HBR = [[Reg(), Reg()], [Reg(), Reg()]]
                    HALO = sb("HALO", [128, NJ, 2, 2], F32, fst)
                    HLR = Reg()
                    WU = [sb("WU%d" % i, [128, 2, 8, 256], BF16, fst) for i in range(2)]
                    WUR = [Reg(), Reg()]
                    CWt = sb("CWt", [128, 4, 2 * NJ], F32, fst)
                    CWc = sb("CWc", [2 * NJ, 4, 128], F32, fst)
                    CWR = Reg()
                    for j_ in range(3):
                        K.dma(sp, CWc[:, j_, :], cw_d[l, j_].rearrange("(c p) -> c p", p=128), [], [CWR])
                    K.dma(sp, CWc[:, 3, :], cb_d[l].rearrange("(c p) -> c p", p=128), [], [CWR])
                    for j_ in range(4):
                        K.op(pe, [CWR, CR], [PSR[6]], lambda: nc.tensor.transpose(PS[6][:, j_ * 2 * NJ:(j_ + 1) * 2 * NJ], CWc[:, j_, :], ident_f[0:2 * NJ, 0:2 * NJ]), inc=(j_ == 3))
                    K.op(dve, [PSR[6]], [CWR], lambda: nc.vector.tensor_copy(out=CWt[:], in_=PS[6][:, 0:8 * NJ].rearrange("p (a c) -> p a c", c=2 * NJ)))
                    CA2 = [[[sb("CA%d_%d_%d" % (p_, g, n_), [128, 512], F32, fst) for n_ in range(TOKH // 512)] for g in range(2)] for p_ in range(2)]
                    CAR2 = [[[Reg() for n_ in range(TOKH // 512)] for g in range(2)] for p_ in range(2)]
                    GL2 = [sb("GL%d" % i_, [128, 512], F32, fst) for i_ in range(2)]
                    GLR2 = [Reg(), Reg()]
                    NBF = alloc_normbufs(fst)
                    final = (l == DEPTH - 1)
                    load_g(gfin_d[0] if final else gmix_d[l + 1])

                    def load_wu(jp):
                        s_ = jp % 2
                        for g in range(2):
                            K.dma(pool, WU[s_][:, g, :, :], wup_d[l][:, g * DFF + jp * 256:g * DFF + (jp + 1) * 256].rearrange("(c p) n -> p c n", p=128), [], [WUR[s_]])

                    for half in range(NH):
                        tk0 = half * TOKH
                        load_wu(0)

                        def st1(j):
                            if j % 2 == 0 and j // 2 + 1 < NJ // 2:
                                load_wu(j // 2 + 1)
                            s_ = j % 2
                            ws_ = (j // 2) % 2
                            wo_ = (j % 2) * 128
                            CA, CAR = CA2[s_], CAR2[s_]
                            for g in range(2):
                                hbuf, hbr = HB[s_][g], HBR[s_][g]
                                if half == 0:
                                    K.op(dve, [], [hbr], lambda: nc.vector.memset(hbuf[:, 0:2], 0.0))
                                else:
                                    K.op(dve, [HLR], [hbr], lambda: nc.vector.tensor_copy(out=hbuf[:, 0:2], in_=HALO[:, j, g, :]))
                                for n in range(NCF):
                                    b = next_bank(0, 6)
                                    tn = (tk0 + n * 512) // 128
                                    for c in range(8):
                                        K.op(pe, [WUR[ws_]] + XNR[tn:tn + 4], [PSR[b]], lambda: nc.tensor.matmul(PS[b][:, :], lhsT=WU[ws_][:, g, c, wo_:wo_ + 128], rhs=xnT[:, c, tk0 + n * 512:tk0 + (n + 1) * 512], start=(c == 0), stop=(c == 7)), inc=(c == 7))
                                    K.op(act, [PSR[b]], [hbr], lambda: nc.scalar.copy(out=hbuf[:, 2 + n * 512:2 + (n + 1) * 512], in_=PS[b][:, :]))
                                    ch_ = g * NJ + j
                                    K.op(act, [PSR[b], CWR], [CAR[g][n]], lambda: nc.scalar.activation(out=CA[g][n][:], in_=PS[b][:, :], func=AF.Identity, scale=CWt[:, 2, ch_:ch_ + 1], bias=CWt[:, 3, ch_:ch_ + 1]))
                                if half + 1 < NH:
                                    K.op(dve, [hbr], [HLR], lambda: nc.vector.tensor_copy(out=HALO[:, j, g, :], in_=hbuf[:, TOKH:TOKH + 2]))

                        def st2(j):
                            s_ = j % 2
                            CA, CAR = CA2[s_], CAR2[s_]
                            for n in range(NCF):
                                for g in range(2):
                                    hbuf, hbr = HB[s_][g], HBR[s_][g]
                                    ch = g * NJ + j
                                    ca, car = CA[g][n], CAR[g][n]
                                    K.op(dve, [hbr, CWR, car], [car], lambda: nc.vector.scalar_tensor_tensor(out=ca[:], in0=hbuf[:, 1 + n * 512:1 + (n + 1) * 512], scalar=CWt[:, 1, ch:ch + 1], in1=ca[:], op0=ALU.mult, op1=ALU.add))
                                    K.op(dve, [hbr, CWR, car], [car], lambda: nc.vector.scalar_tensor_tensor(out=ca[:], in0=hbuf[:, 0 + n * 512:0 + (n + 1) * 512], scalar=CWt[:, 0, ch:ch + 1], in1=ca[:], op0=ALU.mult, op1=ALU.add))
                                gl, glr = GL2[n % 2], GLR2[n % 2]
                                K.op(act, [CAR[0][n]], [glr], lambda: nc.scalar.activation(out=gl[:], in_=CA[0][n][:], func=AF.Gelu))
                                K.op(dve, [glr, CAR[1][n]], [ATR], lambda: nc.vector.tensor_tensor(out=actT[:, j, n * 512:(n + 1) * 512], in0=gl[:], in1=CA[1][n][:], op=ALU.mult))
                        st1(0)
                        for j in range(NJ):
                            if j + 1 < NJ:
                                st1(j + 1)
                            st2(j)
                        def dn_mm(tt):
                            t = tk0 // 128 + tt
                            banks = []
                            for hb in range(2):
                                b = (t % 2) * 2 + hb
                                for j in range(NJ):
                                    K.op(pe, [ATR, WDR], [PSR[b]], lambda: nc.tensor.matmul(PS[b][:, :], lhsT=actT[:, j, tt * 128:(tt + 1) * 128], rhs=WD[:, j, hb * 512:(hb + 1) * 512], start=(j == 0), stop=(j == NJ - 1)), inc=(j == NJ - 1))
                                banks.append((PS[b], PSR[b]))
                            return banks
                        nxt_b = dn_mm(0)
                        for tt in range(TOKH // 128):
                            t = tk0 // 128 + tt
                            cur_b = nxt_b
                            if tt + 1 < TOKH // 128:
                                nxt_b = dn_mm(tt + 1)
                            norm_tile(NBF, seq, t, y_d[seq, t * 128:(t + 1) * 128, :], cur_b, False, t % 2)
                        K.barrier()
                    if final:
                        for t in range(NT):
                            norm_tile(NBF, seq, t, y_d[seq, t * 128:(t + 1) * 128, :], None, True, t % 2)
                    K.barrier()
        K.barrier()
    return nc, hc


_CACHE = {}


def kernel(**inputs):
    S, DEPTH, NSEQ, NCORE = 2048, 4, 2, 8
    if "prog" not in _CACHE:
        _CACHE["prog"] = build(S, DEPTH, NSEQ)
    nc, hc = _CACHE["prog"]
    f = lambda a: np.ascontiguousarray(np.asarray(a, dtype=np.float32))
    x = f(inputs["x"])
    shared = {k: f(inputs[k]) for k in ["norm_mix_g", "norm_ffn_g", "w_in", "fox_b_f", "w_gate", "w_branch", "w_out", "w_up", "conv_w", "conv_b", "w_down"]}
    shared["norm_final_g"] = f(inputs["norm_final_g"]).reshape(1, D)
    shared["t5_bias"] = f(inputs["t5_bias"]).reshape(1, 256)
    shared.update(hc)
    in_maps = []
    for c in range(NCORE):
        m = dict(shared)
        m["x"] = np.ascontiguousarray(x[c * NSEQ:(c + 1) * NSEQ])
        in_maps.append(m)
    res = run_bass_kernel_spmd(nc, in_maps, core_ids=list(range(NCORE)))
    return np.concatenate([r["y"] for r in res.results], axis=0).astype(np.float32)
```

```python
import math
from contextlib import ExitStack
import numpy as np
import concourse.bass as bass
import concourse.mybir as mybir
from concourse.bass_utils import run_bass_kernel_spmd

F32 = mybir.dt.float32
BF16 = mybir.dt.bfloat16
AF = mybir.ActivationFunctionType
ALU = mybir.AluOpType
AX = mybir.AxisListType

D = 1024
HD = 64
PIN = 2988
DFF = 2816
NJ = DFF // 128
C_SB, C_MB, C_FX, C_FXF, C_DQ, C_DK, C_DV, C_IQ, C_IK, C_IW = 0, 768, 1536, 2304, 2308, 2564, 2628, 2692, 2948, 2980
NEG = -30000.0
NIT = 16
SAME_ENGINE_SYNC = True


class Sem:
    def __init__(self, h):
        self.h = h
        self.cnt = 0


class Eng:
    def __init__(self, e, sem, name):
        self.e = e
        self.sem = sem
        self.seen = {}
        self.name = name


class Reg:
    __slots__ = ("w", "r", "name")

    def __init__(self, name=""):
        self.w = None
        self.r = {}
        self.name = name


class KB:
    def __init__(self, nc, es):
        self.nc = nc
        self.es = es
        mk = lambda n: Sem(es.enter_context(nc.semaphore(n)))
        self.pe = Eng(nc.tensor, mk("s_pe"), "pe")
        self.act = Eng(nc.scalar, mk("s_act"), "act")
        self.dve = Eng(nc.vector, mk("s_dve"), "dve")
        self.pool = Eng(nc.gpsimd, mk("s_pool"), "pool")
        self.sp = Eng(nc.sync, mk("s_sp"), "sp")
        self.engs = [self.pe, self.act, self.dve, self.pool, self.sp]
        self.dsems = {"pool": [mk("d_pool%d" % i) for i in range(12)],
                      "sp": [mk("d_sp%d" % i) for i in range(12)]}
        self.drr = {"pool": 0, "sp": 0}
        self.allsems = [e.sem for e in self.engs] + self.dsems["pool"] + self.dsems["sp"]

    def _wait(self, E, deps):
        need = {}
        for (sem, val) in deps:
            if sem is E.sem and (E.name == "pe" or not SAME_ENGINE_SYNC):
                continue
            if need.get(sem, 0) < val:
                need[sem] = val
        for sem, val in need.items():
            if E.seen.get(sem, 0) < val:
                E.e.wait_ge(sem.h, val)
                E.seen[sem] = val

    def _deps(self, reads, writes):
        deps = []
        for r in reads:
            if r.w is not None:
                deps.append(r.w)
        for w in writes:
            if w.w is not None:
                deps.append(w.w)
            deps.extend(w.r.items())
        return deps

    def _record(self, tok, reads, writes):
        sem, val = tok
        for r in reads:
            if r.r.get(sem, 0) < val:
                r.r[sem] = val
        for w in writes:
            w.w = tok
            w.r = {}

    def op(self, E, reads, writes, fn, inc=True):
        self._wait(E, self._deps(reads, writes))
        ins = fn()
        if inc:
            E.sem.cnt += 1
            ins.then_inc(E.sem.h, 1)
            tok = (E.sem, E.sem.cnt)
        else:
            tok = (E.sem, E.sem.cnt + 1)
        self._record(tok, reads, writes)
        return tok

    def dma(self, Q, out, in_, reads, writes):
        sems = self.dsems[Q.name]
        s = sems[self.drr[Q.name] % len(sems)]
        self.drr[Q.name] += 1
        deps = self._deps(reads, writes)
        if s.cnt > 0:
            deps.append((s, s.cnt))
        self._wait(Q, deps)
        Q.e.dma_start(out=out, in_=in_).then_inc(s.h, 16)
        s.cnt += 16
        tok = (s, s.cnt)
        self._record(tok, reads, writes)
        return tok

    def barrier(self):
        for E in self.engs:
            for s in self.allsems:
                if s is E.sem or s.cnt == 0:
                    continue
                if E.seen.get(s, 0) < s.cnt:
                    E.e.wait_ge(s.h, s.cnt)
                    E.seen[s] = s.cnt


def t5_bucket_np(n):
    n = np.maximum(n, 0)
    nf = np.maximum(n, 1).astype(np.float32)
    large = 16 + (np.log(nf / np.float32(16)) / np.float32(math.log(128 / 16)) * np.float32(16)).astype(np.int32)
    large = np.minimum(large, 31)
    return np.where(n < 16, n, large)


def host_consts(S):
    c = {}
    i = np.arange(128)
    c["c_ident"] = np.eye(128, dtype=np.float32)
    c["c_ident4"] = np.tile(np.eye(128, dtype=np.float32), (1, 4))
    c["c_uinclneg"] = -(i[:, None] >= i[None, :]).astype(np.float32)
    c["c_onesneg"] = -np.ones((128, 128), np.float32)
    c["c_cmstrict"] = np.where(i[:, None] >= i[None, :], NEG, 0.0).astype(np.float32)
    c["c_cmincl"] = np.where(i[:, None] > i[None, :], NEG, 0.0).astype(np.float32)
    c["c_adm"] = np.where(i[None, :] <= i[:, None], 0.0, -1e30).astype(np.float32)
    dd = np.arange(256)[None, :] - i[:, None]
    c["c_buck"] = np.where(dd >= 0, t5_bucket_np(dd), 99).astype(np.float32)
    c["c_tmask"] = np.where(dd >= 0, 0.0, NEG).astype(np.float32)
    c["c_blk"] = (np.arange(S)[None, :] // 256 == np.arange(8)[:, None]).astype(np.float32)
    c["c_ones"] = np.ones((8, S), np.float32)
    c["c_pow"] = np.tile((0.5 ** (np.arange(NIT) + 1)).astype(np.float32)[None, :], (128, 1))
    return c


def build(S, DEPTH, NSEQ, dbg=False):
    NT = S // 128
    NCH = S // 512
    NB = S // 256
    TOPK = min(3, NB)
    KSEL = min(256, S // 4)
    TOKH = min(S, 1024)
    nc = bass.Bass("TRN2", target_bir_lowering=False)
    es = ExitStack()
    with es:
        def din(name, shape):
            return nc.dram_tensor(name, list(shape), F32, kind="ExternalInput").ap()
        x_d = din("x", [NSEQ, S, D])
        y_d = nc.dram_tensor("y", [NSEQ, S, D], F32, kind="ExternalOutput").ap()
        gmix_d = din("norm_mix_g", [DEPTH, D])
        gffn_d = din("norm_ffn_g", [DEPTH, D])
        gfin_d = din("norm_final_g", [1, D])
        win_d = din("w_in", [DEPTH, D, PIN])
        fxb_d = din("fox_b_f", [DEPTH, 4])
        t5_d = din("t5_bias", [1, 256])
        wg_d = din("w_gate", [DEPTH, 4, D, D])
        wb_d = din("w_branch", [DEPTH, 4, 256, D])
        wo_d = din("w_out", [DEPTH, D, D])
        wup_d = din("w_up", [DEPTH, D, 2 * DFF])
        cw_d = din("conv_w", [DEPTH, 3, 2 * DFF])
        cb_d = din("conv_b", [DEPTH, 2 * DFF])
        wdn_d = din("w_down", [DEPTH, DFF, D])
        hc = host_consts(S)
        cst = {k: din(k, v.shape) for k, v in hc.items()}
        dbg_d = nc.dram_tensor("dbg", [6, S, D], F32, kind="ExternalOutput").ap() if dbg else None

        K = KB(nc, es)
        pe, act, dve, pool, sp = K.pe, K.act, K.dve, K.pool, K.sp

        uniq = [0]

        def sb(name, shape, dt, st=None):
            uniq[0] += 1
            return (st or es).enter_context(nc.sbuf_tensor("%s_%d" % (name, uniq[0]), list(shape), dt))

        PS = [es.enter_context(nc.psum_tensor("ps%d" % i, [128, 512], F32)) for i in range(7)]
        PSR = [Reg("ps%d" % i) for i in range(7)]
        PSB = es.enter_context(nc.psum_tensor("psb", [128, 1024], BF16))
        PSBR = Reg("psb")

        ident_f = sb("ident_f", [128, 128], F32)
        ident_b = sb("ident_b", [128, 128], BF16)
        ident4 = sb("ident4", [128, 512], BF16)
        uinclneg = sb("uinclneg", [128, 128], BF16)
        onesneg = sb("onesneg", [128, 128], BF16)
        cmstrict = sb("cmstrict", [128, 128], BF16)
        cmincl = sb("cmincl", [128, 128], BF16)
        adm = sb("adm", [128, 128], F32)
        buck = sb("buck", [128, 256], F32)
        tmask = sb("tmask", [128, 256], F32)
        powt = sb("powt", [128, NIT], F32)
        t5bc = sb("t5bc", [128, 256], F32)
        tbias = sb("tbias", [128, 8, 256], BF16)
        CR = Reg("const")
        for (t, k, q) in [(ident_f, "c_ident", sp), (ident_b, "c_ident", pool), (ident4, "c_ident4", pool),
                          (uinclneg, "c_uinclneg", pool), (onesneg, "c_onesneg", pool), (cmstrict, "c_cmstrict", pool),
                          (cmincl, "c_cmincl", pool), (adm, "c_adm", sp), (buck, "c_buck", sp), (tmask, "c_tmask", sp),
                          (powt, "c_pow", sp)]:
            K.dma(q, t[:], cst[k], [], [CR])
        K.dma(sp, t5bc[:], bass.AP(tensor=t5_d.tensor, offset=0, ap=[[0, 128], [1, 256]]), [], [CR])
        with ExitStack() as st:
            tacc = sb("tacc", [128, 256], F32, st)
            ttmp = sb("ttmp", [128, 256], F32, st)
            TR = Reg("tb")
            for h in range(8):
                for b in range(32):
                    col = t5bc[:, b * 8 + h:b * 8 + h + 1]
                    if b == 0:
                        K.op(dve, [CR], [TR], lambda: nc.vector.tensor_scalar(out=tacc[:], in0=buck[:], scalar1=float(b), scalar2=col, op0=ALU.is_equal, op1=ALU.mult))
                    else:
                        K.op(dve, [CR], [TR], lambda: nc.vector.tensor_scalar(out=ttmp[:], in0=buck[:], scalar1=float(b), scalar2=col, op0=ALU.is_equal, op1=ALU.mult))
                        K.op(dve, [TR], [TR], lambda: nc.vector.tensor_tensor(out=tacc[:], in0=tacc[:], in1=ttmp[:], op=ALU.add))
                c31 = t5bc[:, 31 * 8 + h:31 * 8 + h + 1]
                K.op(dve, [TR, CR], [CR], lambda: nc.vector.scalar_tensor_tensor(out=tbias[:, h, :], in0=tacc[:], scalar=c31, in1=tmask[:], op0=ALU.subtract, op1=ALU.add))
            K.barrier()

        xnT = sb("xnT", [128, 8, S], BF16)
        XNR = [Reg("xnT%d" % t) for t in range(NT)]
        gbc = sb("gbc", [128, D], F32)
        GBR = Reg("gbc")

        def load_g(src_row_ap):
            K.dma(sp, gbc[:], bass.AP(tensor=src_row_ap.tensor, offset=src_row_ap.offset, ap=[[0, 128], [1, D]]), [], [GBR])

        YR = [[Reg("y%d_%d" % (s, t)) for t in range(NT)] for s in range(NSEQ)]

        class NormBufs:
            pass

        def norm_tile(NBF, seq, t, src_ap, delta_banks, final, slot):
            xt = NBF.xt[slot]
            XTR = NBF.xtr[slot]
            K.dma(sp, xt[:], src_ap, [YR[seq][t]], [XTR])
            if delta_banks is not None:
                for hb, (bk, bkr) in enumerate(delta_banks):
                    K.op(dve, [XTR, bkr], [XTR], lambda: nc.vector.tensor_tensor(out=xt[:, hb * 512:(hb + 1) * 512], in0=xt[:, hb * 512:(hb + 1) * 512], in1=bk[:, :], op=ALU.add))
                if not final:
                    K.dma(sp, y_d[seq, t * 128:(t + 1) * 128, :], xt[:], [XTR], [YR[seq][t]])
            K.op(act, [XTR], [NBF.sqr[slot]], lambda: nc.scalar.activation(out=NBF.sq[slot][:], in_=xt[:], func=AF.Square, accum_out=NBF.ss[slot][:]))
            K.op(act, [NBF.sqr[slot]], [NBF.sqr[slot]], lambda: nc.scalar.activation(out=NBF.rs[slot][:], in_=NBF.ss[slot][:], func=AF.Sqrt, scale=1.0 / D, bias=NBF.eps[:]))
            K.op(dve, [NBF.sqr[slot]], [NBF.sqr[slot]], lambda: nc.vector.reciprocal(out=NBF.rs[slot][:], in_=NBF.rs[slot][:]))
            if final:
                K.op(dve, [XTR, NBF.sqr[slot], GBR], [XTR], lambda: nc.vector.scalar_tensor_tensor(out=xt[:], in0=xt[:], scalar=NBF.rs[slot][:], in1=gbc[:], op0=ALU.mult, op1=ALU.mult))
                K.dma(sp, y_d[seq, t * 128:(t + 1) * 128, :], xt[:], [XTR], [YR[seq][t]])
                return
            xb = NBF.xb[slot]
            XBR = NBF.xbr[slot]
            K.op(dve, [XTR, NBF.sqr[slot], GBR], [XBR], lambda: nc.vector.scalar_tensor_tensor(out=xb[:], in0=xt[:], scalar=NBF.rs[slot][:], in1=gbc[:], op0=ALU.mult, op1=ALU.mult))
            for c in range(8):
                K.op(pe, [XBR, CR], [PSBR], lambda: nc.tensor.transpose(PSB[:, c * 128:(c + 1) * 128], xb[:, c * 128:(c + 1) * 128], ident_b[:]), inc=(c == 7))
            K.op(act, [PSBR], [XNR[t]], lambda: nc.scalar.copy(out=xnT[:, :, t * 128:(t + 1) * 128], in_=PSB[:, :].rearrange("p (c n) -> p c n", n=128)))

        def alloc_normbufs(st):
            NBF = NormBufs()
            NBF.xt = [sb("nb_xt%d" % i, [128, D], F32, st) for i in range(2)]
            NBF.xtr = [Reg() for i in range(2)]
            NBF.sq = [sb("nb_sq%d" % i, [128, D], BF16, st) for i in range(2)]
            NBF.ss = [sb("nb_ss%d" % i, [128, 1], F32, st) for i in range(2)]
            NBF.rs = [sb("nb_rs%d" % i, [128, 1], F32, st) for i in range(2)]
            NBF.sqr = [Reg() for i in range(2)]
            NBF.xb = [sb("nb_xb%d" % i, [128, D], BF16, st) for i in range(2)]
            NBF.xbr = [Reg() for i in range(2)]
            NBF.eps = sb("nb_eps", [128, 1], F32, st)
            K.op(dve, [], [NBF.sqr[0], NBF.sqr[1]], lambda: nc.vector.memset(NBF.eps[:], 1e-6))
            return NBF

        bank_rr = [0]

        def next_bank(lo=0, hi=7):
            b = lo + bank_rr[0] % (hi - lo)
            bank_rr[0] += 1
            return b

        def proj_fm(W, WR, wcol, M, dst_fn, dst_reg, scale=None, evac_eng=None):
            for n in range(NCH):
                b = next_bank()
                for c in range(8):
                    K.op(pe, [WR] + XNR[n * 4:(n + 1) * 4], [PSR[b]],
                         lambda: nc.tensor.matmul(PS[b][0:M, :], lhsT=W[:, c, wcol:wcol + M], rhs=xnT[:, c, n * 512:(n + 1) * 512], start=(c == 0), stop=(c == 7)), inc=(c == 7))
                dst = dst_fn(n)
                if scale is None:
                    K.op(dve, [PSR[b]], [dst_reg], lambda: nc.vector.tensor_copy(out=dst, in_=PS[b][0:M, :]))
                else:
                    K.op(dve, [PSR[b]], [dst_reg], lambda: nc.vector.tensor_scalar(out=dst, in0=PS[b][0:M, :], scalar1=float(scale), scalar2=None, op0=ALU.mult))

        def proj_tm(W, WR, wcol, ncol, evac_fn):
            for t in range(NT):
                b = next_bank()
                for c in range(8):
                    K.op(pe, [WR, XNR[t]], [PSR[b]],
                         lambda: nc.tensor.matmul(PS[b][:, 0:ncol], lhsT=xnT[:, c, t * 128:(t + 1) * 128], rhs=W[:, c, wcol:wcol + ncol], start=(c == 0), stop=(c == 7)), inc=(c == 7))
                evac_fn(t, PS[b], PSR[b])

        def load_win(l, W, WR, c0, c1, dcol=0):
            K.dma(pool, W[:, :, dcol:dcol + (c1 - c0)], win_d[l][:, c0:c1].rearrange("(c p) n -> p c n", p=128), [], [WR])

        for seq in range(NSEQ):
            with ExitStack() as st:
                NBF = alloc_normbufs(st)
                load_g(gmix_d[0])
                for t in range(NT):
                    slot = t % 2
                    xt = NBF.xt[slot]
                    K.dma(sp, xt[:], x_d[seq, t * 128:(t + 1) * 128, :], [], [NBF.xtr[slot]])
                    K.dma(sp, y_d[seq, t * 128:(t + 1) * 128, :], xt[:], [NBF.xtr[slot]], [YR[seq][t]])
                    norm_tile(NBF, seq, t, y_d[seq, t * 128:(t + 1) * 128, :], None, False, slot)
                K.barrier()

            for l in range(DEPTH):
                with ExitStack() as mst:
                    ast = ExitStack()
                    oT = sb("oT", [128, 4, 2, S], BF16, mst)
                    OTR = [Reg("oT%d" % m) for m in range(4)]
                    QT = [sb("QT%d" % h, [72, S], BF16, ast) for h in range(4)]
                    KT = [sb("KT%d" % h, [72, S], BF16, ast) for h in range(4)]
                    QTR = [Reg("QT%d" % h) for h in range(4)]
                    QAR = [Reg("QA%d" % h) for h in range(4)]
                    KTR = [Reg("KT%d" % h) for h in range(4)]
                    V = sb("V", [128, NT, 4, 65], BF16, ast)
                    VR = Reg("V")
                    OTOK = sb("OTOK", [128, NT, 256], BF16, ast)
                    OKR = Reg("OTOK")
                    W = [sb("Wm%d" % i, [128, 8, 776], BF16, ast) for i in range(2)]
                    WR = [Reg("Wm%d" % i) for i in range(2)]
                    PT = [sb("PT%d" % i, [128, 512], BF16, ast) for i in range(2)]
                    PTR = [Reg("PT%d" % i) for i in range(2)]
                    orec = sb("orec", [128, 4], F32, ast)
                    ORR = Reg("orec")
                    K.op(dve, [], [VR], lambda: nc.vector.memset(V[:], 1.0))

                    def v_evac(nheads):
                        def f(t, ps, psr):
                            K.op(dve, [psr], [VR], lambda: nc.vector.tensor_copy(out=V[:, t, 0:nheads, 0:64], in_=ps[:, 0:nheads * 64].rearrange("p (h d) -> p h d", d=64)))
                        return f

                    def qk_proj(Wt, WRt, base, with_scale=True):
                        for h in range(4):
                            proj_fm(Wt, WRt, base + h * 64, 64, (lambda n, h=h: QT[h][0:64, n * 512:(n + 1) * 512]), QTR[h], scale=0.125)
                            proj_fm(Wt, WRt, base + 256 + h * 64, 64, (lambda n, h=h: KT[h][0:64, n * 512:(n + 1) * 512]), KTR[h])

                    def finish_o(ob, obr, tq0, nsub, h, normalize):
                        o3 = PS[ob][:, 0:nsub * 65].rearrange("p (s d) -> p s d", d=65)
                        dst = OTOK[:, tq0:tq0 + nsub, h * 64:(h + 1) * 64]
                        if normalize:
                            K.op(dve, [obr], [ORR], lambda: nc.vector.reciprocal(out=orec[:, 0:nsub], in_=o3[:, :, 64]))
                            for s_ in range(nsub):
                                K.op(dve, [obr, ORR], [OKR], lambda: nc.vector.tensor_scalar(out=OTOK[:, tq0 + s_, h * 64:(h + 1) * 64], in0=o3[:, s_, 0:64], scalar1=orec[:, s_:s_ + 1], scalar2=None, op0=ALU.mult))
                        else:
                            K.op(dve, [obr], [OKR], lambda: nc.vector.tensor_copy(out=dst, in_=o3[:, :, 0:64]))

                    def otok_to_oT(m):
                        if dbg and seq == 0 and l == 0:
                            K.dma(pool, dbg_d[m, :, 0:256].rearrange("(t p) c -> p t c", p=128), OTOK[:], [OKR], [])
                        for t in range(NT):
                            for k in range(2):
                                K.op(pe, [OKR, CR], [PSBR], lambda: nc.tensor.transpose(PSB[:, k * 128:(k + 1) * 128], OTOK[:, t, k * 128:(k + 1) * 128], ident_b[:]), inc=(k == 1))
                            K.op(act, [PSBR], [OTR[m]], lambda: nc.scalar.copy(out=oT[:, m, :, t * 128:(t + 1) * 128], in_=PSB[:, 0:256].rearrange("p (c n) -> p c n", n=128)))

                    def attn_softmax(h, kdim, near_bias, diag_mask):
                        for cq in range(NCH):
                            ob = 6
                            K.op(dve, [], [PSR[ob]], lambda: nc.vector.memset(PS[ob][:, 0:260], 0.0))
                            t0 = cq * 512
                            na = 4 * cq + 4

                            def stA(a):
                                r = a - 4 * cq
                                off = max(0, r) * 128
                                xb = a % 2
                                extra = []
                                if near_bias is not None:
                                    if r >= 0:
                                        w_ = min(256, 512 - off)
                                        extra.append((ident_b, tbias[:, near_bias, 0:w_], off, w_))
                                    elif r == -1:
                                        extra.append((ident_b, tbias[:, near_bias, 128:256], 0, 128))
                                elif diag_mask is not None and r >= 0:
                                    extra.append((ident_b, diag_mask[:, :], off, 128))
                                K.op(pe, [KTR[h], QTR[h], QAR[h]], [PSR[xb]], lambda: nc.tensor.matmul(PS[xb][:, off:512], lhsT=KT[h][0:kdim, a * 128:(a + 1) * 128], rhs=QT[h][0:kdim, t0 + off:t0 + 512], start=True, stop=(len(extra) == 0), skip_group_check=True), inc=(len(extra) == 0))
                                for ei, (lt, rh, eo, ew) in enumerate(extra):
                                    K.op(pe, [CR], [PSR[xb]], lambda: nc.tensor.matmul(PS[xb][:, eo:eo + ew], lhsT=lt[:, :], rhs=rh, start=False, stop=(ei == len(extra) - 1), skip_group_check=True), inc=(ei == len(extra) - 1))

                            def stB(a):
                                r = a - 4 * cq
                                off = max(0, r) * 128
                                xb = a % 2
                                pt, ptr = PT[a % 2], PTR[a % 2]
                                K.op(act, [PSR[xb]], [ptr], lambda: nc.scalar.activation(out=pt[:, off:512], in_=PS[xb][:, off:512], func=AF.Exp))
                                for s_ in range(max(0, r), 4):
                                    K.op(pe, [ptr, VR], [PSR[ob]], lambda: nc.tensor.matmul(PS[ob][:, s_ * 65:(s_ + 1) * 65], lhsT=pt[:, s_ * 128:(s_ + 1) * 128], rhs=V[:, a, h, :], start=False, stop=False, skip_group_check=True), inc=(s_ == 3))
                            stA(0)
                            for a in range(na):
                                if a + 1 < na:
                                    stA(a + 1)
                                stB(a)
                            finish_o(ob, PSR[ob], cq * 4, 4, h, True)

                    load_win(l, W[0], WR[0], C_SB, C_SB + 768)
                    load_win(l, W[1], WR[1], C_MB, C_MB + 768)
                    proj_tm(W[0], WR[0], 512, 256, v_evac(4))
                    with ExitStack() as st:
                        QS = sb("QS", [128, 2, S], BF16, st)
                        KS = sb("KS", [128, 2, S], BF16, st)
                        QSR, KSR = Reg(), Reg()
                        for k2 in range(2):
                            proj_fm(W[0], WR[0], k2 * 128, 128, (lambda n, k2=k2: QS[:, k2, n * 512:(n + 1) * 512]), QSR, scale=0.125)
                            proj_fm(W[0], WR[0], 256 + k2 * 128, 128, (lambda n, k2=k2: KS[:, k2, n * 512:(n + 1) * 512]), KSR)
                        E1 = sb("E1", [128, 512], F32, st)
                        SPb = [sb("SPb%d" % i, [128, 512], BF16, st) for i in range(2)]
                        SACC = sb("SACC", [128, 512], F32, st)
                        SACCB = sb("SACCB", [128, 512], BF16, st)
                        E1R, SAR, SBR = Reg(), Reg(), Reg()
                        SPR = [Reg(), Reg()]
                        for h in range(4):
                            for cq in range(NCH):
                                ob = 6
                                K.op(dve, [], [PSR[ob]], lambda: nc.vector.memset(PS[ob][:, 0:260], 0.0))
                                K.op(dve, [], [SAR], lambda: nc.vector.memset(SACC[:], 0.0))
                                t0 = cq * 512
                                atop = 4 * cq + 3

                                def offs(a):
                                    return max(0, a - 4 * cq) * 128

                                def stA(a):
                                    r = a - 4 * cq
                                    off = offs(a)
                                    xb = a % 3
                                    spb, spr = SPb[a % 2], SPR[a % 2]
                                    K.op(pe, [KSR, QSR], [PSR[xb]], lambda: nc.tensor.matmul(PS[xb][:, off:512], lhsT=KS[(h % 2) * 64:(h % 2) * 64 + 64, h // 2, a * 128:(a + 1) * 128], rhs=QS[(h % 2) * 64:(h % 2) * 64 + 64, h // 2, t0 + off:t0 + 512], start=True, stop=False, skip_group_check=True), inc=(r < 0))
                                    if r >= 0:
                                        K.op(pe, [CR], [PSR[xb]], lambda: nc.tensor.matmul(PS[xb][:, off:off + 128], lhsT=ident_b[:, :], rhs=cmstrict[:, :], start=False, stop=False, skip_group_check=True))
                                    K.op(act, [PSR[xb]], [E1R], lambda: nc.scalar.activation(out=E1[:, off:512], in_=PS[xb][:, off:512], func=AF.Exp))
                                    K.op(act, [E1R], [spr], lambda: nc.scalar.activation(out=spb[:, off:512], in_=E1[:, off:512], func=AF.Ln, bias=1.0))

                                def stB1(a):
                                    off = offs(a)
                                    xb = a % 3
                                    spb, spr = SPb[a % 2], SPR[a % 2]
                                    first = (a == atop)
                                    prev_off = 512 if first else offs(a + 1)
                                    K.op(pe, [spr, CR], [PSR[xb]], lambda: nc.tensor.matmul(PS[xb][:, off:512], lhsT=uinclneg[:, :], rhs=spb[:, off:512], start=False, stop=first, skip_group_check=True), inc=first)
                                    if not first:
                                        K.op(pe, [SBR, CR], [PSR[xb]], lambda: nc.tensor.matmul(PS[xb][:, prev_off:512], lhsT=onesneg[:, :], rhs=SACCB[:, prev_off:512], start=False, stop=True, skip_group_check=True))
                                    if a > 0:
                                        K.op(dve, [spr, SAR], [SAR], lambda: nc.vector.tensor_tensor(out=SACC[:, off:512], in0=SACC[:, off:512], in1=spb[:, off:512], op=ALU.add))
                                        K.op(dve, [SAR], [SBR], lambda: nc.vector.tensor_copy(out=SACCB[:, off:512], in_=SACC[:, off:512]))

                                def stB2(a):
                                    r = a - 4 * cq
                                    off = offs(a)
                                    xb = a % 3
                                    pt, ptr = PT[a % 2], PTR[a % 2]
                                    K.op(act, [PSR[xb]], [ptr], lambda: nc.scalar.activation(out=pt[:, off:512], in_=PS[xb][:, off:512], func=AF.Exp))
                                    for s_ in range(max(0, r), 4):
                                        K.op(pe, [ptr, VR], [PSR[ob]], lambda: nc.tensor.matmul(PS[ob][:, s_ * 65:s_ * 65 + 64], lhsT=pt[:, s_ * 128:(s_ + 1) * 128], rhs=V[:, a, h, 0:64], start=False, stop=False, skip_group_check=True), inc=(s_ == 3))
                                T_ = list(range(atop, -1, -1))
                                for k_ in range(len(T_) + 2):
                                    if k_ < len(T_):
                                        stA(T_[k_])
                                    if 0 <= k_ - 1 < len(T_):
                                        stB1(T_[k_ - 1])
                                    if 0 <= k_ - 2 < len(T_):
                                        stB2(T_[k_ - 2])
                                finish_o(ob, PSR[ob], cq * 4, 4, h, False)
                        otok_to_oT(0)
                        K.barrier()

                    load_win(l, W[0], WR[0], C_FX, C_FX + 772)
                    qk_proj(W[1], WR[1], 0)
                    proj_tm(W[1], WR[1], 512, 256, v_evac(4))
                    with ExitStack() as st:
                        kmean = sb("kmean", [64, 4, 8], F32, st)
                        kmb = sb("kmb", [64, 4, 8], BF16, st)
                        KMR = Reg()
                        NSET = 2
                        G8s = [sb("G8_%d" % i_, [128, 8], F32, st) for i_ in range(NSET)]
                        M8s = [sb("M8_%d" % i_, [128, 8], F32, st) for i_ in range(NSET)]
                        thrs = [sb("thr_%d" % i_, [128, 1], F32, st) for i_ in range(NSET)]
                        SELTs = [sb("SELT_%d" % i_, [128, 72], F32, st) for i_ in range(NSET)]
                        GRs = [Reg() for i_ in range(NSET)]
                        SELRs = [Reg() for i_ in range(NSET)]
                        PSBRs = [Reg() for i_ in range(NSET)]
                        for i_ in range(NSET):
                            K.op(dve, [], [SELRs[i_]], lambda: nc.vector.memset(SELTs[i_][:], 0.0))
                        for h in range(4):
                            K.dma(pool, KT[h][64:72, :], cst["c_blk"], [], [KTR[h]])
                            K.op(dve, [KTR[h]], [KMR], lambda: nc.vector.tensor_reduce(out=kmean[:, h, 0:NB], in_=KT[h][0:64, :].rearrange("p (n k) -> p n k", k=256), axis=AX.X, op=ALU.add))
                        K.op(dve, [KMR], [KMR], lambda: nc.vector.tensor_scalar(out=kmb[:], in0=kmean[:], scalar1=1.0 / 256, scalar2=None, op0=ALU.mult))
                        chain = 0
                        for h in range(4):
                            for t in range(NT):
                                own = t // 2
                                k_ = chain % NSET
                                chain += 1
                                G8, M8, thr, SELT, GR, SELR = G8s[k_], M8s[k_], thrs[k_], SELTs[k_], GRs[k_], SELRs[k_]
                                K.op(dve, [], [SELR], lambda: nc.vector.memset(SELT[:, 64:72], 0.0))
                                if own > 0:
                                    b = 2 + k_
                                    K.op(pe, [QTR[h], KMR], [PSR[b]], lambda: nc.tensor.matmul(PS[b][:, 0:8], lhsT=QT[h][0:64, t * 128:(t + 1) * 128], rhs=kmb[:, h, :], start=True, stop=True))
                                    K.op(dve, [], [GR], lambda: nc.vector.memset(G8[:], -1e30))
                                    K.op(dve, [PSR[b]], [GR], lambda: nc.vector.tensor_copy(out=G8[:, 0:own], in_=PS[b][:, 0:own]))
                                    K.op(dve, [GR], [GR], lambda: nc.vector.max(out=M8[:], in_=G8[:]))
                                    K.op(dve, [GR], [GR], lambda: nc.vector.tensor_scalar(out=thr[:], in0=M8[:, TOPK - 1:TOPK], scalar1=-1e29, scalar2=None, op0=ALU.max))
                                    K.op(dve, [GR], [SELR], lambda: nc.vector.tensor_scalar(out=SELT[:, 64:64 + own], in0=G8[:, 0:own], scalar1=thr[:], scalar2=NEG, op0=ALU.is_lt, op1=ALU.mult))
                                tb_ = 4 + k_
                                K.op(pe, [SELR, CR], [PSR[tb_]], lambda: nc.tensor.transpose(PS[tb_][0:72, 0:128], SELT[:, :], ident_f[:]))
                                K.op(act, [PSR[tb_]], [QAR[h]], lambda: nc.scalar.copy(out=QT[h][64:72, t * 128:(t + 1) * 128], in_=PS[tb_][64:72, 0:128]))
                        K.barrier()
                        for h in range(4):
                            attn_softmax(h, 72, h, None)
                        otok_to_oT(1)
                        K.barrier()

                    load_win(l, W[1], WR[1], C_DQ, C_DQ + 680)
                    load_win(l, W[1], WR[1], C_IK, C_IK + 32, dcol=680)
                    load_win(l, W[1], WR[1], C_IK, C_IK + 32, dcol=712)
                    qk_proj(W[0], WR[0], 0)
                    proj_tm(W[0], WR[0], 512, 256, v_evac(4))
                    with ExitStack() as st:
                        fb = sb("fb", [4, 1], F32, st)
                        ef = sb("ef", [4, S], F32, st)
                        spf = sb("spf", [4, S], F32, st)
                        ones4 = sb("ones4", [4, S], F32, st)
                        cc = sb("cc", [4, S], F32, st)
                        chi = sb("chi", [4, S], BF16, st)
                        clo = sb("clo", [4, S], BF16, st)
                        nchi = sb("nchi", [4, S], BF16, st)
                        nclo = sb("nclo", [4, S], BF16, st)
                        FR = Reg()
                        K.dma(sp, fb[:], fxb_d[l].rearrange("(p o) -> p o", o=1), [], [FR])
                        K.op(dve, [FR], [FR], lambda: nc.vector.tensor_scalar(out=fb[:], in0=fb[:], scalar1=-1.0, scalar2=None, op0=ALU.mult))
                        K.op(dve, [], [FR], lambda: nc.vector.memset(ones4[:], 1.0))
                        for n in range(NCH):
                            b = next_bank()
                            for c in range(8):
                                K.op(pe, [WR[0]] + XNR[n * 4:(n + 1) * 4], [PSR[b]], lambda: nc.tensor.matmul(PS[b][0:4, :], lhsT=W[0][:, c, 768:772], rhs=xnT[:, c, n * 512:(n + 1) * 512], start=(c == 0), stop=(c == 7)), inc=(c == 7))
                            K.op(act, [PSR[b], FR], [FR], lambda: nc.scalar.activation(out=ef[:, n * 512:(n + 1) * 512], in_=PS[b][0:4, :], func=AF.Exp, scale=-1.0, bias=fb[:]))
                        K.op(act, [FR], [FR], lambda: nc.scalar.activation(out=spf[:], in_=ef[:], func=AF.Ln, bias=1.0))
                        K.op(dve, [FR], [FR], lambda: nc.vector.tensor_tensor_scan(out=cc[:], data0=ones4[:], data1=spf[:], initial=0.0, op0=ALU.mult, op1=ALU.subtract))
                        K.op(dve, [FR], [FR], lambda: nc.vector.tensor_copy(out=chi[:], in_=cc[:]))
                        K.op(dve, [FR], [FR], lambda: nc.vector.tensor_tensor(out=clo[:], in0=cc[:], in1=chi[:], op=ALU.subtract))
                        K.op(dve, [FR], [FR], lambda: nc.vector.tensor_scalar(out=nchi[:], in0=chi[:], scalar1=-1.0, scalar2=None, op0=ALU.mult))
                        K.op(dve, [FR], [FR], lambda: nc.vector.tensor_scalar(out=nclo[:], in0=clo[:], scalar1=-1.0, scalar2=None, op0=ALU.mult))
                        for h in range(4):
                            K.dma(sp, QT[h][64:65, :], chi[h:h + 1, :], [FR], [QTR[h]])
                            K.dma(sp, QT[h][65:66, :], clo[h:h + 1, :], [FR], [QTR[h]])
                            K.dma(pool, QT[h][66:68, :], cst["c_ones"][0:2, :], [], [QTR[h]])
                            K.dma(pool, KT[h][64:66, :], cst["c_ones"][0:2, :], [], [KTR[h]])
                            K.dma(sp, KT[h][66:67, :], nchi[h:h + 1, :], [FR], [KTR[h]])
                            K.dma(sp, KT[h][67:68, :], nclo[h:h + 1, :], [FR], [KTR[h]])
                        for h in range(4):
                            attn_softmax(h, 68, None, cmincl)
                        otok_to_oT(2)
                        K.barrier()

                    for h in range(4):
                        proj_fm(W[1], WR[1], h * 64, 64, (lambda n, h=h: QT[h][0:64, n * 512:(n + 1) * 512]), QTR[h], scale=0.125)
                    proj_fm(W[1], WR[1], 256, 64, (lambda n: KT[0][0:64, n * 512:(n + 1) * 512]), KTR[0])
                    proj_tm(W[1], WR[1], 320, 64, v_evac(1))
                    with ExitStack() as st:
                        QI = [sb("QI%d" % i, [64, S], BF16, st) for i in range(4)]
                        KI = sb("KI", [64, S], BF16, st)
                        QIR, KIR = Reg(), Reg()
                        WI = sb("WI", [128, NT, 8], F32, st)
                        WA = sb("WA", [128, NT, 8], F32, st)
                        WS = sb("WS", [128, NT, 8], F32, st)
                        WIR = Reg()
                        ISC = sb("ISC", [128, S], F32, st)
                        ISR = Reg()
                        RT = [sb("RT%d" % i, [128, 512], F32, st) for i in range(2)]
                        RTR = [Reg(), Reg()]
                        MNB = [sb("MNEG%d" % i_, [128, S], BF16, st) for i_ in range(2)]
                        MNRB = [Reg(), Reg()]
                        MZ = sb("MZ", [128, 128], BF16, st)
                        MZR = Reg()
                        lo = sb("lo", [128, 1], F32, st)
                        hi_ = sb("hi_", [128, 1], F32, st)
                        hw = sb("hw", [128, NIT], F32, st)
                        mid = sb("mid", [128, 1], F32, st)
                        cnt = sb("cnt", [128, 1], F32, st)
                        tmp1 = sb("tmp1", [128, 1], F32, st)
                        BR = Reg()
                        for i in range(4):
                            proj_fm(W[1], WR[1], 384 + i * 64, 64, (lambda n, i=i: QI[i][0:64, n * 512:(n + 1) * 512]), QIR)
                        proj_fm(W[1], WR[1], 680, 64, (lambda n: KI[0:64, n * 512:(n + 1) * 512]), KIR)

                        def wi_evac(t, ps, psr):
                            K.op(dve, [psr], [WIR], lambda: nc.vector.tensor_copy(out=WI[:, t, :], in_=ps[:, 0:8]))
                        proj_tm(W[1], WR[1], 672, 8, wi_evac)
                        K.op(act, [WIR], [WIR], lambda: nc.scalar.activation(out=WA[:], in_=WI[:], func=AF.Abs))
                        K.op(act, [WIR], [WIR], lambda: nc.scalar.activation(out=WS[:], in_=WI[:], func=AF.Sign))
                        K.op(dve, [], [MZR], lambda: nc.vector.memset(MZ[:], 0.0))
                        def sel(i):
                            t1 = (i + 1) * 128
                            MNEG, MNR = MNB[i % 2], MNRB[i % 2]
                            for kc in range((t1 + 511) // 512):
                                k0 = kc * 512
                                kw = min(512, t1 - k0)
                                for hi in range(8):
                                    b = next_bank(2, 6)
                                    rt, rtr = RT[hi % 2], RTR[hi % 2]
                                    K.op(pe, [QIR, KIR], [PSR[b]], lambda: nc.tensor.matmul(PS[b][:, 0:kw], lhsT=QI[hi // 2][(hi % 2) * 32:(hi % 2) * 32 + 32, i * 128:(i + 1) * 128], rhs=KI[(hi % 2) * 32:(hi % 2) * 32 + 32, k0:k0 + kw], start=True, stop=True))
                                    K.op(act, [PSR[b], WIR], [rtr], lambda: nc.scalar.activation(out=rt[:, 0:kw], in_=PS[b][:, 0:kw], func=AF.Relu, scale=WA[:, i, hi:hi + 1]))
                                    if hi == 0:
                                        K.op(dve, [rtr, WIR], [ISR], lambda: nc.vector.tensor_scalar(out=ISC[:, k0:k0 + kw], in0=rt[:, 0:kw], scalar1=WS[:, i, hi:hi + 1], scalar2=None, op0=ALU.mult))
                                    else:
                                        K.op(dve, [rtr, WIR, ISR], [ISR], lambda: nc.vector.scalar_tensor_tensor(out=ISC[:, k0:k0 + kw], in0=rt[:, 0:kw], scalar=WS[:, i, hi:hi + 1], in1=ISC[:, k0:k0 + kw], op0=ALU.mult, op1=ALU.add))
                            K.op(dve, [ISR], [BR], lambda: nc.vector.tensor_reduce(out=lo[:], in_=ISC[:, 0:t1], axis=AX.X, op=ALU.min))
                            K.op(dve, [ISR], [BR], lambda: nc.vector.tensor_reduce(out=hi_[:], in_=ISC[:, 0:t1], axis=AX.X, op=ALU.max))
                            K.op(dve, [ISR, CR], [ISR], lambda: nc.vector.tensor_tensor(out=ISC[:, i * 128:t1], in0=ISC[:, i * 128:t1], in1=adm[:, :], op=ALU.add))
                            K.op(dve, [BR], [BR], lambda: nc.vector.tensor_tensor(out=hi_[:], in0=hi_[:], in1=lo[:], op=ALU.subtract))
                            K.op(dve, [BR, CR], [BR], lambda: nc.vector.tensor_scalar(out=hw[:], in0=powt[:], scalar1=hi_[:], scalar2=None, op0=ALU.mult))
                            K.op(dve, [BR], [BR], lambda: nc.vector.tensor_tensor(out=mid[:], in0=lo[:], in1=hw[:, 0:1], op=ALU.add))
                            for it in range(NIT):
                                K.op(dve, [BR, ISR], [BR, MNR], lambda: nc.vector.tensor_scalar(out=MNEG[:, 0:t1], in0=ISC[:, 0:t1], scalar1=mid[:], scalar2=0.0, op0=ALU.is_ge, op1=ALU.add, accum_out=cnt[:]))
                                K.op(dve, [BR], [BR], lambda: nc.vector.tensor_scalar(out=tmp1[:], in0=cnt[:], scalar1=float(KSEL) - 0.5, scalar2=hw[:, it:it + 1], op0=ALU.is_ge, op1=ALU.mult))
                                nxt = it + 1 if it + 1 < NIT else it
                                K.op(dve, [BR], [BR], lambda: nc.vector.tensor_scalar(out=mid[:], in0=mid[:], scalar1=hw[:, nxt:nxt + 1], scalar2=tmp1[:], op0=ALU.subtract, op1=ALU.add))
                            K.op(dve, [BR], [BR], lambda: nc.vector.tensor_copy(out=lo[:], in_=mid[:]))
                            K.op(dve, [BR, ISR], [MNR], lambda: nc.vector.tensor_scalar(out=MNEG[:, 0:t1], in0=ISC[:, 0:t1], scalar1=lo[:], scalar2=NEG, op0=ALU.is_lt, op1=ALU.mult))
                        def attn(i):
                            t1 = (i + 1) * 128
                            need_sel = t1 > KSEL
                            MNEG, MNR = MNB[i % 2], MNRB[i % 2]
                            ob = 6
                            def dA(a):
                                xb = a % 2
                                K.op(pe, [MNR if need_sel else MZR, CR], [PSR[xb]], lambda: nc.tensor.matmul(PS[xb][:, :], lhsT=(MNEG[:, a * 128:(a + 1) * 128] if need_sel else MZ[:, :]), rhs=ident4[:, :], start=True, stop=False, skip_group_check=True), inc=False)
                                for h in range(4):
                                    lastmm = (h == 3) and (a < i - 1)
                                    K.op(pe, [KTR[0], QTR[h]], [PSR[xb]], lambda: nc.tensor.matmul(PS[xb][:, h * 128:(h + 1) * 128], lhsT=KT[0][0:64, a * 128:(a + 1) * 128], rhs=QT[h][0:64, i * 128:(i + 1) * 128], start=False, stop=lastmm, skip_group_check=True), inc=lastmm)
                                if a >= i - 1:
                                    for h in range(4):
                                        tb = tbias[:, 4 + h, 0:128] if a == i else tbias[:, 4 + h, 128:256]
                                        K.op(pe, [CR], [PSR[xb]], lambda: nc.tensor.matmul(PS[xb][:, h * 128:(h + 1) * 128], lhsT=ident_b[:, :], rhs=tb, start=False, stop=(h == 3), skip_group_check=True), inc=(h == 3))
                            def dB(a):
                                xb = a % 2
                                pt, ptr = PT[a % 2], PTR[a % 2]
                                K.op(act, [PSR[xb]], [ptr], lambda: nc.scalar.activation(out=pt[:, :], in_=PS[xb][:, :], func=AF.Exp))
                                for h in range(4):
                                    K.op(pe, [ptr, VR], [PSR[ob]], lambda: nc.tensor.matmul(PS[ob][:, h * 65:(h + 1) * 65], lhsT=pt[:, h * 128:(h + 1) * 128], rhs=V[:, a, 0, :], start=False, stop=False, skip_group_check=True), inc=(h == 3))
                            dA(0)
                            for a in range(i + 1):
                                if a + 1 <= i:
                                    dA(a + 1)
                                dB(a)
                            o3 = PS[ob][:, 0:260].rearrange("p (s d) -> p s d", d=65)
                            K.op(dve, [PSR[ob]], [ORR], lambda: nc.vector.reciprocal(out=orec[:, 0:4], in_=o3[:, :, 64]))
                            for h in range(4):
                                K.op(dve, [PSR[ob], ORR], [OKR], lambda: nc.vector.tensor_scalar(out=OTOK[:, i, h * 64:(h + 1) * 64], in0=o3[:, h, 0:64], scalar1=orec[:, h:h + 1], scalar2=None, op0=ALU.mult))
                        for i in range(NT):
                            K.op(dve, [], [PSR[6]], lambda: nc.vector.memset(PS[6][:, 0:260], 0.0))
                            if (i + 2) * 128 > KSEL and i + 1 < NT:
                                sel(i + 1)
                            attn(i)
                        otok_to_oT(3)
                        K.barrier()

                    ast.close()
                    with ExitStack() as st:
                        mixT = sb("mixT", [128, 8, S], BF16, st)
                        MXR = Reg()
                        WG = [sb("WG%d" % i, [128, 4, 8, 128], BF16, st) for i in range(2)]
                        WB = [sb("WB%d" % i, [128, 4, 2, 128], BF16, st) for i in range(2)]
                        WGR = [Reg(), Reg()]
                        SG = [sb("SG%d" % i, [128, 512], F32, st) for i in range(2)]
                        SGR = [Reg(), Reg()]
                        ACC = sb("ACCg", [128, 512], F32, st)
                        TMP = sb("TMPg", [128, 512], F32, st)
                        ACR = Reg()

                        def load_gw(f):
                            s_ = f % 2
                            for i in range(4):
                                K.dma(pool, WG[s_][:, i, :, :], wg_d[l, i][:, f * 128:(f + 1) * 128].rearrange("(c p) n -> p c n", p=128), [], [WGR[s_]])
                                K.dma(pool, WB[s_][:, i, :, :], wb_d[l, i][:, f * 128:(f + 1) * 128].rearrange("(c p) n -> p c n", p=128), [], [WGR[s_]])
                        load_gw(0)
                        for f in range(8):
                            if f + 1 < 8:
                                load_gw(f + 1)
                            s_ = f % 2
                            for n in range(NCH):
                                for i in range(4):
                                    gb = next_bank(0, 3)
                                    bb = 3 + next_bank(0, 3)
                                    for c in range(8):
                                        K.op(pe, [WGR[s_]] + XNR[n * 4:(n + 1) * 4], [PSR[gb]], lambda: nc.tensor.matmul(PS[gb][:, :], lhsT=WG[s_][:, i, c, :], rhs=xnT[:, c, n * 512:(n + 1) * 512], start=(c == 0), stop=(c == 7)), inc=(c == 7))
                                    for k in range(2):
                                        K.op(pe, [WGR[s_], OTR[i]], [PSR[bb]], lambda: nc.tensor.matmul(PS[bb][:, :], lhsT=WB[s_][:, i, k, :], rhs=oT[:, i, k, n * 512:(n + 1) * 512], start=(k == 0), stop=(k == 1)), inc=(k == 1))
                                    sg, sgr = SG[i % 2], SGR[i % 2]
                                    K.op(act, [PSR[gb]], [sgr], lambda: nc.scalar.activation(out=sg[:], in_=PS[gb][:, :], func=AF.Sigmoid))
                                    if i == 0:
                                        K.op(dve, [sgr, PSR[bb]], [ACR], lambda: nc.vector.tensor_tensor(out=ACC[:], in0=sg[:], in1=PS[bb][:, :], op=ALU.mult))
                                    else:
                                        K.op(dve, [sgr, PSR[bb]], [ACR], lambda: nc.vector.tensor_tensor(out=TMP[:], in0=sg[:], in1=PS[bb][:, :], op=ALU.mult))
                                        if i < 3:
                                            K.op(dve, [ACR], [ACR], lambda: nc.vector.tensor_tensor(out=ACC[:], in0=ACC[:], in1=TMP[:], op=ALU.add))
                                        else:
                                            K.op(dve, [ACR], [MXR], lambda: nc.vector.tensor_tensor(out=mixT[:, f, n * 512:(n + 1) * 512], in0=ACC[:], in1=TMP[:], op=ALU.add))
                        WO = sb("WO", [128, 8, D], BF16, st)
                        WOR = Reg()
                        K.dma(pool, WO[:], wo_d[l].rearrange("(c p) n -> p c n", p=128), [], [WOR])
                        NBF = alloc_normbufs(st)
                        load_g(gffn_d[l])
                        def wo_mm(t):
                            banks = []
                            for hb in range(2):
                                b = (t % 2) * 2 + hb
                                for c in range(8):
                                    K.op(pe, [MXR, WOR], [PSR[b]], lambda: nc.tensor.matmul(PS[b][:, :], lhsT=mixT[:, c, t * 128:(t + 1) * 128], rhs=WO[:, c, hb * 512:(hb + 1) * 512], start=(c == 0), stop=(c == 7)), inc=(c == 7))
                                banks.append((PS[b], PSR[b]))
                            return banks
                        nxt_b = wo_mm(0)
                        for t in range(NT):
                            cur_b = nxt_b
                            if t + 1 < NT:
                                nxt_b = wo_mm(t + 1)
                            norm_tile(NBF, seq, t, y_d[seq, t * 128:(t + 1) * 128, :], cur_b, False, t % 2)
                        K.barrier()
                        if dbg and seq == 0 and l == 0:
                            K.dma(sp, dbg_d[4], y_d[0], [], [])
                            K.barrier()

                with ExitStack() as fst:
                    NH = S // TOKH
                    NCF = TOKH // 512
                    actT = sb("actT", [128, NJ, TOKH], BF16, fst)
                    ATR = Reg()
                    WD = sb("WD", [128, NJ, D], BF16, fst)
                    WDR = Reg()
                    K.dma(pool, WD[:, 0:11, :], wdn_d[l][0:11 * 128, :].rearrange("(c p) n -> p c n", p=128), [], [WDR])
                    K.dma(pool, WD[:, 11:22, :], wdn_d[l][11 * 128:22 * 128, :].rearrange("(c p) n -> p c n", p=128), [], [WDR])
                    HB = [[sb("HB%d%d" % (i, g), [128, TOKH + 2], F32, fst) for g in range(2)] for i in range(2)]
                    HBR = [[Reg(), Reg()], [Reg(), Reg()]]
                    HALO = sb("HALO", [128, NJ, 2, 2], F32, fst)
                    HLR = Reg()
                    WU = [sb("WU%d" % i, [128, 2, 8, 256], BF16, fst) for i in range(2)]
                    WUR = [Reg(), Reg()]
                    CWt = sb("CWt", [128, 4, 2 * NJ], F32, fst)
                    CWc = sb("CWc", [2 * NJ, 4, 128], F32, fst)
                    CWR = Reg()
                    for j_ in range(3):
                        K.dma(sp, CWc[:, j_, :], cw_d[l, j_].rearrange("(c p) -> c p", p=128), [], [CWR])
                    K.dma(sp, CWc[:, 3, :], cb_d[l].rearrange("(c p) -> c p", p=128), [], [CWR])
                    for j_ in range(4):
                        K.op(pe, [CWR, CR], [PSR[6]], lambda: nc.tensor.transpose(PS[6][:, j_ * 2 * NJ:(j_ + 1) * 2 * NJ], CWc[:, j_, :], ident_f[0:2 * NJ, 0:2 * NJ]), inc=(j_ == 3))
                    K.op(dve, [PSR[6]], [CWR], lambda: nc.vector.tensor_copy(out=CWt[:], in_=PS[6][:, 0:8 * NJ].rearrange("p (a c) -> p a c", c=2 * NJ)))
                    CA2 = [[[sb("CA%d_%d_%d" % (p_, g, n_), [128, 512], F32, fst) for n_ in range(TOKH // 512)] for g in range(2)] for p_ in range(2)]
                    CAR2 = [[[Reg() for n_ in range(TOKH // 512)] for g in range(2)] for p_ in range(2)]
                    GL2 = [sb("GL%d" % i_, [128, 512], F32, fst) for i_ in range(2)]
                    GLR2 = [Reg(), Reg()]
                    NBF = alloc_normbufs(fst)
                    final = (l == DEPTH - 1)
                    load_g(gfin_d[0] if final else gmix_d[l + 1])

                    def load_wu(jp):
                        s_ = jp % 2
                        for g in range(2):
                            K.dma(pool, WU[s_][:, g, :, :], wup_d[l][:, g * DFF + jp * 256:g * DFF + (jp + 1) * 256].rearrange("(c p) n -> p c n", p=128), [], [WUR[s_]])

                    for half in range(NH):
                        tk0 = half * TOKH
                        load_wu(0)

                        def st1(j):
                            if j % 2 == 0 and j // 2 + 1 < NJ // 2:
                                load_wu(j // 2 + 1)
                            s_ = j % 2
                            ws_ = (j // 2) % 2
                            wo_ = (j % 2) * 128
                            CA, CAR = CA2[s_], CAR2[s_]
                            for g in range(2):
                                hbuf, hbr = HB[s_][g], HBR[s_][g]
                                if half == 0:
                                    K.op(dve, [], [hbr], lambda: nc.vector.memset(hbuf[:, 0:2], 0.0))
                                else:
                                    K.op(dve, [HLR], [hbr], lambda: nc.vector.tensor_copy(out=hbuf[:, 0:2], in_=HALO[:, j, g, :]))
                                for n in range(NCF):
                                    b = next_bank(0, 6)
                                    tn = (tk0 + n * 512) // 128
                                    for c in range(8):
                                        K.op(pe, [WUR[ws_]] + XNR[tn:tn + 4], [PSR[b]], lambda: nc.tensor.matmul(PS[b][:, :], lhsT=WU[ws_][:, g, c, wo_:wo_ + 128], rhs=xnT[:, c, tk0 + n * 512:tk0 + (n + 1) * 512], start=(c == 0), stop=(c == 7)), inc=(c == 7))
                                    K.op(act, [PSR[b]], [hbr], lambda: nc.scalar.copy(out=hbuf[:, 2 + n * 512:2 + (n + 1) * 512], in_=PS[b][:, :]))
                                    ch_ = g * NJ + j
                                    K.op(act, [PSR[b], CWR], [CAR[g][n]], lambda: nc.scalar.activation(out=CA[g][n][:], in_=PS[b][:, :], func=AF.Identity, scale=CWt[:, 2, ch_:ch_ + 1], bias=CWt[:, 3, ch_:ch_ + 1]))
                                if half + 1 < NH:
                                    K.op(dve, [hbr], [HLR], lambda: nc.vector.tensor_copy(out=HALO[:, j, g, :], in_=hbuf[:, TOKH:TOKH + 2]))

                        def st2(j):
                            s_ = j % 2
                            CA, CAR = CA2[s_], CAR2[s_]
                            for n in range(NCF):
                                for g in range(2):
                                    hbuf, hbr = HB[s_][g], HBR[s_][g]
                                    ch = g * NJ + j
                                    ca, car = CA[g][n], CAR[g][n]
                                    K.op(dve, [hbr, CWR, car], [car], lambda: nc.vector.scalar_tensor_tensor(out=ca[:], in0=hbuf[:, 1 + n * 512:1 + (n + 1) * 512], scalar=CWt[:, 1, ch:ch + 1], in1=ca[:], op0=ALU.mult, op1=ALU.add))
                                    K.op(dve, [hbr, CWR, car], [car], lambda: nc.vector.scalar_tensor_tensor(out=ca[:], in0=hbuf[:, 0 + n * 512:0 + (n + 1) * 512], scalar=CWt[:, 0, ch:ch + 1], in1=ca[:], op0=ALU.mult, op1=ALU.add))
                                gl, glr = GL2[n % 2], GLR2[n % 2]
                                K.op(act, [CAR[0][n]], [glr], lambda: nc.scalar.activation(out=gl[:], in_=CA[0][n][:], func=AF.Gelu))
                                K.op(dve, [glr, CAR[1][n]], [ATR], lambda: nc.vector.tensor_tensor(out=actT[:, j, n * 512:(n + 1) * 512], in0=gl[:], in1=CA[1][n][:], op=ALU.mult))
                        st1(0)
                        for j in range(NJ):
                            if j + 1 < NJ:
                                st1(j + 1)
                            st2(j)
                        def dn_mm(tt):
                            t = tk0 // 128 + tt
                            banks = []
                            for hb in range(2):
                                b = (t % 2) * 2 + hb
                                for j in range(NJ):
                                    K.op(pe, [ATR, WDR], [PSR[b]], lambda: nc.tensor.matmul(PS[b][:, :], lhsT=actT[:, j, tt * 128:(tt + 1) * 128], rhs=WD[:, j, hb * 512:(hb + 1) * 512], start=(j == 0), stop=(j == NJ - 1)), inc=(j == NJ - 1))
                                banks.append((PS[b], PSR[b]))
                            return banks
                        nxt_b = dn_mm(0)
                        for tt in range(TOKH // 128):
                            t = tk0 // 128 + tt
                            cur_b = nxt_b
                            if tt + 1 < TOKH // 128:
                                nxt_b = dn_mm(tt + 1)
                            norm_tile(NBF, seq, t, y_d[seq, t * 128:(t + 1) * 128, :], cur_b, final, t % 2)
                        K.barrier()
                    K.barrier()
        K.barrier()
    return nc, hc


_CACHE = {}


def kernel(**inputs):
    S, DEPTH, NSEQ, NCORE = 2048, 4, 2, 8
    if "prog" not in _CACHE:
        _CACHE["prog"] = build(S, DEPTH, NSEQ)
    nc, hc = _CACHE["prog"]
    f = lambda a: np.ascontiguousarray(np.asarray(a, dtype=np.float32))
    x = f(inputs["x"])
    shared = {k: f(inputs[k]) for k in ["norm_mix_g", "norm_ffn_g", "w_in", "fox_b_f", "w_gate", "w_branch", "w_out", "w_up", "conv_w", "conv_b", "w_down"]}
    shared["norm_final_g"] = f(inputs["norm_final_g"]).reshape(1, D)
    shared["t5_bias"] = f(inputs["t5_bias"]).reshape(1, 256)
    shared.update(hc)
    in_maps = []
    for c in range(NCORE):
        m = dict(shared)
        m["x"] = np.ascontiguousarray(x[c * NSEQ:(c + 1) * NSEQ])
        in_maps.append(m)
    res = run_bass_kernel_spmd(nc, in_maps, core_ids=list(range(NCORE)))
    return np.concatenate([r["y"] for r in res.results], axis=0).astype(np.float32)
```
